# Optimizing a Trainium2 kernel written in Bass

```python
import math
import jax, jax.numpy as jnp
from jax import lax
import numpy as np

D_MODEL = 2048
BATCH = 2
SEQ = 16384
DEPTH = 2

MIX_WIDTH = D_MODEL
HEAD_DIM = 128
Q_BLOCK = 128
SB_WIDTH = MIX_WIDTH // 2
SB_HEADS = SB_WIDTH // HEAD_DIM
POOL_WINDOWS = (2, 4, 8, 16)
POOL_GROUPS = len(POOL_WINDOWS)
POOL_DIM = (MIX_WIDTH // 2) // POOL_GROUPS
DSA_WIDTH = MIX_WIDTH // 2
DSA_HEADS = DSA_WIDTH // HEAD_DIM
DSA_Q_RANK = D_MODEL // 8
IDX_HEADS = 16
IDX_DIM = 64
DSA_TOPK = 256
DELTA_WIDTH = MIX_WIDTH // 2
DELTA_HEADS = DELTA_WIDTH // HEAD_DIM
DELTA_CONV = 4
DELTA_CHUNK = 64
REL_BUCKETS = 32
REL_MAX_DIST = 2048
MEM_TOKENS = 256
XATTN_HEADS = 4
XATTN_DIM = 128
PEER_HEADS = 8
PEER_KEYS = 128
PEER_EXPERTS = PEER_KEYS * PEER_KEYS
PEER_QUERY_DIM = 256
PEER_TOPK = 16
PEER_BLOCK = 128

kernel_name = 'hybrid_sb_pool_dsa_gdn_peer_trunk'

F32 = jnp.float32


def rms_norm(x, g, eps=1e-6):
    xf = x.astype(F32)
    return (xf * lax.rsqrt(jnp.mean(xf * xf, axis=-1, keepdims=True) + eps)).astype(x.dtype) * g


def l2_norm(t, eps=1e-6):
    tf = t.astype(F32)
    return (tf * lax.rsqrt(jnp.sum(tf * tf, axis=-1, keepdims=True) + eps)).astype(t.dtype)


def split_cols(t, sizes):
    return jnp.split(t, np.cumsum(sizes)[:-1].tolist(), axis=-1)


def to_blocks(t, size):
    b, s = t.shape[:2]
    return jnp.moveaxis(t.reshape(b, s // size, size, *t.shape[2:]), 1, 0)


def from_blocks(t):
    t = jnp.moveaxis(t, 0, 1)
    return t.reshape(t.shape[0], t.shape[1] * t.shape[2], *t.shape[3:])


def t5_bucket(dist):
    n = jnp.maximum(dist, 0)
    exact = REL_BUCKETS // 2
    nf = jnp.maximum(n, 1).astype(F32)
    log_ratio = jnp.log(nf / exact) / math.log(REL_MAX_DIST / exact)
    large = exact + (log_ratio * (REL_BUCKETS - exact)).astype(jnp.int32)
    return jnp.where(n < exact, n, jnp.minimum(large, REL_BUCKETS - 1))


def stick_breaking_attention(q, k, v):
    s = q.shape[1]
    scale = HEAD_DIM ** -0.5
    kpos = jnp.arange(s)

    def block(args):
        q_blk, bi = args
        qpos = bi * Q_BLOCK + jnp.arange(Q_BLOCK)
        z = jnp.einsum('bqhd,bshd->bhqs', q_blk, k).astype(F32) * scale
        before = kpos[None, :] < qpos[:, None]
        log_keep = jnp.where(before, jax.nn.log_sigmoid(-z), 0.0)
        between = lax.cumsum(log_keep, axis=3, reverse=True) - log_keep
        w = jnp.where(before, jnp.exp(jax.nn.log_sigmoid(z) + between), 0.0)
        return jnp.einsum('bhqs,bshd->bqhd', w.astype(v.dtype), v)

    out = lax.map(block, (to_blocks(q, Q_BLOCK), jnp.arange(s // Q_BLOCK)))
    return from_blocks(out)


def multiscale_pool(u, pool_w, pool_scale):
    b, s, c = u.shape
    uf = u.astype(F32)
    cs = jnp.concatenate([jnp.zeros((b, 1, c), F32), jnp.cumsum(uf, axis=1)], axis=1)
    end = jnp.arange(1, s + 1)
    outs = []
    for gi, win in enumerate(POOL_WINDOWS):
        sl = slice(gi * POOL_DIM, (gi + 1) * POOL_DIM)
        start = jnp.maximum(end - win, 0)
        cg = cs[..., sl]
        mean = (cg[:, end] - cg[:, start]) / (end - start).astype(F32)[None, :, None]
        outs.append(mean - uf[..., sl])
    d = jnp.stack(outs, axis=2).astype(u.dtype)
    y = jnp.einsum('bsgc,gcd->bsgd', d, pool_w)
    return y.reshape(b, s, c) * pool_scale


def dsa_sparse_attention(q, k, v, q_idx, k_idx, w_idx, rel_bias):
    s = q.shape[1]
    topk = min(DSA_TOPK, s // 4)
    kpos = jnp.arange(s)
    scale = HEAD_DIM ** -0.5

    def block(args):
        q_blk, qi_blk, wi_blk, bi = args
        qpos = bi * Q_BLOCK + jnp.arange(Q_BLOCK)
        rel = jax.nn.relu(jnp.einsum('bqhd,bsd->bqhs', qi_blk, k_idx) * IDX_DIM ** -0.5)
        score = jnp.einsum('bqh,bqhs->bqs', wi_blk, rel).astype(F32)
        score = jnp.where((kpos[None, :] <= qpos[:, None])[None], score, -jnp.inf)
        _, sel = lax.top_k(score, topk)
        valid = sel <= qpos[None, :, None]
        k_sel = jax.vmap(lambda kb, ib: kb[ib])(k, sel)
        v_sel = jax.vmap(lambda vb, ib: vb[ib])(v, sel)
        bias = rel_bias[t5_bucket(qpos[None, :, None] - sel)]
        logits = jnp.einsum('bqhd,bqkhd->bqhk', q_blk, k_sel).astype(F32) * scale
        logits = logits + jnp.swapaxes(bias, -1, -2).astype(F32)
        logits = jnp.where(valid[:, :, None, :], logits, -jnp.inf)
        p = jax.nn.softmax(logits, axis=-1)
        return jnp.einsum('bqhk,bqkhd->bqhd', p.astype(v.dtype), v_sel)

    xs = (to_blocks(q, Q_BLOCK), to_blocks(q_idx, Q_BLOCK), to_blocks(w_idx, Q_BLOCK), jnp.arange(s // Q_BLOCK))
    return from_blocks(lax.map(block, xs))


def causal_depthwise_conv(x, w):
    kw, c = w.shape
    return lax.conv_general_dilated(x, w[:, None, :].astype(x.dtype), window_strides=(1,),
                                    padding=((kw - 1, 0),), dimension_numbers=('NWC', 'WIO', 'NWC'),
                                    feature_group_count=c)


def gated_delta_rule(q, k, v, g, beta):
    out_dtype = v.dtype
    q, k, v, g, beta = (t.astype(F32) for t in (q, k, v, g, beta))
    b, s, h, dk = q.shape
    dv = v.shape[-1]
    c = DELTA_CHUNK

    def chunks(t):
        return jnp.moveaxis(t.reshape(b, s // c, c, h, *t.shape[3:]), 3, 1)

    qc = chunks(q * dk ** -0.5)
    kc = chunks(k)
    vc = chunks(v)
    gc = jnp.cumsum(chunks(g), axis=-1)
    bc = chunks(beta)
    kb = kc * bc[..., None]
    lower = jnp.tril(jnp.ones((c, c), bool))
    decay = jnp.exp(jnp.where(lower, gc[..., :, None] - gc[..., None, :], -jnp.inf))
    strict = jnp.where(jnp.tril(jnp.ones((c, c), bool), -1),
                       jnp.einsum('bhncd,bhnsd->bhncs', kb, kc) * decay, 0.0)
    unit_lower = strict + jnp.eye(c, dtype=F32)
    rhs = jnp.concatenate([vc * bc[..., None], kb * jnp.exp(gc)[..., None]], axis=-1)
    sol = lax.linalg.triangular_solve(unit_lower, rhs, left_side=True, lower=True, unit_diagonal=True)
    u, w = sol[..., :dv], sol[..., dv:]
    a_intra = jnp.einsum('bhncd,bhnsd->bhncs', qc, kc) * decay

    def step(state, xs):
        q_i, k_i, u_i, w_i, g_i, a_i = xs
        v_new = u_i - jnp.einsum('bhcd,bhde->bhce', w_i, state)
        o = (jnp.einsum('bhcd,bhde->bhce', q_i * jnp.exp(g_i)[..., None], state)
             + jnp.einsum('bhcs,bhse->bhce', a_i, v_new))
        g_last = g_i[..., -1:]
        state = (state * jnp.exp(g_last)[..., None]
                 + jnp.einsum('bhcd,bhce->bhde', k_i * jnp.exp(g_last - g_i)[..., None], v_new))
        return state, o

    xs = tuple(jnp.moveaxis(t, 2, 0) for t in (qc, kc, u, w, gc, a_intra))
    _, o = lax.scan(step, jnp.zeros((b, h, dk, dv), F32), xs)
    return jnp.transpose(o, (1, 0, 3, 2, 4)).reshape(b, s, h, dv).astype(out_dtype)


def stick_pool_mixer(hn, w_in, pool_w, pool_scale, w_out):
    b, s, _ = hn.shape
    q, k, v, u = split_cols(hn @ w_in, [SB_WIDTH, SB_WIDTH, SB_WIDTH, POOL_GROUPS * POOL_DIM])
    heads = lambda t: t.reshape(b, s, SB_HEADS, HEAD_DIM)
    o_a = stick_breaking_attention(heads(q), heads(k), heads(v)).reshape(b, s, SB_WIDTH)
    o_b = multiscale_pool(u, pool_w, pool_scale)
    return jnp.concatenate([o_a, o_b], axis=-1) @ w_out


def dsa_delta_mixer(hn, w_in, w_uq, w_iq, norm_cq, norm_kidx, conv_w, a_log, dt_bias, norm_out, w_out, rel_bias):
    b, s, _ = hn.shape
    c_q, k_c, v_c, k_i, w_i, qkv_d, beta_d, a_d, z_d = split_cols(
        hn @ w_in, [DSA_Q_RANK, DSA_WIDTH, DSA_WIDTH, IDX_DIM, IDX_HEADS,
                    3 * DELTA_WIDTH, DELTA_HEADS, DELTA_HEADS, DELTA_WIDTH])
    c_q = rms_norm(c_q, norm_cq)
    q_c = (c_q @ w_uq).reshape(b, s, DSA_HEADS, HEAD_DIM)
    q_i = (c_q @ w_iq).reshape(b, s, IDX_HEADS, IDX_DIM)
    k_i = rms_norm(k_i, norm_kidx)
    w_i = w_i * IDX_HEADS ** -0.5
    o_c = dsa_sparse_attention(q_c, k_c.reshape(b, s, DSA_HEADS, HEAD_DIM), v_c.reshape(b, s, DSA_HEADS, HEAD_DIM),
                               q_i, k_i, w_i, rel_bias).reshape(b, s, DSA_WIDTH)
    qkv = jax.nn.silu(causal_depthwise_conv(qkv_d, conv_w))
    q_d, k_d, v_d = [t.reshape(b, s, DELTA_HEADS, HEAD_DIM) for t in jnp.split(qkv, 3, axis=-1)]
    beta = jax.nn.sigmoid(beta_d)
    g = -jnp.exp(a_log) * jax.nn.softplus(a_d + dt_bias)
    o_d = gated_delta_rule(l2_norm(q_d), l2_norm(k_d), v_d, g, beta)
    o_d = (rms_norm(o_d, norm_out) * jax.nn.silu(z_d.reshape(b, s, DELTA_HEADS, HEAD_DIM))).reshape(b, s, DELTA_WIDTH)
    return jnp.concatenate([o_c, o_d], axis=-1) @ w_out


def memory_cross_attention(hn, mem_n, w_q, w_kv, w_o):
    b, s, _ = hn.shape
    q = (hn @ w_q).reshape(b, s, XATTN_HEADS, XATTN_DIM)
    kv = (mem_n @ w_kv).reshape(b, mem_n.shape[1], 2, XATTN_HEADS, XATTN_DIM)
    logits = jnp.einsum('bshd,bmhd->bhsm', q, kv[:, :, 0]).astype(F32) * XATTN_DIM ** -0.5
    p = jax.nn.softmax(logits, axis=-1).astype(hn.dtype)
    o = jnp.einsum('bhsm,bmhd->bshd', p, kv[:, :, 1]).reshape(b, s, XATTN_HEADS * XATTN_DIM)
    return o @ w_o


def peer_ffn(hn, w_q, sub_keys, u_tab, v_tab):
    b, s, d = hn.shape
    half = PEER_QUERY_DIM // 2

    def block(xb):
        t = xb.shape[0]
        q = (xb @ w_q).reshape(t, PEER_HEADS, 2, half)
        sub = jnp.einsum('thpd,hpnd->thpn', q, sub_keys).astype(F32)
        sv, si = lax.top_k(sub, PEER_TOPK)
        cand_s = (sv[:, :, 0, :, None] + sv[:, :, 1, None, :]).reshape(t, PEER_HEADS, PEER_TOPK * PEER_TOPK)
        cand_e = (si[:, :, 0, :, None] * PEER_KEYS + si[:, :, 1, None, :]).reshape(t, PEER_HEADS, PEER_TOPK * PEER_TOPK)
        top_s, pos = lax.top_k(cand_s, PEER_TOPK)
        expert = jnp.take_along_axis(cand_e, pos, axis=-1)
        gate = jax.nn.softmax(top_s, axis=-1).astype(xb.dtype)
        act = jax.nn.gelu(jnp.einsum('thkd,td->thk', u_tab[expert], xb), approximate=False)
        return jnp.einsum('thk,thkd->td', gate * act, v_tab[expert])

    out = lax.map(block, hn.reshape(-1, PEER_BLOCK, d))
    return out.reshape(b, s, d)


def setup_inputs(seed: int = 0) -> dict:
    key = jax.random.key(seed)
    ks = jax.random.split(key, 32)
    n_even = (DEPTH + 1) // 2
    n_odd = DEPTH // 2
    nrm = lambda k, shape, scale: jax.random.normal(k, shape, F32) * scale
    gain = lambda k, shape: 1.0 + 0.02 * jax.random.normal(k, shape, F32)
    in_ab = 3 * SB_WIDTH + POOL_GROUPS * POOL_DIM
    in_cd = DSA_Q_RANK + 2 * DSA_WIDTH + IDX_DIM + IDX_HEADS + 4 * DELTA_WIDTH + 2 * DELTA_HEADS
    dt = jnp.exp(jax.random.uniform(ks[17], (n_odd, DELTA_HEADS), F32, math.log(1e-3), math.log(1e-1)))
    return {
        'x': nrm(ks[0], (BATCH, SEQ, D_MODEL), 1.0),
        'mem': nrm(ks[1], (BATCH, MEM_TOKENS, D_MODEL), 1.0),
        'norm_mix': gain(ks[2], (DEPTH, D_MODEL)),
        'norm_cross': gain(ks[3], (DEPTH, D_MODEL)),
        'norm_mem': gain(ks[4], (DEPTH, D_MODEL)),
        'norm_ffn': gain(ks[5], (DEPTH, D_MODEL)),
        'norm_final': gain(ks[6], (D_MODEL,)),
        'w_in_ab': nrm(ks[7], (n_even, D_MODEL, in_ab), D_MODEL ** -0.5),
        'pool_w': nrm(ks[8], (n_even, POOL_GROUPS, POOL_DIM, POOL_DIM), POOL_DIM ** -0.5),
        'pool_scale': gain(ks[9], (n_even, POOL_GROUPS * POOL_DIM)),
        'w_out_ab': nrm(ks[10], (n_even, MIX_WIDTH, D_MODEL), MIX_WIDTH ** -0.5),
        'w_in_cd': nrm(ks[11], (n_odd, D_MODEL, in_cd), D_MODEL ** -0.5),
        'w_uq': nrm(ks[12], (n_odd, DSA_Q_RANK, DSA_WIDTH), DSA_Q_RANK ** -0.5),
        'w_iq': nrm(ks[13], (n_odd, DSA_Q_RANK, IDX_HEADS * IDX_DIM), DSA_Q_RANK ** -0.5),
        'norm_cq': gain(ks[14], (n_odd, DSA_Q_RANK)),
        'norm_kidx': gain(ks[15], (n_odd, IDX_DIM)),
        'conv_w': nrm(ks[16], (n_odd, DELTA_CONV, 3 * DELTA_WIDTH), DELTA_CONV ** -0.5),
        'a_log': jnp.log(jax.random.uniform(ks[18], (n_odd, DELTA_HEADS), F32, 1.0, 16.0)),
        'dt_bias': dt + jnp.log(-jnp.expm1(-dt)),
        'norm_delta_out': gain(ks[19], (n_odd, HEAD_DIM)),
        'w_out_cd': nrm(ks[20], (n_odd, MIX_WIDTH, D_MODEL), MIX_WIDTH ** -0.5),
        'rel_bias': nrm(ks[21], (REL_BUCKETS, DSA_HEADS), 0.5),
        'xattn_wq': nrm(ks[22], (DEPTH, D_MODEL, XATTN_HEADS * XATTN_DIM), D_MODEL ** -0.5),
        'xattn_wkv': nrm(ks[23], (DEPTH, D_MODEL, 2 * XATTN_HEADS * XATTN_DIM), D_MODEL ** -0.5),
        'xattn_wo': nrm(ks[24], (DEPTH, XATTN_HEADS * XATTN_DIM, D_MODEL), (XATTN_HEADS * XATTN_DIM) ** -0.5),
        'peer_wq': nrm(ks[25], (DEPTH, D_MODEL, PEER_HEADS * PEER_QUERY_DIM), D_MODEL ** -0.5),
        'peer_subkeys': nrm(ks[26], (DEPTH, PEER_HEADS, 2, PEER_KEYS, PEER_QUERY_DIM // 2), (PEER_QUERY_DIM // 2) ** -0.5),
        'peer_u': nrm(ks[27], (DEPTH, PEER_EXPERTS, D_MODEL), D_MODEL ** -0.5),
        'peer_v': nrm(ks[28], (DEPTH, PEER_EXPERTS, D_MODEL), (PEER_HEADS * PEER_TOPK) ** -0.5),
    }


def reference(x, mem, norm_mix, norm_cross, norm_mem, norm_ffn, norm_final, w_in_ab, pool_w, pool_scale,
              w_out_ab, w_in_cd, w_uq, w_iq, norm_cq, norm_kidx, conv_w, a_log, dt_bias, norm_delta_out,
              w_out_cd, rel_bias, xattn_wq, xattn_wkv, xattn_wo, peer_wq, peer_subkeys, peer_u, peer_v):
    h = x
    for layer in range(DEPTH):
        j = layer // 2
        hn = rms_norm(h, norm_mix[layer])
        if layer % 2 == 0:
            mix = stick_pool_mixer(hn, w_in_ab[j], pool_w[j], pool_scale[j], w_out_ab[j])
        else:
            mix = dsa_delta_mixer(hn, w_in_cd[j], w_uq[j], w_iq[j], norm_cq[j], norm_kidx[j], conv_w[j],
                                  a_log[j], dt_bias[j], norm_delta_out[j], w_out_cd[j], rel_bias)
        h = h + mix
        h = h + memory_cross_attention(rms_norm(h, norm_cross[layer]), rms_norm(mem, norm_mem[layer]),
                                       xattn_wq[layer], xattn_wkv[layer], xattn_wo[layer])
        h = h + peer_ffn(rms_norm(h, norm_ffn[layer]), peer_wq[layer], peer_subkeys[layer],
                         peer_u[layer], peer_v[layer])
    return rms_norm(h, norm_final)
```

```python
from contextlib import ExitStack
import numpy as np
import math
import concourse.bass as bass
import concourse.mybir as mybir
from concourse.bass_utils import run_bass_kernel_spmd

F32 = mybir.dt.float32
BF16 = mybir.dt.bfloat16
I32 = mybir.dt.int32
U32 = mybir.dt.uint32
AF = mybir.ActivationFunctionType
ALU = mybir.AluOpType
AX = mybir.AxisListType

SAME_ENGINE_SYNC = True
NDMA_SEM = 6


class T:
    def __init__(self, h, parent=None, atomic=False):
        self.h = h
        self.parent = parent
        self.atomic = atomic
        self.kids = {}
        self.w = None
        self.r = []

    def sub(self, key):
        if self.atomic:
            return self
        if key not in self.kids:
            self.kids[key] = T(self.h, self)
        return self.kids[key]

    def __getitem__(self, idx):
        return self.h[idx]

    def _related(self):
        out = [self]
        p = self.parent
        while p is not None:
            out.append(p)
            p = p.parent
        stack = list(self.kids.values())
        while stack:
            k = stack.pop()
            out.append(k)
            stack.extend(k.kids.values())
        return out


class KB:
    def __init__(self):
        self.nc = bass.Bass("TRN2", target_bir_lowering=False)
        nc = self.nc
        self.es = ExitStack()
        self.es.enter_context(nc.allow_low_precision("bf16 matmul operands, fp32 accumulation"))
        self.eng = {"pe": nc.tensor, "act": nc.scalar, "dve": nc.vector, "pool": nc.gpsimd, "sp": nc.sync}
        self.sem = {}
        self.cnt = {}
        for e in ("pe", "act", "dve", "pool"):
            self.sem[e] = self.es.enter_context(nc.semaphore("s_" + e))
            self.cnt[e] = 0
        self.dsem = {}
        self.dcnt = {}
        for q in ("sp", "pool", "act"):
            self.dsem[q] = [self.es.enter_context(nc.semaphore("d_%s%d" % (q, i))) for i in range(NDMA_SEM)]
            self.dcnt[q] = 0
        self.waited = {e: {} for e in self.eng}
        self.out_tokens = []
        self.n_inst = 0
        self._uid = 0

    def dram(self, name, shape, dt, kind):
        return self.nc.dram_tensor(name, list(shape), dt, kind=kind).ap()

    def sb(self, shape, dt, name=None):
        self._uid += 1
        h = self.es.enter_context(self.nc.sbuf_tensor(name or ("t%d" % self._uid), list(shape), dt))
        return T(h)

    def ps(self, shape, dt=F32, name=None):
        self._uid += 1
        h = self.es.enter_context(self.nc.psum_tensor(name or ("p%d" % self._uid), list(shape), dt))
        return T(h, atomic=True)

    def _wait(self, e, tok):
        if tok is None:
            return
        sem, val, src = tok
        if src == e and (e == "pe" or not SAME_ENGINE_SYNC):
            return
        if src == e and e == "sp":
            pass
        w = self.waited[e]
        if w.get(sem.name, 0) >= val:
            return
        self.eng[e].wait_ge(sem, val)
        w[sem.name] = val

    def _deps(self, e, reads, writes):
        for t in reads:
            for x in t._related():
                self._wait(e, x.w)
        for t in writes:
            for x in t._related():
                self._wait(e, x.w)
                for tok in x.r:
                    self._wait(e, tok)

    def _commit(self, tok, reads, writes):
        for t in reads:
            t.r.append(tok)
            if len(t.r) > 24:
                t.r = t.r[-24:] if False else self._compact(t.r)
        for t in writes:
            t.w = tok
            t.r = []
            stack = list(t.kids.values())
            while stack:
                k = stack.pop()
                k.w = None
                k.r = []
                stack.extend(k.kids.values())

    @staticmethod
    def _compact(toks):
        best = {}
        for sem, val, src in toks:
            k = sem.name
            if k not in best or best[k][1] < val:
                best[k] = (sem, val, src)
        return list(best.values())

    def op(self, e, fn, reads=(), writes=()):
        self._deps(e, reads, writes)
        ins = fn(self.eng[e])
        self.cnt[e] += 1
        ins.then_inc(self.sem[e], 1)
        tok = (self.sem[e], self.cnt[e], e)
        self._commit(tok, reads, writes)
        self.n_inst += 1
        return tok

    def dma(self, q, out, in_, reads=(), writes=(), is_output=False, **kw):
        i = self.dcnt[q]
        k = i % NDMA_SEM
        sem = self.dsem[q][k]
        prev = 16 * (i // NDMA_SEM)
        if prev > 0:
            w = self.waited[q]
            if w.get(sem.name, 0) < prev:
                self.eng[q].wait_ge(sem, prev)
                w[sem.name] = prev
        self._deps(q, reads, writes)
        ins = self.eng[q].dma_start(out=out, in_=in_, **kw)
        ins.then_inc(sem, 16)
        self.dcnt[q] += 1
        tok = (sem, prev + 16, "dma_" + q)
        self._commit(tok, reads, writes)
        if is_output:
            self.out_tokens.append(tok)
        self.n_inst += 1
        return tok

    def finish(self):
        for tok in self.out_tokens:
            self._wait("sp", tok)
        for e in ("pe", "act", "dve", "pool"):
            if self.cnt[e] > 0:
                self._wait("sp", (self.sem[e], self.cnt[e], e))
        for q in self.dsem:
            i = self.dcnt[q]
            for k in range(NDMA_SEM):
                n = (i - k + NDMA_SEM - 1) // NDMA_SEM if i > k else 0
                if n > 0:
                    self._wait("sp", (self.dsem[q][k], 16 * n, "dma_" + q))
        self.es.close()
        return self.nc


def run(nc, in_maps, n=8):
    res = run_bass_kernel_spmd(nc, in_maps, core_ids=list(range(n)))
    return res.results


D = 2048
NCH = D // 128
EPS = 1e-6


def emit_identity(kb, dt=BF16):
    ident = kb.sb([128, 128], dt, "ident")
    kb.op("pool", lambda e: e.memset(ident[:], 1.0), writes=[ident])
    kb.op("pool", lambda e: e.affine_select(out=ident[:], in_=ident[:], pattern=[[-1, 128]],
                                             compare_op=ALU.is_equal, fill=0.0, base=0,
                                             channel_multiplier=1), reads=[ident], writes=[ident])
    return ident


def bf16_view(pt):
    ap = pt[:]
    if ap.dtype == BF16:
        return ap
    return ap.bitcast(BF16)


class NormT:
    def __init__(self, kb, ident, nbuf=2, pts=None):
        self.kb = kb
        self.ident = ident
        self.xt = [kb.sb([128, D], F32, "nx%d" % i) for i in range(nbuf)]
        self.sq = kb.sb([128, D], BF16, "nsq")
        self.xn = [kb.sb([128, D], BF16, "nxn%d" % i) for i in range(2)]
        self.ss = [kb.sb([128, 1], F32, "nss%d" % i) for i in range(2)]
        self.rs = [kb.sb([128, 1], F32, "nrs%d" % i) for i in range(2)]
        self.pt = pts if pts is not None else [kb.ps([128, 512], BF16, "npt%d" % i) for i in range(2)]
        self.i = 0
        self.nbuf = nbuf

    def load(self, src_ap, q="sp"):
        kb = self.kb
        b = self.i % self.nbuf
        xt = self.xt[b]
        kb.dma(q, xt[:], src_ap, writes=[xt])
        return xt

    def run(self, src_ap, dst, dst_sl, q="sp", xt=None, gB=None):
        kb = self.kb
        if xt is None:
            xt = self.load(src_ap, q)
        k = self.i % 2
        self.i += 1
        ss, rs, xn = self.ss[k], self.rs[k], self.xn[k]
        kb.op("act", lambda e: e.activation(out=self.sq[:], in_=xt[:], func=AF.Square, accum_out=ss[:]),
              reads=[xt], writes=[self.sq, ss])
        kb.op("act", lambda e: e.activation(out=rs[:], in_=ss[:], func=AF.Sqrt, bias=EPS, scale=1.0 / D),
              reads=[ss], writes=[rs])
        kb.op("dve", lambda e: e.reciprocal(out=rs[:], in_=rs[:]), reads=[rs], writes=[rs])
        kb.op("dve", lambda e: e.tensor_scalar_mul(out=xn[:], in0=xt[:], scalar1=rs[:, 0:1]),
              reads=[xt, rs], writes=[xn])
        for g in range(4):
            pt = self.pt[g % 2]
            ptv = bf16_view(pt)
            for j in range(4):
                c = g * 4 + j
                kb.op("pe", lambda e: e.transpose(out=ptv[:, j * 128:(j + 1) * 128], in_=xn[:, c * 128:(c + 1) * 128],
                                                  identity=self.ident[:]),
                      reads=[xn, self.ident], writes=[pt.sub(j)])
            eng = "act" if g % 2 == 0 else "dve"
            o = dst[:, g * 4:(g + 1) * 4, dst_sl]
            i_ = ptv[:, 0:512].rearrange("p (a b) -> p a b", a=4)
            if gB is not None:
                kb.op("dve", lambda e: e.tensor_tensor(out=o, in0=i_, in1=gB[:, g * 4:(g + 1) * 4, :], op=ALU.mult),
                      reads=[pt, gB], writes=[dst.sub(("n", g, dst_sl.start))])
            elif eng == "act":
                kb.op("act", lambda e: e.copy(out=o, in_=i_), reads=[pt], writes=[dst.sub(("n", g, dst_sl.start))])
            else:
                kb.op("dve", lambda e: e.tensor_copy(out=o, in_=i_), reads=[pt], writes=[dst.sub(("n", g, dst_sl.start))])
        return xt


_eps_cache = {}


def EPS_AP(kb):
    if id(kb) not in _eps_cache:
        t = kb.sb([128, 1], F32, "epsc")
        kb.op("pool", lambda e: e.memset(t[:], EPS), writes=[t])
        _eps_cache[id(kb)] = t
    t = _eps_cache[id(kb)]
    return t[:, 0:1]


def load_weight_bf16(kb, w_ap, rows, cols, name, gain=None, stage=None, q="sp", col_off=0):
    rc = rows // 128
    wb = kb.sb([128, rc, cols], BF16, name)
    if stage is None:
        stage = [kb.sb([128, 512], F32, name + "_st%d" % i) for i in range(3)]
    n = 0
    for c in range(rc):
        for j0 in range(0, cols, 512):
            jw = min(512, cols - j0)
            st = stage[n % len(stage)]
            kb.dma(q if n % 2 == 0 else "pool", st[:, :jw], w_ap[c * 128:(c + 1) * 128, col_off + j0:col_off + j0 + jw], writes=[st])
            if gain is not None:
                kb.op("dve", lambda e: e.tensor_scalar_mul(out=wb[:, c, j0:j0 + jw], in0=st[:, :jw], scalar1=gain[:, c:c + 1]),
                      reads=[st, gain], writes=[wb.sub((c, j0))])
            else:
                if n % 2 == 0:
                    kb.op("dve", lambda e: e.tensor_copy(out=wb[:, c, j0:j0 + jw], in_=st[:, :jw]), reads=[st], writes=[wb.sub((c, j0))])
                else:
                    kb.op("act", lambda e: e.copy(out=wb[:, c, j0:j0 + jw], in_=st[:, :jw]), reads=[st], writes=[wb.sub((c, j0))])
            n += 1
    return wb, stage


def load_gain(kb, g_ap, n, name, q="sp"):
    t = kb.sb([128, n // 128], F32, name)
    kb.dma(q, t[:], g_ap.rearrange("(c p) -> p c", p=128), writes=[t], allow_slow_non_contiguous=True)
    return t


def build_p1(T):
    kb = KB()
    NT = T // 128
    x = kb.dram("x", [T + 128, D], F32, "ExternalInput")
    g = kb.dram("g", [D], F32, "ExternalInput")
    w = kb.dram("w", [D, 4096], F32, "ExternalInput")
    pw = kb.dram("pw", [4, 256, 256], F32, "ExternalInput")
    psc = kb.dram("psc", [1024], F32, "ExternalInput")
    fm = kb.dram("fm", [3, 4, 128, 128], F32, "ExternalInput")
    qkT = kb.dram("qkT", [2048, T], BF16, "ExternalOutput")
    v = kb.dram("v", [T, 1024], BF16, "ExternalOutput")
    obT = kb.dram("obT", [1024, T], BF16, "ExternalOutput")

    ident = emit_identity(kb)
    gain = load_gain(kb, g, D, "gain")
    nt = NormT(kb, ident)
    wb = kb.sb([128, NCH, 2048], BF16, "wb")
    stage = None

    def load_w(col_off):
        nonlocal stage
        n = 0
        if stage is None:
            stage = [kb.sb([128, 512], F32, "wst%d" % i) for i in range(3)]
        for c in range(NCH):
            for j0 in range(0, 2048, 512):
                st = stage[n % 3]
                kb.dma("sp" if n % 2 == 0 else "pool", st[:], w[c * 128:(c + 1) * 128, col_off + j0:col_off + j0 + 512], writes=[st])
                sc = 128 ** -0.5 if (col_off == 0 and j0 < 1024) else 1.0
                kb.op("dve", lambda e: e.tensor_scalar(out=wb[:, c, j0:j0 + 512], in0=st[:], scalar1=gain[:, c:c + 1], scalar2=float(sc),
                                                       op0=ALU.mult, op1=ALU.mult),
                      reads=[st, gain], writes=[wb.sub((c, j0))])
                n += 1

    load_w(0)
    hnT = [kb.sb([128, NCH, 512], BF16, "hnT%d" % i) for i in range(2)]
    pA = [kb.ps([128, 512], F32, "pA%d" % i) for i in range(2)]
    oA = [kb.sb([128, 512], BF16, "oA%d" % i) for i in range(3)]
    n_o = 0
    for gi in range(T // 512):
        h = hnT[gi % 2]
        for j in range(4):
            r0 = 128 + gi * 512 + j * 128
            nt.run(x[r0:r0 + 128, :], h, slice(j * 128, (j + 1) * 128))
        for cb in range(16):
            p = pA[cb % 2]
            for c in range(NCH):
                kb.op("pe", lambda e: e.matmul(p[:], lhsT=wb[:, c, cb * 128:(cb + 1) * 128], rhs=h[:, c, :],
                                               start=(c == 0), stop=(c == NCH - 1)),
                      reads=[wb, h], writes=[p])
            o = oA[n_o % 3]
            n_o += 1
            if cb % 2 == 0:
                kb.op("act", lambda e: e.copy(out=o[:], in_=p[:]), reads=[p], writes=[o])
            else:
                kb.op("dve", lambda e: e.tensor_copy(out=o[:], in_=p[:]), reads=[p], writes=[o])
            kb.dma("sp", qkT[cb * 128:(cb + 1) * 128, gi * 512:(gi + 1) * 512], o[:], reads=[o], is_output=True)

    load_w(2048)
    fmt = kb.sb([128, 12, 128], F32, "fmt")
    kb.dma("sp", fmt[:], fm.rearrange("a g s t -> s (a g) t"), writes=[fmt], allow_slow_non_contiguous=True)
    pwb, _ = load_weight_bf16(kb, pw.rearrange("g c d -> (g c) d"), 1024, 256, "pwb", stage=stage)
    pscale = load_gain(kb, psc, 1024, "pscale")
    hB = [kb.sb([128, NCH, 128], BF16, "hB%d" % i) for i in range(2)]
    pV = [kb.ps([128, 512], F32, "pV%d" % i) for i in range(2)]
    vo = [kb.sb([128, 1024], BF16, "vo%d" % i) for i in range(2)]
    ut = [kb.sb([128, 1024], F32, "ut%d" % i) for i in range(2)]
    pD = [kb.ps([128, 512], F32, "pD%d" % i) for i in range(2)]
    dT = [kb.sb([128, 8, 128], BF16, "dT%d" % i) for i in range(2)]
    ob = [kb.sb([128, 8, 512], BF16, "ob%d" % i) for i in range(2)]
    for ti in range(-1, NT):
        r0 = 128 + ti * 128
        k = (ti + 1) % 2
        h = hB[k]
        nt.run(x[r0:r0 + 128, :], h, slice(0, 128))
        u_cur, u_prev = ut[k], ut[1 - k]
        for half in range(4):
            if ti < 0 and half < 2:
                continue
            p = pV[half % 2]
            for c in range(NCH):
                kb.op("pe", lambda e: e.matmul(p[:], lhsT=h[:, c, :], rhs=wb[:, c, half * 512:(half + 1) * 512],
                                               start=(c == 0), stop=(c == NCH - 1)), reads=[h, wb], writes=[p])
            if half < 2:
                dst = vo[ti % 2]
                kb.op("act", lambda e: e.copy(out=dst[:, half * 512:(half + 1) * 512], in_=p[:]), reads=[p], writes=[dst.sub(half)])
            else:
                hh = half - 2
                kb.op("dve", lambda e: e.tensor_copy(out=u_cur[:, hh * 512:(hh + 1) * 512], in_=p[:]), reads=[p], writes=[u_cur.sub(hh)])
        if ti < 0:
            continue
        kb.dma("pool", v[ti * 128:(ti + 1) * 128, :], vo[ti % 2][:], reads=[vo[ti % 2]], is_output=True)
        d = dT[ti % 2]
        for half in range(2):
            p = pD[half]
            for j in range(4):
                cc = half * 4 + j
                gq = cc // 2
                fcur = fmt[:, (0 if ti == 0 else 4) + gq, :]
                fprev = fmt[:, 8 + gq, :]
                kb.op("pe", lambda e: e.matmul(p[:, j * 128:(j + 1) * 128], lhsT=u_cur[:, cc * 128:(cc + 1) * 128], rhs=fcur,
                                               start=True, stop=False), reads=[u_cur, fmt], writes=[p.sub(j)])
                kb.op("pe", lambda e: e.matmul(p[:, j * 128:(j + 1) * 128], lhsT=u_prev[:, cc * 128:(cc + 1) * 128], rhs=fprev,
                                               start=False, stop=True), reads=[u_prev, fmt], writes=[p.sub(j)])
            kb.op("act" if half == 0 else "dve",
                  (lambda e: e.copy(out=d[:, half * 4:(half + 1) * 4, :], in_=p[:].rearrange("p (a b) -> p a b", a=4))) if half == 0 else
                  (lambda e: e.tensor_copy(out=d[:, half * 4:(half + 1) * 4, :], in_=p[:].rearrange("p (a b) -> p a b", a=4))),
                  reads=[p], writes=[d.sub(half)])
        o = ob[(ti // 4) % 2]
        tj = ti % 4
        for half in range(2):
            p = pD[half]
            for j in range(4):
                oc = half * 4 + j
                gq, dh = oc // 2, oc % 2
                for cc in range(2):
                    kb.op("pe", lambda e: e.matmul(p[:, j * 128:(j + 1) * 128], lhsT=pwb[:, gq * 2 + cc, dh * 128:(dh + 1) * 128],
                                                   rhs=d[:, gq * 2 + cc, :], start=(cc == 0), stop=(cc == 1)),
                          reads=[pwb, d], writes=[p.sub(j)])
                kb.op("dve", lambda e: e.tensor_scalar_mul(out=o[:, oc, tj * 128:(tj + 1) * 128], in0=p[:, j * 128:(j + 1) * 128],
                                                           scalar1=pscale[:, oc:oc + 1]),
                      reads=[p.sub(j), pscale], writes=[o.sub((oc, tj))])
        if tj == 3:
            t0 = (ti // 4) * 512
            kb.dma("pool", obT[:, t0:t0 + 512].rearrange("(c p) t -> p c t", p=128), o[:], reads=[o], is_output=True)
    return kb.finish()


def build_p2(S, NU=2):
    kb = KB()
    NG = S // 512
    NB = S // 128
    qT = kb.dram("qT", [NU, 128, S], BF16, "ExternalInput")
    kT = kb.dram("kT", [NU, 128, S], BF16, "ExternalInput")
    v = kb.dram("v", [NU, S, 128], BF16, "ExternalInput")
    oT = kb.dram("oT", [NU, 128, S], BF16, "ExternalOutput")

    ntri = kb.sb([128, 128], F32, "ntri")
    kb.op("pool", lambda e: e.memset(ntri[:], -1.0), writes=[ntri])
    kb.op("pool", lambda e: e.affine_select(out=ntri[:], in_=ntri[:], pattern=[[-1, 128]], compare_op=ALU.is_ge,
                                             fill=0.0, base=0, channel_multiplier=1), reads=[ntri], writes=[ntri])
    nones = kb.sb([128, 128], F32, "nones")
    kb.op("pool", lambda e: e.memset(nones[:], -1.0), writes=[nones])

    qs = kb.sb([128, S], BF16, "qs")
    ks = kb.sb([128, S], BF16, "ks")
    vs = kb.sb([128, NB, 128], BF16, "vs")
    pz = [kb.ps([128, 512], F32, "pz%d" % i) for i in range(2)]
    px = [kb.ps([128, 512], F32, "px%d" % i) for i in range(2)]
    po = [kb.ps([128, 512], F32, "po%d" % i) for i in range(2)]
    et = [kb.sb([128, 512], F32, "et%d" % i) for i in range(2)]
    spt = [kb.sb([128, 512], F32, "spt%d" % i) for i in range(3)]
    wt = [kb.sb([128, 512], BF16, "wt%d" % i) for i in range(3)]
    srun = kb.sb([128, 512], F32, "srun")
    ot = [kb.sb([128, 512], BF16, "ot%d" % i) for i in range(2)]
    n = 0
    for u in range(NU):
        nq = 4 if S >= 2048 else 1
        for i in range(nq):
            sl = slice(i * S // nq, (i + 1) * S // nq)
            kb.dma("sp", qs[:, sl], qT[u, :, sl], writes=[qs.sub(i)] if u == 0 else [qs])
            kb.dma("pool", ks[:, sl], kT[u, :, sl], writes=[ks.sub(i)] if u == 0 else [ks])
        nv = max(1, NB // 16)
        for i in range(nv):
            j0, j1 = i * NB // nv, (i + 1) * NB // nv
            kb.dma("sp" if i % 2 == 0 else "pool", vs[:, j0:j1, :], v[u, j0 * 128:j1 * 128, :].rearrange("(j p) d -> p j d", p=128),
                   writes=[vs.sub(i)] if u == 0 else [vs])
        for G in range(NG):
            qg = qs[:, G * 512:(G + 1) * 512]
            oacc = po[G % 2]
            jlist = list(range(4 * G + 3, -1, -1))
            for idx, j in enumerate(jlist):
                diag = j >= 4 * G
                first = idx == 0
                last = idx == len(jlist) - 1
                z = pz[n % 2]
                xx = px[n % 2]
                e_t = et[n % 2]
                sp_t = spt[n % 3]
                w_t = wt[n % 3]
                n += 1
                kblk = ks[:, j * 128:(j + 1) * 128]
                kb.op("pe", lambda e: e.matmul(z[:], lhsT=kblk, rhs=qg, start=True, stop=True), reads=[qs, ks], writes=[z])
                kb.op("act", lambda e: e.activation(out=e_t[:], in_=z[:], func=AF.Exp), reads=[z], writes=[e_t])
                kb.op("act", lambda e: e.activation(out=sp_t[:], in_=e_t[:], func=AF.Ln, bias=1.0), reads=[e_t], writes=[sp_t])
                if diag:
                    kb.op("pool", lambda e: e.affine_select(out=sp_t[:], in_=sp_t[:], pattern=[[1, 512]], compare_op=ALU.is_gt,
                                                             fill=0.0, base=512 * G - 128 * j, channel_multiplier=-1),
                          reads=[sp_t], writes=[sp_t])
                kb.op("pe", lambda e: e.matmul(xx[:], lhsT=kblk, rhs=qg, start=True, stop=False), reads=[qs, ks], writes=[xx])
                kb.op("pe", lambda e: e.matmul(xx[:], lhsT=ntri[:], rhs=sp_t[:], start=False, stop=first), reads=[ntri, sp_t], writes=[xx])
                if not first:
                    kb.op("pe", lambda e: e.matmul(xx[:], lhsT=nones[:], rhs=srun[:], start=False, stop=True), reads=[nones, srun], writes=[xx])
                kb.op("act", lambda e: e.activation(out=w_t[:], in_=xx[:], func=AF.Exp), reads=[xx], writes=[w_t])
                if diag:
                    kb.op("pool", lambda e: e.affine_select(out=w_t[:], in_=w_t[:], pattern=[[1, 512]], compare_op=ALU.is_gt,
                                                             fill=0.0, base=512 * G - 128 * j, channel_multiplier=-1),
                          reads=[w_t], writes=[w_t])
                if not last:
                    if first:
                        kb.op("dve", lambda e: e.tensor_copy(out=srun[:], in_=sp_t[:]), reads=[sp_t], writes=[srun])
                    else:
                        kb.op("dve", lambda e: e.tensor_add(out=srun[:], in0=srun[:], in1=sp_t[:]), reads=[sp_t, srun], writes=[srun])
                kb.op("pe", lambda e: e.matmul(oacc[:], lhsT=vs[:, j, :], rhs=w_t[:], start=first, stop=last), reads=[vs, w_t], writes=[oacc])
            o_t = ot[G % 2]
            kb.op("dve", lambda e: e.tensor_copy(out=o_t[:], in_=oacc[:]), reads=[oacc], writes=[o_t])
            kb.dma("sp", oT[u, :, G * 512:(G + 1) * 512], o_t[:], reads=[o_t], is_output=True)
    return kb.finish()


def load_w_scaled(kb, w_ap, rows, cols, name, gain, scale, stage, dst=None, col_off=0):
    rc = rows // 128
    wb = dst if dst is not None else kb.sb([128, rc, cols], BF16, name)
    n = 0
    for c in range(rc):
        for j0 in range(0, cols, 512):
            jw = min(512, cols - j0)
            st = stage[n % len(stage)]
            kb.dma("sp" if n % 2 == 0 else "pool", st[:, :jw], w_ap[c * 128:(c + 1) * 128, col_off + j0:col_off + j0 + jw], writes=[st])
            kb.op("dve", lambda e: e.tensor_scalar(out=wb[:, c, j0:j0 + jw], in0=st[:, :jw], scalar1=gain[:, c:c + 1], scalar2=float(scale),
                                                   op0=ALU.mult, op1=ALU.mult), reads=[st, gain], writes=[wb.sub((c, j0))])
            n += 1
    return wb


def build_p3a(T):
    kb = KB()
    NT = T // 128
    oT = kb.dram("oT", [2048, T], BF16, "ExternalInput")
    hin = kb.dram("hin", [T, D], F32, "ExternalInput")
    wout = kb.dram("wout", [D, D], F32, "ExternalInput")
    gx = kb.dram("gx", [D], F32, "ExternalInput")
    mem = kb.dram("mem", [256, D], F32, "ExternalInput")
    gm = kb.dram("gm", [D], F32, "ExternalInput")
    wq = kb.dram("wq", [D, 512], F32, "ExternalInput")
    wkv = kb.dram("wkv", [D, 1024], F32, "ExternalInput")
    wo = kb.dram("wo", [512, D], F32, "ExternalInput")
    hout = kb.dram("hout", [T, D], F32, "ExternalOutput")

    ident = emit_identity(kb)
    ones_b = kb.sb([128, 128], BF16, "ones_b")
    kb.op("pool", lambda e: e.memset(ones_b[:], 1.0), writes=[ones_b])
    nt = NormT(kb, ident, nbuf=1)
    wout_b, stage = load_weight_bf16(kb, wout, D, D, "wout_b")
    gxg = load_gain(kb, gx, D, "gxg")
    gmg = load_gain(kb, gm, D, "gmg")
    wq_b = load_w_scaled(kb, wq, D, 512, "wq_b", gxg, 128 ** -0.5, stage)
    wo_b, _ = load_weight_bf16(kb, wo, 512, D, "wo_b", stage=stage)
    wkv_b = load_w_scaled(kb, wkv, D, 512, "wkv_b", gmg, 1.0, stage)

    pmm = [kb.ps([128, 512], F32, "pmm%d" % i) for i in range(2)]
    pq = kb.ps([128, 512], F32, "pq")
    pl = [kb.ps([128, 512], F32, "pl%d" % i) for i in range(2)]
    po = kb.ps([128, 512], F32, "po")

    memT = kb.sb([128, NCH, 256], BF16, "memT")
    for mt in range(2):
        nt.run(mem[mt * 128:(mt + 1) * 128, :], memT, slice(mt * 128, (mt + 1) * 128))
    KT = kb.sb([128, 4, 256], BF16, "KT")
    Vs = kb.sb([128, 2, 512], BF16, "Vs")
    for hd in range(4):
        p = pmm[hd % 2]
        for c in range(NCH):
            kb.op("pe", lambda e: e.matmul(p[:, 0:256], lhsT=wkv_b[:, c, hd * 128:(hd + 1) * 128], rhs=memT[:, c, :],
                                           start=(c == 0), stop=(c == NCH - 1)), reads=[wkv_b, memT], writes=[p])
        kb.op("act", lambda e: e.copy(out=KT[:, hd, :], in_=p[:, 0:256]), reads=[p], writes=[KT.sub(hd)])
    load_w_scaled(kb, wkv, D, 512, "wkv_b", gmg, 1.0, stage, dst=wkv_b, col_off=512)
    for mc in range(2):
        p = pmm[mc % 2]
        for c in range(NCH):
            kb.op("pe", lambda e: e.matmul(p[:], lhsT=memT[:, c, mc * 128:(mc + 1) * 128], rhs=wkv_b[:, c, 0:512],
                                           start=(c == 0), stop=(c == NCH - 1)), reads=[wkv_b, memT], writes=[p])
        kb.op("act", lambda e: e.copy(out=Vs[:, mc, :], in_=p[:]), reads=[p], writes=[Vs.sub(mc)])

    oTs = [kb.sb([128, NCH, 512], BF16, "oTs%d" % i) for i in range(1)]
    xin = [kb.sb([128, D], F32, "xin%d" % i) for i in range(1)]
    ht = [kb.sb([128, D], F32, "ht%d" % i) for i in range(2)]
    hnT = [kb.sb([128, NCH, 128], BF16, "hnT%d" % i) for i in range(1)]
    qxT = kb.sb([128, 4, 128], BF16, "qxT")
    pT = kb.sb([128, 8, 128], BF16, "pT")
    rden = kb.sb([128, 512], F32, "rden")
    oxT = kb.sb([128, 4, 128], BF16, "oxT")
    for ti in range(NT):
        gi, tj = ti // 4, ti % 4
        og = oTs[0]
        if tj == 0:
            kb.dma("pool", og[:], oT[:, gi * 512:(gi + 1) * 512].rearrange("(c p) t -> p c t", p=128), writes=[og])
        xt = xin[0]
        kb.dma("sp", xt[:], hin[ti * 128:(ti + 1) * 128, :], writes=[xt])
        h = ht[ti % 2]
        for cg in range(4):
            p = pmm[cg % 2]
            for c in range(NCH):
                kb.op("pe", lambda e: e.matmul(p[:], lhsT=og[:, c, tj * 128:(tj + 1) * 128], rhs=wout_b[:, c, cg * 512:(cg + 1) * 512],
                                               start=(c == 0), stop=(c == NCH - 1)), reads=[og, wout_b], writes=[p])
            kb.op("dve", lambda e: e.tensor_tensor(out=h[:, cg * 512:(cg + 1) * 512], in0=p[:], in1=xt[:, cg * 512:(cg + 1) * 512], op=ALU.add),
                  reads=[p, xt], writes=[h.sub(cg)])
        hn = hnT[0]
        nt.run(None, hn, slice(0, 128), xt=h)
        for hd in range(4):
            for c in range(NCH):
                kb.op("pe", lambda e: e.matmul(pq[:, hd * 128:(hd + 1) * 128], lhsT=wq_b[:, c, hd * 128:(hd + 1) * 128], rhs=hn[:, c, :],
                                               start=(c == 0), stop=(c == NCH - 1)), reads=[wq_b, hn], writes=[pq])
        kb.op("act", lambda e: e.copy(out=qxT[:], in_=pq[:].rearrange("p (a b) -> p a b", a=4)), reads=[pq], writes=[qxT])
        for hd in range(4):
            for mc in range(2):
                b = hd * 2 + mc
                kb.op("pe", lambda e: e.matmul(pl[b // 4][:, (b % 4) * 128:(b % 4 + 1) * 128], lhsT=KT[:, hd, mc * 128:(mc + 1) * 128],
                                               rhs=qxT[:, hd, :], start=True, stop=True), reads=[KT, qxT], writes=[pl[b // 4]])
        for k2 in range(2):
            kb.op("act", lambda e: e.activation(out=pT[:, k2 * 4:(k2 + 1) * 4, :], in_=pl[k2][:].rearrange("p (a b) -> p a b", a=4), func=AF.Exp),
                  reads=[pl[k2]], writes=[pT.sub(k2)])
        for hd in range(4):
            for mc in range(2):
                kb.op("pe", lambda e: e.matmul(po[:, hd * 128:(hd + 1) * 128], lhsT=Vs[:, mc, hd * 128:(hd + 1) * 128], rhs=pT[:, hd * 2 + mc, :],
                                               start=(mc == 0), stop=(mc == 1)), reads=[Vs, pT], writes=[po])
        for hd in range(4):
            for mc in range(2):
                kb.op("pe", lambda e: e.matmul(pq[:, hd * 128:(hd + 1) * 128], lhsT=ones_b[:], rhs=pT[:, hd * 2 + mc, :],
                                               start=(mc == 0), stop=(mc == 1)), reads=[ones_b, pT], writes=[pq])
        kb.op("dve", lambda e: e.reciprocal(out=rden[:], in_=pq[:]), reads=[pq], writes=[rden])
        kb.op("dve", lambda e: e.tensor_tensor(out=oxT[:], in0=po[:].rearrange("p (a b) -> p a b", a=4),
                                               in1=rden[:].rearrange("p (a b) -> p a b", a=4), op=ALU.mult), reads=[po, rden], writes=[oxT])
        for cg in range(4):
            p = pmm[cg % 2]
            for hd in range(4):
                kb.op("pe", lambda e: e.matmul(p[:], lhsT=oxT[:, hd, :], rhs=wo_b[:, hd, cg * 512:(cg + 1) * 512],
                                               start=(hd == 0), stop=(hd == 3)), reads=[oxT, wo_b], writes=[p])
            kb.op("dve", lambda e: e.tensor_tensor(out=h[:, cg * 512:(cg + 1) * 512], in0=p[:], in1=h[:, cg * 512:(cg + 1) * 512], op=ALU.add),
                  reads=[p, h.sub(cg)], writes=[h.sub(cg)])
        kb.dma("pool", hout[ti * 128:(ti + 1) * 128, :], h[:], reads=[h], is_output=True)
    return kb.finish()


def build_cast(N, CH=4096):
    kb = KB()
    a = kb.dram("a", [128, N], F32, "ExternalInput")
    o = kb.dram("o", [128, N], BF16, "ExternalOutput")
    st = [kb.sb([128, CH], F32, "cs%d" % i) for i in range(3)]
    ob = [kb.sb([128, CH], BF16, "co%d" % i) for i in range(3)]
    for i in range(N // CH):
        s_, o_ = st[i % 3], ob[i % 3]
        kb.dma("sp", s_[:], a[:, i * CH:(i + 1) * CH], writes=[s_])
        if i % 2 == 0:
            kb.op("dve", lambda e: e.tensor_copy(out=o_[:], in_=s_[:]), reads=[s_], writes=[o_])
        else:
            kb.op("act", lambda e: e.copy(out=o_[:], in_=s_[:]), reads=[s_], writes=[o_])
        kb.dma("pool", o[:, i * CH:(i + 1) * CH], o_[:], reads=[o_], is_output=True)
    return kb.finish()


NEG = -1.0e30


def build_p3b(T, final=False, NE=16384):
    kb = KB()
    NT = T // 128
    NEG_ = NE // 512
    hin = kb.dram("hin", [T, D], F32, "ExternalInput")
    gf = kb.dram("gf", [D], F32, "ExternalInput")
    wqb = kb.dram("wqb", [4, 128, NCH, 512], BF16, "ExternalInput")
    skT = kb.dram("skT", [128, 16, 128], F32, "ExternalInput")
    uT = kb.dram("uT", [NE // 512, 128, NCH, 512], BF16, "ExternalInput")
    vv = kb.dram("vv", [NE // 512, 128, 4, D], BF16, "ExternalInput")
    gfin = kb.dram("gfin", [D], F32, "ExternalInput")
    hout = kb.dram("hout", [T, D], F32, "ExternalOutput")

    ident = emit_identity(kb)
    B = [kb.ps([128, 512], F32, "B%d" % i) for i in range(8)]
    nt = NormT(kb, ident, nbuf=1, pts=[B[4], B[5]])
    gfg = load_gain(kb, gf, D, "gfg")
    gB = kb.sb([128, NCH, 128], F32, "gB")
    for c in range(NCH):
        kb.op("dve", lambda e: e.tensor_copy(out=gB[:, c, :], in_=gfg[:, c:c + 1].to_broadcast([128, 128])), reads=[gfg], writes=[gB.sub(c)])
    skst = kb.sb([128, 16, 128], F32, "skst")
    kb.dma("sp", skst[:], skT, writes=[skst])
    skb = kb.sb([128, 16, 128], BF16, "skb")
    kb.op("dve", lambda e: e.tensor_copy(out=skb[:], in_=skst[:]), reads=[skst], writes=[skb])
    if final:
        gfb = kb.sb([128, D], F32, "gfb")
        kb.dma("sp", gfb[:], gfin.partition_broadcast(128), writes=[gfb])

    G = kb.sb([128, NE], BF16, "G")
    sub = skst
    sub2 = kb.sb([128, 16, 128], F32, "sub2")
    m8 = kb.sb([128, 16, 16], F32, "m8")
    cand = kb.sb([128, 256], F32, "cand")
    cand2 = kb.sb([128, 256], F32, "cand2")
    tv = kb.sb([128, 8, 16], F32, "tv")
    e16 = kb.sb([128, 16], F32, "e16")
    negm = kb.sb([128, 8], F32, "negm")
    Z = kb.sb([128, 8], F32, "Z")
    nb = kb.sb([128, 8], F32, "nb")
    St = [kb.sb([128, 16, 128], F32, "St%d" % i) for i in range(2)]
    Et = kb.sb([128, 2048], BF16, "Et")
    Mt = kb.sb([128, 2048], BF16, "Mt")
    hnT = kb.sb([128, NCH, 128], BF16, "hnT")
    qTs = kb.sb([128, 16, 128], BF16, "qTs")
    ug = [kb.sb([128, NCH, 512], BF16, "ug%d" % i) for i in range(2)]
    vg = [kb.sb([128, 4, D], BF16, "vg%d" % i) for i in range(2)]
    at = [kb.sb([128, 512], BF16, "at%d" % i) for i in range(2)]
    gat = [kb.sb([128, 512], BF16, "gat%d" % i) for i in range(2)]
    gaT = [kb.sb([128, 4, 128], BF16, "gaT%d" % i) for i in range(2)]
    ss = kb.sb([128, 1], F32, "fss")
    rs = kb.sb([128, 1], F32, "frs")
    nug = 0
    for ti in range(NT):
        xt = nt.run(hin[ti * 128:(ti + 1) * 128, :], hnT, slice(0, 128), gB=gB)
        for grp in range(4):
            w_ = ug[nug % 2]
            nug += 1
            kb.dma("sp", w_[:], wqb[grp], writes=[w_])
            pq = B[6 + grp % 2]
            for j in range(4):
                for c in range(NCH):
                    kb.op("pe", lambda e: e.matmul(pq[:, j * 128:(j + 1) * 128], lhsT=w_[:, c, j * 128:(j + 1) * 128], rhs=hnT[:, c, :],
                                                   start=(c == 0), stop=(c == NCH - 1)), reads=[w_, hnT], writes=[pq])
            kb.op("act", lambda e: e.copy(out=qTs[:, grp * 4:(grp + 1) * 4, :], in_=pq[:].rearrange("p (a b) -> p a b", a=4)),
                  reads=[pq], writes=[qTs.sub(grp)])
        for b4 in range(4):
            ps_ = B[b4]
            for j in range(4):
                hp = b4 * 4 + j
                kb.op("pe", lambda e: e.matmul(ps_[:, j * 128:(j + 1) * 128], lhsT=qTs[:, hp, :], rhs=skb[:, hp, :], start=True, stop=True),
                      reads=[qTs, skb], writes=[ps_])
            if b4 % 2 == 0:
                kb.op("act", lambda e: e.copy(out=sub[:, b4 * 4:(b4 + 1) * 4, :], in_=ps_[:].rearrange("p (a b) -> p a b", a=4)), reads=[ps_], writes=[sub.sub(b4)])
            else:
                kb.op("dve", lambda e: e.tensor_copy(out=sub[:, b4 * 4:(b4 + 1) * 4, :], in_=ps_[:].rearrange("p (a b) -> p a b", a=4)), reads=[ps_], writes=[sub.sub(b4)])
        for hp in range(16):
            kb.op("dve", lambda e: e.max(out=m8[:, hp, 0:8], in_=sub[:, hp, :]), reads=[sub], writes=[m8.sub(hp)])
            kb.op("dve", lambda e: e.match_replace(out=sub2[:, hp, :], in_to_replace=m8[:, hp, 0:8], in_values=sub[:, hp, :], imm_value=NEG),
                  reads=[sub, m8.sub(hp)], writes=[sub2.sub(hp)])
            kb.op("dve", lambda e: e.max(out=m8[:, hp, 8:16], in_=sub2[:, hp, :]), reads=[sub2.sub(hp)], writes=[m8.sub(hp)])
        for h in range(8):
            kb.op("dve", lambda e: e.tensor_tensor(out=cand[:].rearrange("p (a b) -> p a b", a=16),
                                                   in0=m8[:, 2 * h, :].unsqueeze(2).to_broadcast([128, 16, 16]),
                                                   in1=m8[:, 2 * h + 1, :].unsqueeze(1).to_broadcast([128, 16, 16]), op=ALU.add),
                  reads=[m8], writes=[cand])
            kb.op("dve", lambda e: e.max(out=tv[:, h, 0:8], in_=cand[:]), reads=[cand], writes=[tv.sub(h)])
            kb.op("dve", lambda e: e.match_replace(out=cand2[:], in_to_replace=tv[:, h, 0:8], in_values=cand[:], imm_value=NEG),
                  reads=[cand, tv.sub(h)], writes=[cand2])
            kb.op("dve", lambda e: e.max(out=tv[:, h, 8:16], in_=cand2[:]), reads=[cand2], writes=[tv.sub(h)])
        kb.op("dve", lambda e: e.tensor_scalar_mul(out=negm[:], in0=tv[:, :, 0], scalar1=-1.0), reads=[tv], writes=[negm])
        for h in range(8):
            kb.op("act", lambda e: e.activation(out=e16[:], in_=tv[:, h, :], func=AF.Exp, bias=negm[:, h:h + 1], accum_out=Z[:, h:h + 1]),
                  reads=[tv, negm], writes=[e16, Z.sub(h)])
        kb.op("act", lambda e: e.activation(out=Z[:], in_=Z[:], func=AF.Ln), reads=[Z], writes=[Z])
        kb.op("dve", lambda e: e.tensor_sub(out=nb[:], in0=negm[:], in1=Z[:]), reads=[negm, Z], writes=[nb])
        npc = 0
        for h in range(8):
            for pc in range(8):
                S_ = St[npc % 2]
                npc += 1
                kb.op("pool", lambda e: e.tensor_tensor(out=S_[:], in0=sub[:, 2 * h, pc * 16:(pc + 1) * 16].unsqueeze(2).to_broadcast([128, 16, 128]),
                                                        in1=sub[:, 2 * h + 1, :].unsqueeze(1).to_broadcast([128, 16, 128]), op=ALU.add),
                      reads=[sub], writes=[S_])
                Sf = S_[:].rearrange("p a b -> p (a b)")
                kb.op("act", lambda e: e.activation(out=Et[:], in_=Sf, func=AF.Exp, bias=nb[:, h:h + 1]), reads=[S_, nb], writes=[Et])
                Gp = G[:, pc * 2048:(pc + 1) * 2048]
                if h == 0:
                    kb.op("dve", lambda e: e.scalar_tensor_tensor(out=Gp, in0=Sf, scalar=tv[:, h, 15:16], in1=Et[:], op0=ALU.is_ge, op1=ALU.mult),
                          reads=[S_, tv, Et], writes=[G.sub(pc)])
                else:
                    kb.op("dve", lambda e: e.scalar_tensor_tensor(out=Mt[:], in0=Sf, scalar=tv[:, h, 15:16], in1=Et[:], op0=ALU.is_ge, op1=ALU.mult),
                          reads=[S_, tv, Et], writes=[Mt])
                    kb.op("pool", lambda e: e.tensor_tensor(out=Gp, in0=Gp, in1=Mt[:], op=ALU.add), reads=[Mt, G.sub(pc)], writes=[G.sub(pc)])
        for eg in range(NEG_):
            u_ = ug[nug % 2]
            nug += 1
            v_ = vg[eg % 2]
            kb.dma("sp", u_[:], uT[eg], writes=[u_])
            kb.dma("pool", v_[:], vv[eg], writes=[v_])
            pa = B[4 + eg % 2]
            for c in range(NCH):
                kb.op("pe", lambda e: e.matmul(pa[:], lhsT=hnT[:, c, :], rhs=u_[:, c, :], start=(c == 0), stop=(c == NCH - 1)),
                      reads=[hnT, u_], writes=[pa])
            a_ = at[eg % 2]
            g_ = gat[eg % 2]
            kb.op("act", lambda e: e.activation(out=a_[:], in_=pa[:], func=AF.Gelu), reads=[pa], writes=[a_])
            kb.op("dve", lambda e: e.tensor_tensor(out=g_[:], in0=a_[:], in1=G[:, eg * 512:(eg + 1) * 512], op=ALU.mult),
                  reads=[a_, G], writes=[g_])
            ptb = B[6 + eg % 2]
            ptv = bf16_view(ptb)
            for j in range(4):
                kb.op("pe", lambda e: e.transpose(out=ptv[:, j * 128:(j + 1) * 128], in_=g_[:, j * 128:(j + 1) * 128], identity=ident[:]),
                      reads=[g_, ident], writes=[ptb])
            gT = gaT[eg % 2]
            kb.op("act", lambda e: e.copy(out=gT[:], in_=ptv[:, 0:512].rearrange("p (a b) -> p a b", a=4)), reads=[ptb], writes=[gT])
            for cg in range(4):
                for j in range(4):
                    kb.op("pe", lambda e: e.matmul(B[cg][:], lhsT=gT[:, j, :], rhs=v_[:, j, cg * 512:(cg + 1) * 512],
                                                   start=(eg == 0 and j == 0), stop=(eg == NEG_ - 1 and j == 3)), reads=[gT, v_], writes=[B[cg]])
        for cg in range(4):
            kb.op("dve", lambda e: e.tensor_tensor(out=xt[:, cg * 512:(cg + 1) * 512], in0=B[cg][:], in1=xt[:, cg * 512:(cg + 1) * 512], op=ALU.add),
                  reads=[B[cg], xt], writes=[xt])
        if final:
            kb.op("act", lambda e: e.activation(out=nt.sq[:], in_=xt[:], func=AF.Square, accum_out=ss[:]), reads=[xt], writes=[nt.sq, ss])
            kb.op("act", lambda e: e.activation(out=rs[:], in_=ss[:], func=AF.Sqrt, bias=EPS, scale=1.0 / D), reads=[ss], writes=[rs])
            kb.op("dve", lambda e: e.reciprocal(out=rs[:], in_=rs[:]), reads=[rs], writes=[rs])
            kb.op("dve", lambda e: e.scalar_tensor_tensor(out=xt[:], in0=xt[:], scalar=rs[:, 0:1], in1=gfb[:], op0=ALU.mult, op1=ALU.mult),
                  reads=[xt, rs, gfb], writes=[xt])
        kb.dma("pool", hout[ti * 128:(ti + 1) * 128, :], xt[:], reads=[xt], is_output=True)
    return kb.finish()


import ml_dtypes
_BF = ml_dtypes.bfloat16
_progs = {}


def _prog(key, fn, *a, **k):
    if key not in _progs:
        _progs[key] = fn(*a, **k)
    return _progs[key]


def _pool_mats(first):
    fm = np.zeros((3, 4, 128, 128), np.float32)
    s = np.arange(128)[:, None]
    t = np.arange(128)[None, :]
    for gi, win in enumerate((2, 4, 8, 16)):
        inwin = (s > t - win) & (s <= t)
        cnt_first = np.minimum(win, t + 1).astype(np.float32)
        cur = inwin / np.float32(win) - (s == t)
        cur0 = inwin / cnt_first - (s == t)
        prev = ((s - 128) > (t - win)) / np.float32(win)
        fm[0, gi] = cur0 if first else cur
        fm[1, gi] = cur
        fm[2, gi] = prev
    return fm


def _layer_tail(h_chunks, oT_chunks, layer, P, S, final):
    T = S // 4
    j = layer
    p3a = _prog(("p3a", T), build_p3a, T)
    ims = []
    for c in range(8):
        b = c // 4
        ims.append(dict(oT=oT_chunks[c], hin=h_chunks[c], wout=P["w_out"], gx=P["norm_cross"][j], mem=P["mem"][b], gm=P["norm_mem"][j],
                        wq=P["xattn_wq"][j], wkv=P["xattn_wkv"][j], wo=P["xattn_wo"][j]))
    r = run(p3a, ims)
    h1 = [np.asarray(r[c]["hout"]) for c in range(8)]
    flat = np.concatenate([np.ascontiguousarray(P["peer_u"][j].T).ravel(), P["peer_v"][j].ravel(), P["peer_wq"][j].ravel()])
    NPC = flat.size // (8 * 128)
    pc = _prog(("cast", NPC), build_cast, NPC)
    fl = flat.reshape(8, 128, NPC)
    rc = run(pc, [dict(a=fl[c]) for c in range(8)])
    fb = np.concatenate([np.asarray(rc[c]["o"]).reshape(-1) for c in range(8)])
    n_u = 16384 * D
    uTb = np.ascontiguousarray(fb[:n_u].reshape(NCH, 128, 32, 512).transpose(2, 1, 0, 3))
    vb = np.ascontiguousarray(fb[n_u:2 * n_u].reshape(32, 4, 128, D).transpose(0, 2, 1, 3))
    wqb = np.ascontiguousarray(fb[2 * n_u:].reshape(NCH, 128, 4, 512).transpose(2, 1, 0, 3))
    skT = np.ascontiguousarray(P["peer_subkeys"][j].reshape(16, 128, 128).transpose(2, 0, 1))
    p3b = _prog(("p3b", T, final), build_p3b, T, final=final)
    ims = [dict(hin=h1[c], gf=P["norm_ffn"][j], wqb=wqb, skT=skT, uT=uTb, vv=vb, gfin=P["norm_final"]) for c in range(8)]
    r = run(p3b, ims)
    return [np.asarray(r[c]["hout"]) for c in range(8)]


def _layer0(xc, P, S):
    T = S // 4
    p1 = _prog(("p1", T), build_p1, T)
    ims = []
    for c in range(8):
        b, ch = c // 4, c % 4
        halo = np.zeros((128, D), np.float32) if ch == 0 else xc[c - 1][-128:]
        ims.append(dict(x=np.concatenate([halo, xc[c]], 0), g=P["norm_mix"][0], w=P["w_in_ab"][0], pw=P["pool_w"][0],
                        psc=P["pool_scale"][0], fm=_pool_mats(ch == 0)))
    r = run(p1, ims)
    qkT = [np.asarray(r[c]["qkT"]) for c in range(8)]
    vtm = [np.asarray(r[c]["v"]) for c in range(8)]
    obT = [np.asarray(r[c]["obT"]) for c in range(8)]
    p2 = _prog(("p2", S), build_p2, S, 2)
    ims = []
    for c in range(8):
        qs, ks, vs = [], [], []
        for u in range(2):
            b, h = divmod(2 * c + u, 8)
            qs.append(np.concatenate([qkT[b * 4 + ch][h * 128:(h + 1) * 128] for ch in range(4)], 1))
            ks.append(np.concatenate([qkT[b * 4 + ch][1024 + h * 128:1024 + (h + 1) * 128] for ch in range(4)], 1))
            vs.append(np.concatenate([vtm[b * 4 + ch][:, h * 128:(h + 1) * 128] for ch in range(4)], 0))
        ims.append(dict(qT=np.stack(qs), kT=np.stack(ks), v=np.stack(vs)))
    r = run(p2, ims)
    oa = {}
    for c in range(8):
        o = np.asarray(r[c]["oT"])
        for u in range(2):
            oa[divmod(2 * c + u, 8)] = o[u]
    oT_chunks = []
    for c in range(8):
        b, ch = c // 4, c % 4
        oaT = np.concatenate([oa[(b, h)][:, ch * T:(ch + 1) * T] for h in range(8)], 0)
        oT_chunks.append(np.concatenate([oaT, obT[c]], 0))
    P0 = dict(P)
    P0["w_out"] = P["w_out_ab"][0]
    return _layer_tail(xc, oT_chunks, 0, P0, S, final=False)


def _layer1(hc, P, S):
    T = S // 4
    NR = S // 512
    p4 = _prog(("p4", T), build_p4, T)
    ims = []
    for c in range(8):
        ch = c % 4
        halo = np.zeros((128, D), np.float32) if ch == 0 else hc[c - 1][-128:]
        ims.append(dict(x=np.concatenate([halo, hc[c]], 0), g=P["norm_mix"][1], w=P["w_in_cd"][0], wuq=P["w_uq"][0], wiq=P["w_iq"][0],
                        ncq=P["norm_cq"][0], nki=P["norm_kidx"][0], cw=P["conv_w"][0], alog=P["a_log"][0], dtb=P["dt_bias"][0]))
    r = run(p4, ims)
    cat = lambda key, b, axis: np.concatenate([np.asarray(r[b * 4 + ch][key]) for ch in range(4)], axis)
    p5 = _prog(("p5", S), build_p5, S)
    ims = []
    per_b = {}
    for b in range(2):
        per_b[b] = dict(kiT=cat("kiT", b, 1), kcT=cat("kcT", b, 1).reshape(8, 128, S), vc=cat("vc", b, 0),
                        qiT=cat("qiT", b, 1), wi=cat("wi", b, 0), qcT=cat("qcT", b, 1))
    for c in range(8):
        b, rr = c // 4, c % 4
        pb = per_b[b]
        tiles = [4 * m + rr for m in range(NR)]
        cm, cm01, BT, b31 = _dsa_consts(rr, P["rel_bias"])
        qiT = np.stack([pb["qiT"][:, i * 128:(i + 1) * 128].reshape(16, 64, 128).transpose(1, 0, 2) for i in tiles])
        wi = np.stack([pb["wi"][i * 128:(i + 1) * 128] for i in tiles])
        qc = np.stack([pb["qcT"][:, i * 128:(i + 1) * 128].reshape(8, 128, 128) for i in tiles])
        ims.append(dict(kiT=pb["kiT"], qiT=np.ascontiguousarray(qiT), wi=np.ascontiguousarray(wi), cm=cm, cm01=cm01, kcT=pb["kcT"], vc=pb["vc"],
                        qc=np.ascontiguousarray(qc), BT=BT, b31=b31))
    r5 = run(p5, ims)
    ocT = [np.zeros((1024, S), _BF) for _ in range(2)]
    for c in range(8):
        b, rr = c // 4, c % 4
        o = np.asarray(r5[c]["ocT"])
        for m in range(NR):
            i = 4 * m + rr
            ocT[b][:, i * 128:(i + 1) * 128] = o[m].reshape(1024, 128)
    p6 = _prog(("p6", S), build_p6, S, 2)
    qkv = {b: cat("qkvT", b, 1) for b in range(2)}
    gb = {b: cat("gb", b, 1) for b in range(2)}
    sz = {b: cat("szT", b, 1) for b in range(2)}
    ims = []
    for c in range(8):
        d = {k_: [] for k_ in ("qT", "kT", "vT", "g", "beta", "szT")}
        for u in range(2):
            b, h = divmod(2 * c + u, 8)
            d["qT"].append(qkv[b][h * 128:(h + 1) * 128])
            d["kT"].append(qkv[b][1024 + h * 128:1024 + (h + 1) * 128])
            d["vT"].append(qkv[b][2048 + h * 128:2048 + (h + 1) * 128])
            d["g"].append(gb[b][8 + h])
            d["beta"].append(gb[b][h])
            d["szT"].append(sz[b][h * 128:(h + 1) * 128])
        im = {k_: np.ascontiguousarray(np.stack(v_)) for k_, v_ in d.items()}
        im["gno"] = P["norm_delta_out"][0]
        ims.append(im)
    r6 = run(p6, ims)
    odT = [np.zeros((1024, S), _BF) for _ in range(2)]
    for c in range(8):
        o = np.asarray(r6[c]["odT"])
        for u in range(2):
            b, h = divmod(2 * c + u, 8)
            odT[b][h * 128:(h + 1) * 128] = o[u]
    oT_chunks = []
    for c in range(8):
        b, ch = c // 4, c % 4
        oT_chunks.append(np.ascontiguousarray(np.concatenate([ocT[b][:, ch * T:(ch + 1) * T], odT[b][:, ch * T:(ch + 1) * T]], 0)))
    P1 = dict(P)
    P1["w_out"] = P["w_out_cd"][0]
    return _layer_tail(hc, oT_chunks, 1, P1, S, final=True)


def kernel(**inp):
    P = {k: np.asarray(v) for k, v in inp.items()}
    x = P["x"]
    S = x.shape[1]
    T = S // 4
    xc = [np.ascontiguousarray(x[c // 4, (c % 4) * T:(c % 4 + 1) * T]) for c in range(8)]
    h = _layer0(xc, P, S)
    h = _layer1(h, P, S)
    out = np.stack([np.concatenate(h[b * 4:(b + 1) * 4], 0) for b in range(2)])
    return out.astype(np.float32)


C_CQ, C_KC, C_VC, C_KI, C_WI, C_QKV, C_BETA, C_A, C_Z = 0, 256, 1280, 2304, 2368, 2384, 5456, 5464, 5472
N_CD = 6496


def build_p4(T):
    kb = KB()
    NGp = T // 512
    x = kb.dram("x", [T + 128, D], F32, "ExternalInput")
    g = kb.dram("g", [D], F32, "ExternalInput")
    w = kb.dram("w", [D, N_CD], F32, "ExternalInput")
    wuq = kb.dram("wuq", [256, 1024], F32, "ExternalInput")
    wiq = kb.dram("wiq", [256, 1024], F32, "ExternalInput")
    ncq = kb.dram("ncq", [256], F32, "ExternalInput")
    nki = kb.dram("nki", [64], F32, "ExternalInput")
    cw = kb.dram("cw", [4, 3072], F32, "ExternalInput")
    alog = kb.dram("alog", [8], F32, "ExternalInput")
    dtb = kb.dram("dtb", [8], F32, "ExternalInput")
    o_qcT = kb.dram("qcT", [1024, T], BF16, "ExternalOutput")
    o_kcT = kb.dram("kcT", [1024, T], BF16, "ExternalOutput")
    o_vc = kb.dram("vc", [T, 1024], BF16, "ExternalOutput")
    o_qiT = kb.dram("qiT", [1024, T], BF16, "ExternalOutput")
    o_kiT = kb.dram("kiT", [64, T], BF16, "ExternalOutput")
    o_wi = kb.dram("wi", [T, 32], F32, "ExternalOutput")
    o_qkvT = kb.dram("qkvT", [3072, T], F32, "ExternalOutput")
    o_gb = kb.dram("gb", [16, T], F32, "ExternalOutput")
    o_szT = kb.dram("szT", [1024, T], BF16, "ExternalOutput")

    ident = emit_identity(kb)
    gain = load_gain(kb, g, D, "gain")
    nt = NormT(kb, ident, nbuf=1)
    wb = kb.sb([128, NCH, 3072], BF16, "wb")
    stage = [kb.sb([128, 512], F32, "wst%d" % i) for i in range(3)]
    ones_f = kb.sb([128, 128], F32, "ones_f")
    kb.op("pool", lambda e: e.memset(ones_f[:], 1.0), writes=[ones_f])

    def load_w(col_off, ncols):
        n = 0
        for c in range(NCH):
            for j0 in range(0, ncols, 512):
                jw = min(512, ncols - j0)
                st = stage[n % 3]
                kb.dma("sp" if n % 2 == 0 else "pool", st[:, :jw], w[c * 128:(c + 1) * 128, col_off + j0:col_off + j0 + jw], writes=[st])
                kb.op("dve", lambda e: e.tensor_scalar_mul(out=wb[:, c, j0:j0 + jw], in0=st[:, :jw], scalar1=gain[:, c:c + 1]),
                      reads=[st, gain], writes=[wb.sub((c, j0))])
                n += 1

    hnT = [kb.sb([128, NCH, 512], BF16, "hnT%d" % i) for i in range(2)]
    pm = [kb.ps([128, 512], F32, "pm%d" % i) for i in range(3)]
    pw_ = kb.ps([128, 512], F32, "pw_")
    npm = [0]

    def norm_group(gi, ntile=4):
        h = hnT[gi % 2]
        for j in range(ntile):
            r0 = 128 + gi * 512 + j * 128
            nt.run(x[r0:r0 + 128, :], h, slice(j * 128, (j + 1) * 128))
        return h

    def fm_mm(h, col0, ncols, ntok=512):
        p = pm[npm[0] % 3]
        npm[0] += 1
        for c in range(NCH):
            kb.op("pe", lambda e: e.matmul(p[0:ncols, 0:ntok], lhsT=wb[:, c, col0:col0 + ncols], rhs=h[:, c, 0:ntok],
                                           start=(c == 0), stop=(c == NCH - 1)), reads=[wb, h], writes=[p])
        return p

    load_w(0, 2384)
    gcq = load_gain(kb, ncq, 256, "gcq")
    gki = kb.sb([64, 1], F32, "gki")
    kb.dma("sp", gki[:], nki.rearrange("(p o) -> p o", o=1), writes=[gki], allow_slow_non_contiguous=True)
    wuq_b = load_w_scaled(kb, wuq, 256, 1024, "wuq_b", gcq, 128 ** -0.5, stage)
    wiq_b = load_w_scaled(kb, wiq, 256, 1024, "wiq_b", gcq, 1.0, stage)
    cq = kb.sb([128, 2, 512], F32, "cq")
    cqs = kb.sb([128, 2, 512], F32, "cqs")
    rsd = kb.sb([128, 512], F32, "rsd")
    cqn = kb.sb([128, 2, 512], BF16, "cqn")
    ob16 = [kb.sb([128, 512], BF16, "ob16_%d" % i) for i in range(3)]
    vo = [kb.sb([128, 1024], BF16, "vo%d" % i) for i in range(2)]
    wo_ = [kb.sb([128, 32], F32, "wo_%d" % i) for i in range(2)]
    kis = kb.sb([64, 512], F32, "kis")
    kiq = kb.sb([64, 512], F32, "kiq")
    n16 = [0]

    def out16(p, nrow, dst_ap, eng="act", ntok=512):
        o = ob16[n16[0] % 3]
        n16[0] += 1
        if eng == "act":
            kb.op("act", lambda e: e.copy(out=o[0:nrow, 0:ntok], in_=p[0:nrow, 0:ntok]), reads=[p], writes=[o])
        else:
            kb.op("dve", lambda e: e.tensor_copy(out=o[0:nrow, 0:ntok], in_=p[0:nrow, 0:ntok]), reads=[p], writes=[o])
        kb.dma("sp", dst_ap, o[0:nrow, 0:ntok], reads=[o], is_output=True)

    for gi in range(NGp):
        h = norm_group(gi)
        tsl = slice(gi * 512, (gi + 1) * 512)
        for c2 in range(2):
            p = fm_mm(h, C_CQ + c2 * 128, 128)
            kb.op("act", lambda e: e.copy(out=cq[:, c2, :], in_=p[:]), reads=[p], writes=[cq.sub(c2)])
            kb.op("act", lambda e: e.activation(out=cqs[:, c2, :], in_=p[:], func=AF.Square), reads=[p], writes=[cqs.sub(c2)])
        for c2 in range(2):
            kb.op("pe", lambda e: e.matmul(pw_[:], lhsT=ones_f[:], rhs=cqs[:, c2, :], start=(c2 == 0), stop=(c2 == 1)), reads=[ones_f, cqs], writes=[pw_])
        kb.op("act", lambda e: e.activation(out=rsd[:], in_=pw_[:], func=AF.Sqrt, bias=EPS, scale=1.0 / 256), reads=[pw_], writes=[rsd])
        kb.op("dve", lambda e: e.reciprocal(out=rsd[:], in_=rsd[:]), reads=[rsd], writes=[rsd])
        for c2 in range(2):
            kb.op("dve", lambda e: e.tensor_tensor(out=cqn[:, c2, :], in0=cq[:, c2, :], in1=rsd[:], op=ALU.mult), reads=[cq, rsd], writes=[cqn.sub(c2)])
        for which, wsb, dst in ((0, wuq_b, o_qcT), (1, wiq_b, o_qiT)):
            for jc in range(8):
                p = pm[npm[0] % 3]
                npm[0] += 1
                for c2 in range(2):
                    kb.op("pe", lambda e: e.matmul(p[:], lhsT=wsb[:, c2, jc * 128:(jc + 1) * 128], rhs=cqn[:, c2, :], start=(c2 == 0), stop=(c2 == 1)),
                          reads=[wsb, cqn], writes=[p])
                out16(p, 128, dst[jc * 128:(jc + 1) * 128, tsl], "act" if jc % 2 == 0 else "dve")
        for jc in range(8):
            p = fm_mm(h, C_KC + jc * 128, 128)
            out16(p, 128, o_kcT[jc * 128:(jc + 1) * 128, tsl], "act" if jc % 2 == 0 else "dve")
        p = fm_mm(h, C_KI, 64)
        kb.op("act", lambda e: e.copy(out=kis[:], in_=p[0:64, :]), reads=[p], writes=[kis])
        kb.op("act", lambda e: e.activation(out=kiq[:], in_=p[0:64, :], func=AF.Square), reads=[p], writes=[kiq])
        kb.op("pe", lambda e: e.matmul(pw_[0:64, :], lhsT=ones_f[0:64, 0:64], rhs=kiq[:], start=True, stop=True), reads=[ones_f, kiq], writes=[pw_])
        kb.op("act", lambda e: e.activation(out=kiq[:], in_=pw_[0:64, :], func=AF.Sqrt, bias=EPS, scale=1.0 / 64), reads=[pw_], writes=[kiq])
        kb.op("dve", lambda e: e.reciprocal(out=kiq[:], in_=kiq[:]), reads=[kiq], writes=[kiq])
        o = ob16[n16[0] % 3]
        n16[0] += 1
        kb.op("dve", lambda e: e.scalar_tensor_tensor(out=o[0:64, :], in0=kis[:], scalar=gki[:, 0:1], in1=kiq[:], op0=ALU.mult, op1=ALU.mult),
              reads=[kis, gki, kiq], writes=[o])
        kb.dma("sp", o_kiT[:, tsl], o[0:64, :], reads=[o], is_output=True)
        for j in range(4):
            ti = gi * 4 + j
            v_ = vo[ti % 2]
            for half in range(2):
                p = pm[npm[0] % 3]
                npm[0] += 1
                for c in range(NCH):
                    kb.op("pe", lambda e: e.matmul(p[:], lhsT=h[:, c, j * 128:(j + 1) * 128], rhs=wb[:, c, C_VC + half * 512:C_VC + (half + 1) * 512],
                                                   start=(c == 0), stop=(c == NCH - 1)), reads=[h, wb], writes=[p])
                kb.op("act" if half == 0 else "dve",
                      (lambda e: e.copy(out=v_[:, 0:512], in_=p[:])) if half == 0 else (lambda e: e.tensor_copy(out=v_[:, 512:1024], in_=p[:])),
                      reads=[p], writes=[v_.sub(half)])
            kb.dma("pool", o_vc[ti * 128:(ti + 1) * 128, :], v_[:], reads=[v_], is_output=True)
            p = pm[npm[0] % 3]
            npm[0] += 1
            for c in range(NCH):
                kb.op("pe", lambda e: e.matmul(p[:, 0:16], lhsT=h[:, c, j * 128:(j + 1) * 128], rhs=wb[:, c, C_WI:C_WI + 16],
                                               start=(c == 0), stop=(c == NCH - 1)), reads=[h, wb], writes=[p])
            w2 = wo_[ti % 2]
            kb.op("act", lambda e: e.activation(out=w2[:, 0:16], in_=p[:, 0:16], func=AF.Abs, scale=float(16 ** -0.5 * 64 ** -0.5)),
                  reads=[p], writes=[w2.sub(0)])
            kb.op("act", lambda e: e.activation(out=w2[:, 16:32], in_=p[:, 0:16], func=AF.Sign), reads=[p], writes=[w2.sub(1)])
            kb.dma("pool", o_wi[ti * 128:(ti + 1) * 128, :], w2[:], reads=[w2], is_output=True)

    load_w(C_QKV, 3072)
    cwt = kb.sb([128, 4, 24], F32, "cwt")
    for k_ in range(4):
        kb.dma("sp", cwt[:, k_, :], cw[k_].rearrange("(c p) -> p c", p=128), writes=[cwt.sub(k_)], allow_slow_non_contiguous=True)
    xc = [kb.sb([128, 515], F32, "xc%d" % i) for i in range(2)]
    carry = kb.sb([128, 24, 3], F32, "carry")
    acc = [kb.sb([128, 512], F32, "acc%d" % i) for i in range(2)]
    sq_ = rsd
    of32 = [kb.sb([128, 512], F32, "of32_%d" % i) for i in range(3)]
    nxc = 0
    for gi in range(-1, NGp):
        if gi < 0:
            h = hnT[1]
            nt.run(x[0:128, :], h, slice(0, 128))
            ntok = 128
        else:
            h = norm_group(gi)
            ntok = 512
        for cc in range(24):
            p = fm_mm(h, cc * 128, 128, ntok)
            xb = xc[nxc % 2]
            a_ = acc[nxc % 2]
            nxc += 1
            if gi >= 0:
                kb.op("dve", lambda e: e.tensor_copy(out=xb[:, 0:3], in_=carry[:, cc, :]), reads=[carry.sub(cc)], writes=[xb])
            kb.op("act", lambda e: e.copy(out=xb[:, 3:3 + ntok], in_=p[:, 0:ntok]), reads=[p], writes=[xb])
            kb.op("dve", lambda e: e.tensor_copy(out=carry[:, cc, :], in_=xb[:, ntok:ntok + 3]), reads=[xb], writes=[carry.sub(cc)])
            if gi < 0:
                continue
            kb.op("dve", lambda e: e.tensor_scalar_mul(out=a_[:], in0=xb[:, 3:515], scalar1=cwt[:, 3, cc:cc + 1]), reads=[xb, cwt], writes=[a_])
            for k_ in range(3):
                kb.op("dve", lambda e: e.scalar_tensor_tensor(out=a_[:], in0=xb[:, k_:k_ + 512], scalar=cwt[:, k_, cc:cc + 1], in1=a_[:],
                                                              op0=ALU.mult, op1=ALU.add), reads=[xb, cwt, a_], writes=[a_])
            o = of32[nxc % 3]
            kb.op("act", lambda e: e.activation(out=o[:], in_=a_[:], func=AF.Silu), reads=[a_], writes=[o])
            if cc < 16:
                kb.op("act", lambda e: e.activation(out=sq_[:], in_=o[:], func=AF.Square), reads=[o], writes=[sq_])
                kb.op("pe", lambda e: e.matmul(pw_[:], lhsT=ones_f[:], rhs=sq_[:], start=True, stop=True), reads=[ones_f, sq_], writes=[pw_])
                kb.op("act", lambda e: e.activation(out=sq_[:], in_=pw_[:], func=AF.Sqrt, bias=EPS, scale=1.0), reads=[pw_], writes=[sq_])
                kb.op("dve", lambda e: e.reciprocal(out=sq_[:], in_=sq_[:]), reads=[sq_], writes=[sq_])
                kb.op("dve", lambda e: e.tensor_tensor(out=o[:], in0=o[:], in1=sq_[:], op=ALU.mult), reads=[o, sq_], writes=[o])
            kb.dma("pool" if cc % 2 else "sp", o_qkvT[cc * 128:(cc + 1) * 128, gi * 512:(gi + 1) * 512], o[:], reads=[o], is_output=True)

    load_w(C_BETA, 1040)
    ab = kb.sb([16, 2], F32, "ab")
    kb.op("pool", lambda e: e.memset(ab[:], 0.0), writes=[ab])
    kb.dma("sp", ab[8:16, 0:1], dtb.rearrange("(p o) -> p o", o=1), reads=[], writes=[ab], allow_slow_non_contiguous=True)
    kb.dma("sp", ab[8:16, 1:2], alog.rearrange("(p o) -> p o", o=1), reads=[], writes=[ab], allow_slow_non_contiguous=True)
    nea = kb.sb([16, 1], F32, "nea")
    kb.op("act", lambda e: e.activation(out=nea[:], in_=ab[:, 1:2], func=AF.Exp), reads=[ab], writes=[nea])
    kb.op("dve", lambda e: e.tensor_scalar_mul(out=nea[:], in0=nea[:], scalar1=-1.0), reads=[nea], writes=[nea])
    gbt = [acc[0], acc[1]]
    gtm = [of32[0], of32[1]]
    for gi in range(NGp):
        h = norm_group(gi)
        tsl = slice(gi * 512, (gi + 1) * 512)
        p = fm_mm(h, 0, 16)
        t_ = gbt[gi % 2]
        g_ = gtm[gi % 2]
        kb.op("act", lambda e: e.activation(out=g_[0:16, :], in_=p[0:16, :], func=AF.Exp, bias=ab[:, 0:1]), reads=[p, ab], writes=[g_])
        kb.op("act", lambda e: e.activation(out=g_[0:16, :], in_=g_[0:16, :], func=AF.Ln, bias=1.0), reads=[g_], writes=[g_])
        kb.op("dve", lambda e: e.tensor_scalar_mul(out=g_[0:16, :], in0=g_[0:16, :], scalar1=nea[:, 0:1]), reads=[g_, nea], writes=[g_])
        kb.op("act", lambda e: e.activation(out=t_[0:16, :], in_=p[0:16, :], func=AF.Sigmoid), reads=[p], writes=[t_])
        kb.dma("sp", o_gb[0:8, tsl], t_[0:8, :], reads=[t_], is_output=True)
        kb.dma("sp", o_gb[8:16, tsl], g_[8:16, :], reads=[g_], is_output=True)
        for jc in range(8):
            p = fm_mm(h, 16 + jc * 128, 128)
            o = ob16[n16[0] % 3]
            n16[0] += 1
            kb.op("act", lambda e: e.activation(out=o[:], in_=p[:], func=AF.Silu), reads=[p], writes=[o])
            kb.dma("pool", o_szT[jc * 128:(jc + 1) * 128, tsl], o[:], reads=[o], is_output=True)
    return kb.finish()


def build_p6(S, NU=2, NBC=32):
    kb = KB()
    CH = 64
    BT = NBC * CH
    NBLK = S // BT
    qT = kb.dram("qT", [NU, 128, S], F32, "ExternalInput")
    kT = kb.dram("kT", [NU, 128, S], F32, "ExternalInput")
    vT = kb.dram("vT", [NU, 128, S], F32, "ExternalInput")
    gg = kb.dram("g", [NU, S], F32, "ExternalInput")
    bb = kb.dram("beta", [NU, S], F32, "ExternalInput")
    szT = kb.dram("szT", [NU, 128, S], BF16, "ExternalInput")
    gno = kb.dram("gno", [128], F32, "ExternalInput")
    odT = kb.dram("odT", [NU, 128, S], BF16, "ExternalOutput")

    identf = emit_identity(kb, F32)
    ones_f = kb.sb([128, 128], F32, "ones_f")
    kb.op("pool", lambda e: e.memset(ones_f[:], 1.0), writes=[ones_f])
    Lt = kb.sb([64, 64], F32, "Lt")
    kb.op("pool", lambda e: e.memset(Lt[:], 1.0), writes=[Lt])
    kb.op("pool", lambda e: e.affine_select(out=Lt[:], in_=Lt[:], pattern=[[1, 64]], compare_op=ALU.is_ge, fill=0.0, base=0,
                                             channel_multiplier=-1), reads=[Lt], writes=[Lt])
    gnt = kb.sb([128, 1], F32, "gnt")
    kb.dma("sp", gnt[:], gno.rearrange("(p o) -> p o", o=1), writes=[gnt], allow_slow_non_contiguous=True)

    B = [kb.ps([128, 512], F32, "B%d" % i) for i in range(8)]
    qb = [kb.sb([128, BT], F32, "qb%d" % i) for i in range(2)]
    kbk = [kb.sb([128, BT], F32, "kbk%d" % i) for i in range(2)]
    vb = [kb.sb([128, BT], F32, "vb%d" % i) for i in range(2)]
    szb = [kb.sb([128, BT], BF16, "szb%d" % i) for i in range(2)]
    gB = [kb.sb([64, NBC], F32, "gB%d" % i) for i in range(2)]
    bB = [kb.sb([64, NBC], F32, "bB%d" % i) for i in range(2)]
    gcB = [kb.sb([64, NBC], F32, "gcB%d" % i) for i in range(2)]
    egl = [kb.sb([128, NBC], F32, "egl%d" % i) for i in range(2)]
    ekg = [kb.sb([64, NBC], F32, "ekg%d" % i) for i in range(2)]
    sqe = [kb.sb([64, NBC], F32, "sqe%d" % i) for i in range(2)]
    bw = [kb.sb([64, NBC], F32, "bw%d" % i) for i in range(2)]
    nbt = [kb.sb([64, NBC], F32, "nbt%d" % i) for i in range(2)]
    ob = [kb.sb([128, BT], BF16, "ob%d" % i) for i in range(2)]
    rr = [kb.sb([64, 256], F32, "rr%d" % i) for i in range(4)]
    kg = [kb.sb([64, 128], F32, "kg%d" % i) for i in range(2)]
    dg = [kb.sb([64, 64], F32, "dg%d" % i) for i in range(2)]
    Dm = [kb.sb([64, 64], F32, "Dm%d" % i) for i in range(2)]
    CC = [kb.sb([64, 128], F32, "CC%d" % i) for i in range(4)]
    c0a = [kb.sb([64, 128], F32, "c0a%d" % i) for i in range(2)]
    aT = [kb.sb([64, 64], F32, "aT%d" % i) for i in range(2)]
    wT = [kb.sb([128, 64], F32, "wT%d" % i) for i in range(2)]
    vnew = [kb.sb([64, 128], F32, "vnew%d" % i) for i in range(2)]
    t1 = [kb.sb([64, 128], F32, "t1_%d" % i) for i in range(2)]
    ot = [kb.sb([64, 128], F32, "ot%d" % i) for i in range(2)]
    osq = kb.sb([64, 128], F32, "osq")
    oss = [kb.sb([64, 1], F32, "oss%d" % i) for i in range(2)]
    St = [kb.sb([128, 128], F32, "St%d" % i) for i in range(2)]
    nblk = 0
    nch = 0
    for u in range(NU):
        S_cur = St[0]
        kb.op("pool", lambda e: e.memset(S_cur[:], 0.0), writes=[S_cur])
        si = 0
        for blk in range(NBLK):
            k2 = nblk % 2
            nblk += 1
            tsl = slice(blk * BT, (blk + 1) * BT)
            kb.dma("sp", qb[k2][:], qT[u, :, tsl], writes=[qb[k2]])
            kb.dma("pool", kbk[k2][:], kT[u, :, tsl], writes=[kbk[k2]])
            kb.dma("sp", vb[k2][:], vT[u, :, tsl], writes=[vb[k2]])
            kb.dma("pool", szb[k2][:], szT[u, :, tsl], writes=[szb[k2]])
            kb.dma("sp", gB[k2][:], gg[u, tsl].rearrange("(n t) -> t n", t=CH), writes=[gB[k2]], allow_slow_non_contiguous=True)
            kb.dma("pool", bB[k2][:], bb[u, tsl].rearrange("(n t) -> t n", t=CH), writes=[bB[k2]], allow_slow_non_contiguous=True)
            pg = B[3]
            kb.op("pe", lambda e: e.matmul(pg[0:64, 0:NBC], lhsT=Lt[:], rhs=gB[k2][:], start=True, stop=True), reads=[Lt, gB[k2]], writes=[pg])
            kb.op("pe", lambda e: e.matmul(pg[:, 64:64 + NBC], lhsT=ones_f[0:64, :], rhs=gB[k2][:], start=True, stop=True), reads=[ones_f, gB[k2]], writes=[pg])
            kb.op("act", lambda e: e.copy(out=gcB[k2][:], in_=pg[0:64, 0:NBC]), reads=[pg], writes=[gcB[k2]])
            kb.op("act", lambda e: e.activation(out=egl[k2][:], in_=pg[:, 64:64 + NBC], func=AF.Exp), reads=[pg], writes=[egl[k2]])
            kb.op("dve", lambda e: e.tensor_tensor(out=ekg[k2][:], in0=pg[0:64, 64:64 + NBC], in1=gcB[k2][:], op=ALU.subtract), reads=[pg, gcB[k2]], writes=[ekg[k2]])
            kb.op("act", lambda e: e.activation(out=ekg[k2][:], in_=ekg[k2][:], func=AF.Exp), reads=[ekg[k2]], writes=[ekg[k2]])
            kb.op("act", lambda e: e.activation(out=sqe[k2][:], in_=gcB[k2][:], func=AF.Exp), reads=[gcB[k2]], writes=[sqe[k2]])
            kb.op("dve", lambda e: e.tensor_tensor(out=bw[k2][:], in0=sqe[k2][:], in1=bB[k2][:], op=ALU.mult), reads=[sqe[k2], bB[k2]], writes=[bw[k2]])
            kb.op("dve", lambda e: e.tensor_scalar_mul(out=sqe[k2][:], in0=sqe[k2][:], scalar1=float(128 ** -0.5)), reads=[sqe[k2]], writes=[sqe[k2]])
            kb.op("dve", lambda e: e.tensor_scalar_mul(out=nbt[k2][:], in0=bB[k2][:], scalar1=-1.0), reads=[bB[k2]], writes=[nbt[k2]])
            for n in range(NBC):
                c2 = nch % 2
                nch += 1
                cs = slice(n * CH, (n + 1) * CH)
                kTc, qTc, vTc = kbk[k2][:, cs], qb[k2][:, cs], vb[k2][:, cs]
                col = lambda t_: t_[:, n:n + 1]
                p0 = B[0]
                kb.op("pe", lambda e: e.transpose(out=p0[0:64, 0:128], in_=vTc, identity=identf[:]), reads=[vb[k2], identf], writes=[p0])
                kb.op("pe", lambda e: e.transpose(out=p0[0:64, 128:256], in_=kTc, identity=identf[:]), reads=[kbk[k2], identf], writes=[p0])
                r0 = rr[(nch * 2) % 4]
                r1 = rr[(nch * 2 + 1) % 4]
                kb.op("dve", lambda e: e.tensor_scalar_mul(out=r0[:, 0:128], in0=p0[0:64, 0:128], scalar1=col(bB[k2])), reads=[p0, bB[k2]], writes=[r0])
                kb.op("act", lambda e: e.activation(out=r0[:, 128:256], in_=p0[0:64, 128:256], func=AF.Copy, scale=col(bw[k2])), reads=[p0, bw[k2]], writes=[r0])
                kg_ = kg[c2]
                kb.op("act", lambda e: e.activation(out=kg_[:], in_=p0[0:64, 128:256], func=AF.Copy, scale=col(ekg[k2])), reads=[p0, ekg[k2]], writes=[kg_])
                dg_ = dg[c2]
                kb.op("dve", lambda e: e.tensor_scalar_mul(out=dg_[:], in0=identf[0:64, 0:64], scalar1=col(gcB[k2])), reads=[identf, gcB[k2]], writes=[dg_])
                p1 = B[1]
                kb.op("pe", lambda e: e.matmul(p1[0:64, 0:64], lhsT=ones_f[0:64, 0:64], rhs=dg_[:], start=True, stop=True), reads=[ones_f, dg_], writes=[p1])
                kb.op("pe", lambda e: e.matmul(p1[0:64, 64:128], lhsT=kTc, rhs=kTc, start=True, stop=True), reads=[kbk[k2]], writes=[p1])
                kb.op("pe", lambda e: e.matmul(p1[0:64, 128:192], lhsT=qTc, rhs=kTc, start=True, stop=True), reads=[qb[k2], kbk[k2]], writes=[p1])
                Dm_ = Dm[c2]
                kb.op("act", lambda e: e.activation(out=Dm_[:], in_=p1[0:64, 0:64], func=AF.Exp, scale=-1.0, bias=col(gcB[k2])), reads=[p1, gcB[k2]], writes=[Dm_])
                kb.op("pool", lambda e: e.affine_select(out=Dm_[:], in_=Dm_[:], pattern=[[-1, 64]], compare_op=ALU.is_ge, fill=0.0, base=0,
                                                         channel_multiplier=1), reads=[Dm_], writes=[Dm_])
                ca = c0a[c2]
                kb.op("dve", lambda e: e.scalar_tensor_tensor(out=ca[:, 0:64], in0=p1[0:64, 64:128], scalar=col(nbt[k2]), in1=Dm_[:], op0=ALU.mult, op1=ALU.mult),
                      reads=[p1, nbt[k2], Dm_], writes=[ca])
                kb.op("dve", lambda e: e.scalar_tensor_tensor(out=ca[:, 64:128], in0=p1[0:64, 128:192], scalar=float(128 ** -0.5), in1=Dm_[:], op0=ALU.mult, op1=ALU.mult),
                      reads=[p1, Dm_], writes=[ca])
                kb.op("pool", lambda e: e.affine_select(out=ca[:, 0:64], in_=ca[:, 0:64], pattern=[[-1, 64]], compare_op=ALU.is_gt, fill=0.0, base=0,
                                                         channel_multiplier=1), reads=[ca], writes=[ca])
                p2 = B[2]
                kb.op("pe", lambda e: e.transpose(out=p2[0:64, 0:64], in_=ca[:, 0:64], identity=identf[0:64, 0:64]), reads=[ca, identf], writes=[p2])
                kb.op("pe", lambda e: e.transpose(out=p2[0:64, 64:128], in_=ca[:, 64:128], identity=identf[0:64, 0:64]), reads=[ca, identf], writes=[p2])
                cc = CC[(nch * 2) % 4]
                cc2 = CC[(nch * 2 + 1) % 4]
                kb.op("act", lambda e: e.copy(out=cc[:, 0:64], in_=ca[:, 0:64]), reads=[ca], writes=[cc])
                kb.op("dve", lambda e: e.tensor_copy(out=cc[:, 64:128], in_=p2[0:64, 0:64]), reads=[p2], writes=[cc])
                aT_ = aT[c2]
                kb.op("act", lambda e: e.copy(out=aT_[:], in_=p2[0:64, 64:128]), reads=[p2], writes=[aT_])
                rc, rn = r0, r1
                ck, cn = cc, cc2
                for k_ in range(6):
                    p3 = B[3]
                    kb.op("pe", lambda e: e.matmul(p3[0:64, 0:256], lhsT=ck[:, 64:128], rhs=rc[:], start=True, stop=True), reads=[ck, rc], writes=[p3])
                    kb.op("dve", lambda e: e.tensor_tensor(out=rn[:], in0=p3[0:64, 0:256], in1=rc[:], op=ALU.add), reads=[p3, rc], writes=[rn])
                    rc, rn = rn, rc
                    if k_ < 5:
                        p4 = B[4]
                        kb.op("pe", lambda e: e.matmul(p4[0:64, 0:64], lhsT=ck[:, 64:128], rhs=ck[:, 0:64], start=True, stop=True), reads=[ck], writes=[p4])
                        kb.op("pe", lambda e: e.matmul(p4[0:64, 64:128], lhsT=ck[:, 0:64], rhs=ck[:, 64:128], start=True, stop=True), reads=[ck], writes=[p4])
                        kb.op("act", lambda e: e.copy(out=cn[:], in_=p4[0:64, 0:128]), reads=[p4], writes=[cn])
                        ck, cn = cn, ck
                kb.op("pe", lambda e: e.transpose(out=p0[:, 256:320], in_=rc[:, 128:256], identity=identf[0:64, 0:64]), reads=[rc, identf], writes=[p0])
                wT_ = wT[c2]
                kb.op("act", lambda e: e.copy(out=wT_[:], in_=p0[:, 256:320]), reads=[p0], writes=[wT_])
                p5 = B[5]
                kb.op("pe", lambda e: e.matmul(p5[0:64, 0:128], lhsT=wT_[:], rhs=S_cur[:], start=True, stop=True), reads=[wT_, S_cur], writes=[p5])
                kb.op("pe", lambda e: e.matmul(p5[0:64, 128:256], lhsT=qTc, rhs=S_cur[:], start=True, stop=True), reads=[qb[k2], S_cur], writes=[p5])
                vn = vnew[c2]
                kb.op("dve", lambda e: e.tensor_tensor(out=vn[:], in0=rc[:, 0:128], in1=p5[0:64, 0:128], op=ALU.subtract), reads=[rc, p5], writes=[vn])
                t1_ = t1[c2]
                kb.op("act", lambda e: e.activation(out=t1_[:], in_=p5[0:64, 128:256], func=AF.Copy, scale=col(sqe[k2])), reads=[p5, sqe[k2]], writes=[t1_])
                p6 = B[6]
                kb.op("pe", lambda e: e.matmul(p6[0:64, 0:128], lhsT=aT_[:], rhs=vn[:], start=True, stop=True), reads=[aT_, vn], writes=[p6])
                p7 = B[7]
                kb.op("pe", lambda e: e.matmul(p7[:, 0:128], lhsT=kg_[:], rhs=vn[:], start=True, stop=True), reads=[kg_, vn], writes=[p7])
                S_nxt = St[1 - si]
                kb.op("dve", lambda e: e.scalar_tensor_tensor(out=S_nxt[:], in0=S_cur[:], scalar=col(egl[k2]), in1=p7[:, 0:128], op0=ALU.mult, op1=ALU.add),
                      reads=[S_cur, egl[k2], p7], writes=[S_nxt])
                S_cur = S_nxt
                si = 1 - si
                o_ = ot[c2]
                kb.op("dve", lambda e: e.tensor_tensor(out=o_[:], in0=p6[0:64, 0:128], in1=t1_[:], op=ALU.add), reads=[p6, t1_], writes=[o_])
                ss_ = oss[c2]
                kb.op("act", lambda e: e.activation(out=osq[:], in_=o_[:], func=AF.Square, accum_out=ss_[:]), reads=[o_], writes=[osq, ss_])
                kb.op("act", lambda e: e.activation(out=ss_[:], in_=ss_[:], func=AF.Sqrt, bias=EPS, scale=1.0 / 128), reads=[ss_], writes=[ss_])
                kb.op("dve", lambda e: e.reciprocal(out=ss_[:], in_=ss_[:]), reads=[ss_], writes=[ss_])
                kb.op("dve", lambda e: e.tensor_scalar_mul(out=o_[:], in0=o_[:], scalar1=ss_[:, 0:1]), reads=[o_, ss_], writes=[o_])
                kb.op("pe", lambda e: e.transpose(out=p2[:, 128:192], in_=o_[:], identity=identf[0:64, 0:64]), reads=[o_, identf], writes=[p2])
                kb.op("dve", lambda e: e.scalar_tensor_tensor(out=ob[k2][:, cs], in0=p2[:, 128:192], scalar=gnt[:, 0:1], in1=szb[k2][:, cs], op0=ALU.mult, op1=ALU.mult),
                      reads=[p2, gnt, szb[k2]], writes=[ob[k2].sub(n)])
            kb.dma("sp", odT[u, :, tsl], ob[k2][:], reads=[ob[k2]], is_output=True)
    return kb.finish()


MARK = -2.0e30


def build_p5(S, TOPK=256):
    kb = KB()
    NR = S // 512
    NB = S // 128
    kiT = kb.dram("kiT", [64, S], BF16, "ExternalInput")
    qiT = kb.dram("qiT", [NR, 64, 16, 128], BF16, "ExternalInput")
    wi = kb.dram("wi", [NR, 128, 32], F32, "ExternalInput")
    cm = kb.dram("cm", [128, 512], F32, "ExternalInput")
    cm01 = kb.dram("cm01", [128, 512], BF16, "ExternalInput")
    kcT = kb.dram("kcT", [8, 128, S], BF16, "ExternalInput")
    vc = kb.dram("vc", [S, 1024], BF16, "ExternalInput")
    qc = kb.dram("qc", [NR, 8, 128, 128], BF16, "ExternalInput")
    BTd = kb.dram("BT", [8, 128, 16, 128], F32, "ExternalInput")
    b31 = kb.dram("b31", [128, 8], F32, "ExternalInput")
    ocT = kb.dram("ocT", [NR, 8, 128, 128], BF16, "ExternalOutput")
    MS = kb.nc.dram_tensor("MS", [NR, 128, NR * 512], BF16).ap()
    ms_t = [T(None) for _ in range(NR)]

    ident = emit_identity(kb)
    ones_b = kb.sb([128, 128], BF16, "ones_b")
    kb.op("pool", lambda e: e.memset(ones_b[:], 1.0), writes=[ones_b])
    kis = kb.sb([64, S], BF16, "kis")
    kb.dma("sp", kis[:], kiT, writes=[kis])
    cmt = kb.sb([128, 512], F32, "cmt")
    kb.dma("pool", cmt[:], cm, writes=[cmt])
    cm1 = kb.sb([128, 512], BF16, "cm1")
    kb.dma("pool", cm1[:], cm01, writes=[cm1])
    b31t = kb.sb([128, 8], F32, "b31t")
    kb.dma("sp", b31t[:], b31, writes=[b31t])
    work = kb.sb([128, S], F32, "work")
    mk = kb.sb([128, S], BF16, "mk")
    B = [kb.ps([128, 512], F32, "B%d" % i) for i in range(8)]
    qit = [kb.sb([64, 16, 128], BF16, "qit%d" % i) for i in range(2)]
    wit = [kb.sb([128, 32], F32, "wit%d" % i) for i in range(2)]
    rt = [kb.sb([128, 512], F32, "rt%d" % i) for i in range(2)]
    m8 = kb.sb([128, 8], F32, "m8")
    mts = [kb.sb([128, 512], BF16, "mts%d" % i) for i in range(3)]
    nps = 0
    nmt = 0
    for m in range(NR):
        q_, w_ = qit[m % 2], wit[m % 2]
        kb.dma("sp", q_[:], qiT[m], writes=[q_])
        kb.dma("pool", w_[:], wi[m], writes=[w_])
        nel = (m + 1) * 512
        for kg in range(m + 1):
            gsl = slice(kg * 512, (kg + 1) * 512)
            wk = work.sub(("g", kg))
            for h in range(16):
                ps = B[nps % 2]
                r_ = rt[nps % 2]
                nps += 1
                kb.op("pe", lambda e: e.matmul(ps[:], lhsT=q_[:, h, :], rhs=kis[:, gsl], start=True, stop=True), reads=[q_, kis], writes=[ps])
                kb.op("act", lambda e: e.activation(out=r_[:], in_=ps[:], func=AF.Relu, scale=w_[:, h:h + 1]), reads=[ps, w_], writes=[r_])
                if h == 0:
                    kb.op("dve", lambda e: e.tensor_scalar_mul(out=work[:, gsl], in0=r_[:], scalar1=w_[:, 16:17]), reads=[r_, w_], writes=[wk])
                else:
                    kb.op("dve", lambda e: e.scalar_tensor_tensor(out=work[:, gsl], in0=r_[:], scalar=w_[:, 16 + h:17 + h], in1=work[:, gsl],
                                                                  op0=ALU.mult, op1=ALU.add), reads=[r_, w_, wk], writes=[wk])
            if kg == m:
                kb.op("dve", lambda e: e.tensor_tensor(out=work[:, gsl], in0=work[:, gsl], in1=cmt[:], op=ALU.add), reads=[wk, cmt], writes=[wk])
        for rd in range(TOPK // 8):
            kb.op("dve", lambda e: e.max(out=m8[:], in_=work[:, 0:nel]), reads=[work], writes=[m8])
            kb.op("dve", lambda e: e.match_replace(out=work[:, 0:nel], in_to_replace=m8[:], in_values=work[:, 0:nel], imm_value=MARK),
                  reads=[work, m8], writes=[work])
        kb.op("dve", lambda e: e.tensor_single_scalar(out=mk[:, 0:nel], in_=work[:, 0:nel], scalar=-1.5e30, op=ALU.is_lt), reads=[work], writes=[mk])
        kb.op("dve", lambda e: e.tensor_tensor(out=mk[:, m * 512:nel], in0=mk[:, m * 512:nel], in1=cm1[:], op=ALU.mult), reads=[mk, cm1], writes=[mk])
        for kg in range(m + 1):
            pt = B[2 + kg % 2]
            ptv = bf16_view(pt)
            for jj in range(4):
                blk = kg * 4 + jj
                kb.op("pe", lambda e: e.transpose(out=ptv[:, jj * 128:(jj + 1) * 128], in_=mk[:, blk * 128:(blk + 1) * 128], identity=ident[:]),
                      reads=[mk, ident], writes=[pt])
            ms_ = mts[nmt % 3]
            nmt += 1
            if kg % 2 == 0:
                kb.op("act", lambda e: e.copy(out=ms_[:], in_=ptv[:, 0:512]), reads=[pt], writes=[ms_])
            else:
                kb.op("dve", lambda e: e.tensor_copy(out=ms_[:], in_=ptv[:, 0:512]), reads=[pt], writes=[ms_])
            kb.dma("pool", MS[m, :, kg * 512:(kg + 1) * 512], ms_[:], reads=[ms_], writes=[ms_t[m]])
    wbf = work[:].bitcast(BF16)
    mtb = [work.sub(("mt", 0)), work.sub(("mt", 1))]
    ks = kb.sb([128, S], BF16, "ks")
    vs = mk.sub("vs")
    vs.h = mk[:].rearrange("p (j d) -> p j d", d=128)
    bts = kb.sb([128, 16, 128], BF16, "bts")
    btf = kb.sb([128, 16, 128], F32, "btf")
    qct = [kb.sb([128, 128], BF16, "qct%d" % i) for i in range(2)]
    pt_ = [kb.sb([128, 512], BF16, "pt_%d" % i) for i in range(2)]
    pmt = [kb.sb([128, 512], BF16, "pmt%d" % i) for i in range(2)]
    rden = kb.sb([128, 128], F32, "rden")
    ost = [kb.sb([128, 128], BF16, "ost%d" % i) for i in range(2)]
    nz = 0
    nrd = 0
    for h in range(8):
        kb.dma("sp", ks[:], kcT[h], writes=[ks])
        nv = max(1, NB // 16)
        for i in range(nv):
            j0, j1 = i * NB // nv, (i + 1) * NB // nv
            kb.dma("sp" if i % 2 == 0 else "pool", vs[:, j0:j1, :], vc[j0 * 128:j1 * 128, h * 128:(h + 1) * 128].rearrange("(j p) d -> p j d", p=128),
                   writes=[vs])
        kb.dma("pool", btf[:], BTd[h], writes=[btf])
        kb.op("dve", lambda e: e.tensor_copy(out=bts[:], in_=btf[:]), reads=[btf], writes=[bts])
        for m in range(NR):
            k2 = nrd % 2
            nrd += 1
            nel = (m + 1) * 512
            mt_ap = wbf[:, k2 * S:k2 * S + nel]
            kb.dma("sp", mt_ap, MS[m, :, 0:nel], reads=[ms_t[m]], writes=[mtb[k2]])
            qc_ = qct[k2]
            kb.dma("pool", qc_[:], qc[m, h], writes=[qc_])
            po, pden = B[6], B[7]
            ngrp = m + 1
            for kg in range(ngrp):
                near = (m - kg) <= 3
                pz = B[4 + nz % 2]
                p_ = pt_[nz % 2]
                pm_ = pmt[nz % 2]
                nz += 1
                for jj in range(4):
                    j = kg * 4 + jj
                    kb.op("pe", lambda e: e.matmul(pz[:, jj * 128:(jj + 1) * 128], lhsT=ks[:, j * 128:(j + 1) * 128], rhs=qc_[:], start=True, stop=not near),
                          reads=[ks, qc_], writes=[pz])
                    if near:
                        e_ = 4 * (m - kg) + (3 - jj)
                        kb.op("pe", lambda e: e.matmul(pz[:, jj * 128:(jj + 1) * 128], lhsT=ident[:], rhs=bts[:, e_, :], start=False, stop=True),
                              reads=[ident, bts], writes=[pz])
                if near:
                    kb.op("act", lambda e: e.activation(out=p_[:], in_=pz[:], func=AF.Exp), reads=[pz], writes=[p_])
                else:
                    kb.op("act", lambda e: e.activation(out=p_[:], in_=pz[:], func=AF.Exp, bias=b31t[:, h:h + 1]), reads=[pz, b31t], writes=[p_])
                kb.op("dve", lambda e: e.tensor_tensor(out=pm_[:], in0=p_[:], in1=wbf[:, k2 * S + kg * 512:k2 * S + (kg + 1) * 512], op=ALU.mult),
                      reads=[p_, mtb[k2]], writes=[pm_])
                for jj in range(4):
                    j = kg * 4 + jj
                    first = (kg == 0 and jj == 0)
                    last = (kg == ngrp - 1 and jj == 3)
                    kb.op("pe", lambda e: e.matmul(po[:, 0:128], lhsT=vs[:, j, :], rhs=pm_[:, jj * 128:(jj + 1) * 128], start=first, stop=last),
                          reads=[vs, pm_], writes=[po])
                    kb.op("pe", lambda e: e.matmul(pden[:, 0:128], lhsT=ones_b[:], rhs=pm_[:, jj * 128:(jj + 1) * 128], start=first, stop=last),
                          reads=[ones_b, pm_], writes=[pden])
            kb.op("dve", lambda e: e.reciprocal(out=rden[:], in_=pden[:, 0:128]), reads=[pden], writes=[rden])
            o_ = ost[k2]
            kb.op("dve", lambda e: e.tensor_tensor(out=o_[:], in0=po[:, 0:128], in1=rden[:], op=ALU.mult), reads=[po, rden], writes=[o_])
            kb.dma("pool", ocT[m, h], o_[:], reads=[o_], is_output=True)
    return kb.finish()


def _t5_bucket_np(dist):
    n = np.maximum(dist, 0)
    nf = np.maximum(n, 1).astype(np.float32)
    lr = np.log(nf / np.float32(16)) / np.float32(math.log(2048 / 16))
    large = 16 + (lr * np.float32(16)).astype(np.int32)
    return np.where(n < 16, n, np.minimum(large, 31))


def _dsa_consts(r, rel_bias):
    t = np.arange(128)[:, None]
    sp = np.arange(512)[None, :]
    ok = sp <= 128 * r + t
    cm = np.where(ok, 0.0, -1.0e30).astype(np.float32)
    cm01 = ok.astype(np.float32).astype(_BF)
    s = np.arange(128)[:, None, None]
    e = np.arange(16)[None, :, None]
    tt = np.arange(128)[None, None, :]
    dist = 128 * (e - 3 + r) + tt - s
    bucket = _t5_bucket_np(dist)
    BT = np.ascontiguousarray(np.transpose(rel_bias[bucket], (3, 0, 1, 2)))
    BT = np.where((dist >= 0)[None], BT, 0.0).astype(np.float32)
    b31 = np.ascontiguousarray(np.broadcast_to(rel_bias[31][None, :], (128, 8))).astype(np.float32)
    return cm, cm01, BT, b31
```

```python
from contextlib import ExitStack
import numpy as np
import math
import concourse.bass as bass
import concourse.mybir as mybir
from concourse.bass_utils import run_bass_kernel_spmd

F32 = mybir.dt.float32
BF16 = mybir.dt.bfloat16
I32 = mybir.dt.int32
U32 = mybir.dt.uint32
AF = mybir.ActivationFunctionType
ALU = mybir.AluOpType
AX = mybir.AxisListType

SAME_ENGINE_SYNC = True
NDMA_SEM = 6


class T:
    def __init__(self, h, parent=None, atomic=False):
        self.h = h
        self.parent = parent
        self.atomic = atomic
        self.kids = {}
        self.w = None
        self.r = []

    def sub(self, key):
        if self.atomic:
            return self
        if key not in self.kids:
            self.kids[key] = T(self.h, self)
        return self.kids[key]

    def __getitem__(self, idx):
        return self.h[idx]

    def _related(self):
        out = [self]
        p = self.parent
        while p is not None:
            out.append(p)
            p = p.parent
        stack = list(self.kids.values())
        while stack:
            k = stack.pop()
            out.append(k)
            stack.extend(k.kids.values())
        return out


class KB:
    def __init__(self):
        self.nc = bass.Bass("TRN2", target_bir_lowering=False)
        nc = self.nc
        self.es = ExitStack()
        self.es.enter_context(nc.allow_low_precision("bf16 matmul operands, fp32 accumulation"))
        self.eng = {"pe": nc.tensor, "act": nc.scalar, "dve": nc.vector, "pool": nc.gpsimd, "sp": nc.sync}
        self.sem = {}
        self.cnt = {}
        for e in ("pe", "act", "dve", "pool"):
            self.sem[e] = self.es.enter_context(nc.semaphore("s_" + e))
            self.cnt[e] = 0
        self.dsem = {}
        self.dcnt = {}
        for q in ("sp", "pool", "act"):
            self.dsem[q] = [self.es.enter_context(nc.semaphore("d_%s%d" % (q, i))) for i in range(NDMA_SEM)]
            self.dcnt[q] = 0
        self.waited = {e: {} for e in self.eng}
        self.out_tokens = []
        self.n_inst = 0
        self._uid = 0

    def dram(self, name, shape, dt, kind):
        return self.nc.dram_tensor(name, list(shape), dt, kind=kind).ap()

    def sb(self, shape, dt, name=None):
        self._uid += 1
        h = self.es.enter_context(self.nc.sbuf_tensor(name or ("t%d" % self._uid), list(shape), dt))
        return T(h)

    def ps(self, shape, dt=F32, name=None):
        self._uid += 1
        h = self.es.enter_context(self.nc.psum_tensor(name or ("p%d" % self._uid), list(shape), dt))
        return T(h, atomic=True)

    def _wait(self, e, tok):
        if tok is None:
            return
        sem, val, src = tok
        if src == e and (e == "pe" or not SAME_ENGINE_SYNC):
            return
        if src == e and e == "sp":
            pass
        w = self.waited[e]
        if w.get(sem.name, 0) >= val:
            return
        self.eng[e].wait_ge(sem, val)
        w[sem.name] = val

    def _deps(self, e, reads, writes):
        for t in reads:
            for x in t._related():
                self._wait(e, x.w)
        for t in writes:
            for x in t._related():
                self._wait(e, x.w)
                for tok in x.r:
                    self._wait(e, tok)

    def _commit(self, tok, reads, writes):
        for t in reads:
            t.r.append(tok)
            if len(t.r) > 24:
                t.r = t.r[-24:] if False else self._compact(t.r)
        for t in writes:
            t.w = tok
            t.r = []
            stack = list(t.kids.values())
            while stack:
                k = stack.pop()
                k.w = None
                k.r = []
                stack.extend(k.kids.values())

    @staticmethod
    def _compact(toks):
        best = {}
        for sem, val, src in toks:
            k = sem.name
            if k not in best or best[k][1] < val:
                best[k] = (sem, val, src)
        return list(best.values())

    def op(self, e, fn, reads=(), writes=()):
        self._deps(e, reads, writes)
        ins = fn(self.eng[e])
        self.cnt[e] += 1
        ins.then_inc(self.sem[e], 1)
        tok = (self.sem[e], self.cnt[e], e)
        self._commit(tok, reads, writes)
        self.n_inst += 1
        return tok

    def dma(self, q, out, in_, reads=(), writes=(), is_output=False, **kw):
        i = self.dcnt[q]
        k = i % NDMA_SEM
        sem = self.dsem[q][k]
        prev = 16 * (i // NDMA_SEM)
        if prev > 0:
            w = self.waited[q]
            if w.get(sem.name, 0) < prev:
                self.eng[q].wait_ge(sem, prev)
                w[sem.name] = prev
        self._deps(q, reads, writes)
        ins = self.eng[q].dma_start(out=out, in_=in_, **kw)
        ins.then_inc(sem, 16)
        self.dcnt[q] += 1
        tok = (sem, prev + 16, "dma_" + q)
        self._commit(tok, reads, writes)
        if is_output:
            self.out_tokens.append(tok)
        self.n_inst += 1
        return tok

    def finish(self):
        for tok in self.out_tokens:
            self._wait("sp", tok)
        for e in ("pe", "act", "dve", "pool"):
            if self.cnt[e] > 0:
                self._wait("sp", (self.sem[e], self.cnt[e], e))
        for q in self.dsem:
            i = self.dcnt[q]
            for k in range(NDMA_SEM):
                n = (i - k + NDMA_SEM - 1) // NDMA_SEM if i > k else 0
                if n > 0:
                    self._wait("sp", (self.dsem[q][k], 16 * n, "dma_" + q))
        self.es.close()
        return self.nc


def run(nc, in_maps, n=8):
    res = run_bass_kernel_spmd(nc, in_maps, core_ids=list(range(n)))
    return res.results


D = 2048
NCH = D // 128
EPS = 1e-6


def emit_identity(kb, dt=BF16):
    ident = kb.sb([128, 128], dt, "ident")
    kb.op("pool", lambda e: e.memset(ident[:], 1.0), writes=[ident])
    kb.op("pool", lambda e: e.affine_select(out=ident[:], in_=ident[:], pattern=[[-1, 128]],
                                             compare_op=ALU.is_equal, fill=0.0, base=0,
                                             channel_multiplier=1), reads=[ident], writes=[ident])
    return ident


def bf16_view(pt):
    ap = pt[:]
    if ap.dtype == BF16:
        return ap
    return ap.bitcast(BF16)


class NormT:
    def __init__(self, kb, ident, nbuf=2, pts=None):
        self.kb = kb
        self.ident = ident
        self.xt = [kb.sb([128, D], F32, "nx%d" % i) for i in range(nbuf)]
        self.sq = kb.sb([128, D], BF16, "nsq")
        self.xn = [kb.sb([128, D], BF16, "nxn%d" % i) for i in range(2)]
        self.ss = [kb.sb([128, 1], F32, "nss%d" % i) for i in range(2)]
        self.rs = [kb.sb([128, 1], F32, "nrs%d" % i) for i in range(2)]
        self.pt = pts if pts is not None else [kb.ps([128, 512], BF16, "npt%d" % i) for i in range(2)]
        self.i = 0
        self.nbuf = nbuf

    def load(self, src_ap, q="sp"):
        kb = self.kb
        b = self.i % self.nbuf
        xt = self.xt[b]
        kb.dma(q, xt[:], src_ap, writes=[xt])
        return xt

    def run(self, src_ap, dst, dst_sl, q="sp", xt=None, gB=None):
        kb = self.kb
        if xt is None:
            xt = self.load(src_ap, q)
        k = self.i % 2
        self.i += 1
        ss, rs, xn = self.ss[k], self.rs[k], self.xn[k]
        kb.op("act", lambda e: e.activation(out=self.sq[:], in_=xt[:], func=AF.Square, accum_out=ss[:]),
              reads=[xt], writes=[self.sq, ss])
        kb.op("act", lambda e: e.activation(out=rs[:], in_=ss[:], func=AF.Sqrt, bias=EPS, scale=1.0 / D),
              reads=[ss], writes=[rs])
        kb.op("dve", lambda e: e.reciprocal(out=rs[:], in_=rs[:]), reads=[rs], writes=[rs])
        kb.op("dve", lambda e: e.tensor_scalar_mul(out=xn[:], in0=xt[:], scalar1=rs[:, 0:1]),
              reads=[xt, rs], writes=[xn])
        for g in range(4):
            pt = self.pt[g % 2]
            ptv = bf16_view(pt)
            for j in range(4):
                c = g * 4 + j
                kb.op("pe", lambda e: e.transpose(out=ptv[:, j * 128:(j + 1) * 128], in_=xn[:, c * 128:(c + 1) * 128],
                                                  identity=self.ident[:]),
                      reads=[xn, self.ident], writes=[pt.sub(j)])
            eng = "act" if g % 2 == 0 else "dve"
            o = dst[:, g * 4:(g + 1) * 4, dst_sl]
            i_ = ptv[:, 0:512].rearrange("p (a b) -> p a b", a=4)
            if gB is not None:
                kb.op("dve", lambda e: e.tensor_tensor(out=o, in0=i_, in1=gB[:, g * 4:(g + 1) * 4, :], op=ALU.mult),
                      reads=[pt, gB], writes=[dst.sub(("n", g, dst_sl.start))])
            elif eng == "act":
                kb.op("act", lambda e: e.copy(out=o, in_=i_), reads=[pt], writes=[dst.sub(("n", g, dst_sl.start))])
            else:
                kb.op("dve", lambda e: e.tensor_copy(out=o, in_=i_), reads=[pt], writes=[dst.sub(("n", g, dst_sl.start))])
        return xt


_eps_cache = {}


def EPS_AP(kb):
    if id(kb) not in _eps_cache:
        t = kb.sb([128, 1], F32, "epsc")
        kb.op("pool", lambda e: e.memset(t[:], EPS), writes=[t])
        _eps_cache[id(kb)] = t
    t = _eps_cache[id(kb)]
    return t[:, 0:1]


def load_weight_bf16(kb, w_ap, rows, cols, name, gain=None, stage=None, q="sp", col_off=0):
    rc = rows // 128
    wb = kb.sb([128, rc, cols], BF16, name)
    if stage is None:
        stage = [kb.sb([128, 512], F32, name + "_st%d" % i) for i in range(3)]
    n = 0
    for c in range(rc):
        for j0 in range(0, cols, 512):
            jw = min(512, cols - j0)
            st = stage[n % len(stage)]
            kb.dma(q if n % 2 == 0 else "pool", st[:, :jw], w_ap[c * 128:(c + 1) * 128, col_off + j0:col_off + j0 + jw], writes=[st])
            if gain is not None:
                kb.op("dve", lambda e: e.tensor_scalar_mul(out=wb[:, c, j0:j0 + jw], in0=st[:, :jw], scalar1=gain[:, c:c + 1]),
                      reads=[st, gain], writes=[wb.sub((c, j0))])
            else:
                if n % 2 == 0:
                    kb.op("dve", lambda e: e.tensor_copy(out=wb[:, c, j0:j0 + jw], in_=st[:, :jw]), reads=[st], writes=[wb.sub((c, j0))])
                else:
                    kb.op("act", lambda e: e.copy(out=wb[:, c, j0:j0 + jw], in_=st[:, :jw]), reads=[st], writes=[wb.sub((c, j0))])
            n += 1
    return wb, stage


def load_gain(kb, g_ap, n, name, q="sp"):
    t = kb.sb([128, n // 128], F32, name)
    kb.dma(q, t[:], g_ap.rearrange("(c p) -> p c", p=128), writes=[t], allow_slow_non_contiguous=True)
    return t


def build_p1(T):
    kb = KB()
    NT = T // 128
    x = kb.dram("x", [T + 128, D], F32, "ExternalInput")
    g = kb.dram("g", [D], F32, "ExternalInput")
    w = kb.dram("w", [D, 4096], F32, "ExternalInput")
    pw = kb.dram("pw", [4, 256, 256], F32, "ExternalInput")
    psc = kb.dram("psc", [1024], F32, "ExternalInput")
    fm = kb.dram("fm", [3, 4, 128, 128], F32, "ExternalInput")
    qkT = kb.dram("qkT", [2048, T], BF16, "ExternalOutput")
    v = kb.dram("v", [T, 1024], BF16, "ExternalOutput")
    obT = kb.dram("obT", [1024, T], BF16, "ExternalOutput")

    ident = emit_identity(kb)
    gain = load_gain(kb, g, D, "gain")
    nt = NormT(kb, ident)
    wb = kb.sb([128, NCH, 2048], BF16, "wb")
    stage = None

    def load_w(col_off):
        nonlocal stage
        n = 0
        if stage is None:
            stage = [kb.sb([128, 512], F32, "wst%d" % i) for i in range(3)]
        for c in range(NCH):
            for j0 in range(0, 2048, 512):
                st = stage[n % 3]
                kb.dma("sp" if n % 2 == 0 else "pool", st[:], w[c * 128:(c + 1) * 128, col_off + j0:col_off + j0 + 512], writes=[st])
                sc = 128 ** -0.5 if (col_off == 0 and j0 < 1024) else 1.0
                kb.op("dve", lambda e: e.tensor_scalar(out=wb[:, c, j0:j0 + 512], in0=st[:], scalar1=gain[:, c:c + 1], scalar2=float(sc),
                                                       op0=ALU.mult, op1=ALU.mult),
                      reads=[st, gain], writes=[wb.sub((c, j0))])
                n += 1

    load_w(0)
    hnT = [kb.sb([128, NCH, 512], BF16, "hnT%d" % i) for i in range(2)]
    pA = [kb.ps([128, 512], F32, "pA%d" % i) for i in range(2)]
    oA = [kb.sb([128, 512], BF16, "oA%d" % i) for i in range(3)]
    n_o = 0
    for gi in range(T // 512):
        h = hnT[gi % 2]
        for j in range(4):
            r0 = 128 + gi * 512 + j * 128
            nt.run(x[r0:r0 + 128, :], h, slice(j * 128, (j + 1) * 128))
        for cb in range(16):
            p = pA[cb % 2]
            for c in range(NCH):
                kb.op("pe", lambda e: e.matmul(p[:], lhsT=wb[:, c, cb * 128:(cb + 1) * 128], rhs=h[:, c, :],
                                               start=(c == 0), stop=(c == NCH - 1)),
                      reads=[wb, h], writes=[p])
            o = oA[n_o % 3]
            n_o += 1
            if cb % 2 == 0:
                kb.op("act", lambda e: e.copy(out=o[:], in_=p[:]), reads=[p], writes=[o])
            else:
                kb.op("dve", lambda e: e.tensor_copy(out=o[:], in_=p[:]), reads=[p], writes=[o])
            kb.dma("sp", qkT[cb * 128:(cb + 1) * 128, gi * 512:(gi + 1) * 512], o[:], reads=[o], is_output=True)

    load_w(2048)
    fmt = kb.sb([128, 12, 128], F32, "fmt")
    kb.dma("sp", fmt[:], fm.rearrange("a g s t -> s (a g) t"), writes=[fmt], allow_slow_non_contiguous=True)
    pwb, _ = load_weight_bf16(kb, pw.rearrange("g c d -> (g c) d"), 1024, 256, "pwb", stage=stage)
    pscale = load_gain(kb, psc, 1024, "pscale")
    hB = [kb.sb([128, NCH, 128], BF16, "hB%d" % i) for i in range(2)]
    pV = [kb.ps([128, 512], F32, "pV%d" % i) for i in range(2)]
    vo = [kb.sb([128, 1024], BF16, "vo%d" % i) for i in range(2)]
    ut = [kb.sb([128, 1024], F32, "ut%d" % i) for i in range(2)]
    pD = [kb.ps([128, 512], F32, "pD%d" % i) for i in range(2)]
    dT = [kb.sb([128, 8, 128], BF16, "dT%d" % i) for i in range(2)]
    ob = [kb.sb([128, 8, 512], BF16, "ob%d" % i) for i in range(2)]
    for ti in range(-1, NT):
        r0 = 128 + ti * 128
        k = (ti + 1) % 2
        h = hB[k]
        nt.run(x[r0:r0 + 128, :], h, slice(0, 128))
        u_cur, u_prev = ut[k], ut[1 - k]
        for half in range(4):
            if ti < 0 and half < 2:
                continue
            p = pV[half % 2]
            for c in range(NCH):
                kb.op("pe", lambda e: e.matmul(p[:], lhsT=h[:, c, :], rhs=wb[:, c, half * 512:(half + 1) * 512],
                                               start=(c == 0), stop=(c == NCH - 1)), reads=[h, wb], writes=[p])
            if half < 2:
                dst = vo[ti % 2]
                kb.op("act", lambda e: e.copy(out=dst[:, half * 512:(half + 1) * 512], in_=p[:]), reads=[p], writes=[dst.sub(half)])
            else:
                hh = half - 2
                kb.op("dve", lambda e: e.tensor_copy(out=u_cur[:, hh * 512:(hh + 1) * 512], in_=p[:]), reads=[p], writes=[u_cur.sub(hh)])
        if ti < 0:
            continue
        kb.dma("pool", v[ti * 128:(ti + 1) * 128, :], vo[ti % 2][:], reads=[vo[ti % 2]], is_output=True)
        d = dT[ti % 2]
        for half in range(2):
            p = pD[half]
            for j in range(4):
                cc = half * 4 + j
                gq = cc // 2
                fcur = fmt[:, (0 if ti == 0 else 4) + gq, :]
                fprev = fmt[:, 8 + gq, :]
                kb.op("pe", lambda e: e.matmul(p[:, j * 128:(j + 1) * 128], lhsT=u_cur[:, cc * 128:(cc + 1) * 128], rhs=fcur,
                                               start=True, stop=False), reads=[u_cur, fmt], writes=[p.sub(j)])
                kb.op("pe", lambda e: e.matmul(p[:, j * 128:(j + 1) * 128], lhsT=u_prev[:, cc * 128:(cc + 1) * 128], rhs=fprev,
                                               start=False, stop=True), reads=[u_prev, fmt], writes=[p.sub(j)])
            kb.op("act" if half == 0 else "dve",
                  (lambda e: e.copy(out=d[:, half * 4:(half + 1) * 4, :], in_=p[:].rearrange("p (a b) -> p a b", a=4))) if half == 0 else
                  (lambda e: e.tensor_copy(out=d[:, half * 4:(half + 1) * 4, :], in_=p[:].rearrange("p (a b) -> p a b", a=4))),
                  reads=[p], writes=[d.sub(half)])
        o = ob[(ti // 4) % 2]
        tj = ti % 4
        for half in range(2):
            p = pD[half]
            for j in range(4):
                oc = half * 4 + j
                gq, dh = oc // 2, oc % 2
                for cc in range(2):
                    kb.op("pe", lambda e: e.matmul(p[:, j * 128:(j + 1) * 128], lhsT=pwb[:, gq * 2 + cc, dh * 128:(dh + 1) * 128],
                                                   rhs=d[:, gq * 2 + cc, :], start=(cc == 0), stop=(cc == 1)),
                          reads=[pwb, d], writes=[p.sub(j)])
                kb.op("dve", lambda e: e.tensor_scalar_mul(out=o[:, oc, tj * 128:(tj + 1) * 128], in0=p[:, j * 128:(j + 1) * 128],
                                                           scalar1=pscale[:, oc:oc + 1]),
                      reads=[p.sub(j), pscale], writes=[o.sub((oc, tj))])
        if tj == 3:
            t0 = (ti // 4) * 512
            kb.dma("pool", obT[:, t0:t0 + 512].rearrange("(c p) t -> p c t", p=128), o[:], reads=[o], is_output=True)
    return kb.finish()


def build_p2(S, NU=2):
    kb = KB()
    NG = S // 512
    NB = S // 128
    qT = kb.dram("qT", [NU, 128, S], BF16, "ExternalInput")
    kT = kb.dram("kT", [NU, 128, S], BF16, "ExternalInput")
    v = kb.dram("v", [NU, S, 128], BF16, "ExternalInput")
    oT = kb.dram("oT", [NU, 128, S], BF16, "ExternalOutput")

    ntri = kb.sb([128, 128], F32, "ntri")
    kb.op("pool", lambda e: e.memset(ntri[:], -1.0), writes=[ntri])
    kb.op("pool", lambda e: e.affine_select(out=ntri[:], in_=ntri[:], pattern=[[-1, 128]], compare_op=ALU.is_ge,
                                             fill=0.0, base=0, channel_multiplier=1), reads=[ntri], writes=[ntri])
    nones = kb.sb([128, 128], F32, "nones")
    kb.op("pool", lambda e: e.memset(nones[:], -1.0), writes=[nones])

    qs = kb.sb([128, S], BF16, "qs")
    ks = kb.sb([128, S], BF16, "ks")
    vs = kb.sb([128, NB, 128], BF16, "vs")
    pz = [kb.ps([128, 512], F32, "pz%d" % i) for i in range(2)]
    px = [kb.ps([128, 512], F32, "px%d" % i) for i in range(2)]
    po = [kb.ps([128, 512], F32, "po%d" % i) for i in range(2)]
    et = [kb.sb([128, 512], F32, "et%d" % i) for i in range(2)]
    spt = [kb.sb([128, 512], F32, "spt%d" % i) for i in range(3)]
    wt = [kb.sb([128, 512], BF16, "wt%d" % i) for i in range(3)]
    srun = kb.sb([128, 512], F32, "srun")
    ot = [kb.sb([128, 512], BF16, "ot%d" % i) for i in range(2)]
    n = 0
    for u in range(NU):
        nq = 4 if S >= 2048 else 1
        for i in range(nq):
            sl = slice(i * S // nq, (i + 1) * S // nq)
            kb.dma("sp", qs[:, sl], qT[u, :, sl], writes=[qs.sub(i)] if u == 0 else [qs])
            kb.dma("pool", ks[:, sl], kT[u, :, sl], writes=[ks.sub(i)] if u == 0 else [ks])
        nv = max(1, NB // 16)
        for i in range(nv):
            j0, j1 = i * NB // nv, (i + 1) * NB // nv
            kb.dma("sp" if i % 2 == 0 else "pool", vs[:, j0:j1, :], v[u, j0 * 128:j1 * 128, :].rearrange("(j p) d -> p j d", p=128),
                   writes=[vs.sub(i)] if u == 0 else [vs])
        pairs = []
        for G in range(NG):
            jl = list(range(4 * G + 3, -1, -1))
            for idx, j in enumerate(jl):
                pairs.append((G, j, idx == 0, idx == len(jl) - 1))
        st1 = {}

        def stage1(pi):
            G, j, first, last = pairs[pi]
            n = n0 + pi
            qg = qs[:, G * 512:(G + 1) * 512]
            z, e_t, sp_t = pz[n % 2], et[n % 2], spt[n % 3]
            kblk = ks[:, j * 128:(j + 1) * 128]
            kb.op("pe", lambda e: e.matmul(z[:], lhsT=kblk, rhs=qg, start=True, stop=True), reads=[qs, ks], writes=[z])
            kb.op("act", lambda e: e.activation(out=e_t[:], in_=z[:], func=AF.Exp), reads=[z], writes=[e_t])
            kb.op("act", lambda e: e.activation(out=sp_t[:], in_=e_t[:], func=AF.Ln, bias=1.0), reads=[e_t], writes=[sp_t])
            if j >= 4 * G:
                kb.op("pool", lambda e: e.affine_select(out=sp_t[:], in_=sp_t[:], pattern=[[1, 512]], compare_op=ALU.is_gt,
                                                         fill=0.0, base=512 * G - 128 * j, channel_multiplier=-1),
                      reads=[sp_t], writes=[sp_t])

        def stage2(pi):
            G, j, first, last = pairs[pi]
            n = n0 + pi
            qg = qs[:, G * 512:(G + 1) * 512]
            oacc = po[G % 2]
            xx, sp_t, w_t = px[n % 2], spt[n % 3], wt[n % 3]
            kblk = ks[:, j * 128:(j + 1) * 128]
            kb.op("pe", lambda e: e.matmul(xx[:], lhsT=kblk, rhs=qg, start=True, stop=False), reads=[qs, ks], writes=[xx])
            kb.op("pe", lambda e: e.matmul(xx[:], lhsT=ntri[:], rhs=sp_t[:], start=False, stop=first), reads=[ntri, sp_t], writes=[xx])
            if not first:
                kb.op("pe", lambda e: e.matmul(xx[:], lhsT=nones[:], rhs=srun[:], start=False, stop=True), reads=[nones, srun], writes=[xx])
            kb.op("act", lambda e: e.activation(out=w_t[:], in_=xx[:], func=AF.Exp), reads=[xx], writes=[w_t])
            if j >= 4 * G:
                kb.op("pool", lambda e: e.affine_select(out=w_t[:], in_=w_t[:], pattern=[[1, 512]], compare_op=ALU.is_gt,
                                                         fill=0.0, base=512 * G - 128 * j, channel_multiplier=-1),
                      reads=[w_t], writes=[w_t])
            if not last:
                if first:
                    kb.op("dve", lambda e: e.tensor_copy(out=srun[:], in_=sp_t[:]), reads=[sp_t], writes=[srun])
                else:
                    kb.op("dve", lambda e: e.tensor_add(out=srun[:], in0=srun[:], in1=sp_t[:]), reads=[sp_t, srun], writes=[srun])
            kb.op("pe", lambda e: e.matmul(oacc[:], lhsT=vs[:, j, :], rhs=w_t[:], start=first, stop=last), reads=[vs, w_t], writes=[oacc])
            if last:
                o_t = ot[G % 2]
                kb.op("dve", lambda e: e.tensor_copy(out=o_t[:], in_=oacc[:]), reads=[oacc], writes=[o_t])
                kb.dma("sp", oT[u, :, G * 512:(G + 1) * 512], o_t[:], reads=[o_t], is_output=True)

        n0 = n
        stage1(0)
        for pi in range(len(pairs)):
            if pi + 1 < len(pairs):
                stage1(pi + 1)
            stage2(pi)
        n += len(pairs)
    return kb.finish()


def load_w_scaled(kb, w_ap, rows, cols, name, gain, scale, stage, dst=None, col_off=0):
    rc = rows // 128
    wb = dst if dst is not None else kb.sb([128, rc, cols], BF16, name)
    n = 0
    for c in range(rc):
        for j0 in range(0, cols, 512):
            jw = min(512, cols - j0)
            st = stage[n % len(stage)]
            kb.dma("sp" if n % 2 == 0 else "pool", st[:, :jw], w_ap[c * 128:(c + 1) * 128, col_off + j0:col_off + j0 + jw], writes=[st])
            kb.op("dve", lambda e: e.tensor_scalar(out=wb[:, c, j0:j0 + jw], in0=st[:, :jw], scalar1=gain[:, c:c + 1], scalar2=float(scale),
                                                   op0=ALU.mult, op1=ALU.mult), reads=[st, gain], writes=[wb.sub((c, j0))])
            n += 1
    return wb


def build_p3a(T):
    kb = KB()
    NT = T // 128
    oT = kb.dram("oT", [2048, T], BF16, "ExternalInput")
    hin = kb.dram("hin", [T, D], F32, "ExternalInput")
    wout = kb.dram("wout", [D, D], F32, "ExternalInput")
    gx = kb.dram("gx", [D], F32, "ExternalInput")
    mem = kb.dram("mem", [256, D], F32, "ExternalInput")
    gm = kb.dram("gm", [D], F32, "ExternalInput")
    wq = kb.dram("wq", [D, 512], F32, "ExternalInput")
    wkv = kb.dram("wkv", [D, 1024], F32, "ExternalInput")
    wo = kb.dram("wo", [512, D], F32, "ExternalInput")
    hout = kb.dram("hout", [T, D], F32, "ExternalOutput")

    ident = emit_identity(kb)
    ones_b = kb.sb([128, 128], BF16, "ones_b")
    kb.op("pool", lambda e: e.memset(ones_b[:], 1.0), writes=[ones_b])
    nt = NormT(kb, ident, nbuf=1)
    wout_b, stage = load_weight_bf16(kb, wout, D, D, "wout_b")
    gxg = load_gain(kb, gx, D, "gxg")
    gmg = load_gain(kb, gm, D, "gmg")
    wq_b = load_w_scaled(kb, wq, D, 512, "wq_b", gxg, 128 ** -0.5, stage)
    wo_b, _ = load_weight_bf16(kb, wo, 512, D, "wo_b", stage=stage)
    wkv_b = load_w_scaled(kb, wkv, D, 512, "wkv_b", gmg, 1.0, stage)

    pmm = [kb.ps([128, 512], F32, "pmm%d" % i) for i in range(2)]
    pq = kb.ps([128, 512], F32, "pq")
    pl = [kb.ps([128, 512], F32, "pl%d" % i) for i in range(2)]
    po = kb.ps([128, 512], F32, "po")

    memT = kb.sb([128, NCH, 256], BF16, "memT")
    for mt in range(2):
        nt.run(mem[mt * 128:(mt + 1) * 128, :], memT, slice(mt * 128, (mt + 1) * 128))
    KT = kb.sb([128, 4, 256], BF16, "KT")
    Vs = kb.sb([128, 2, 512], BF16, "Vs")
    for hd in range(4):
        p = pmm[hd % 2]
        for c in range(NCH):
            kb.op("pe", lambda e: e.matmul(p[:, 0:256], lhsT=wkv_b[:, c, hd * 128:(hd + 1) * 128], rhs=memT[:, c, :],
                                           start=(c == 0), stop=(c == NCH - 1)), reads=[wkv_b, memT], writes=[p])
        kb.op("act", lambda e: e.copy(out=KT[:, hd, :], in_=p[:, 0:256]), reads=[p], writes=[KT.sub(hd)])
    load_w_scaled(kb, wkv, D, 512, "wkv_b", gmg, 1.0, stage, dst=wkv_b, col_off=512)
    for mc in range(2):
        p = pmm[mc % 2]
        for c in range(NCH):
            kb.op("pe", lambda e: e.matmul(p[:], lhsT=memT[:, c, mc * 128:(mc + 1) * 128], rhs=wkv_b[:, c, 0:512],
                                           start=(c == 0), stop=(c == NCH - 1)), reads=[wkv_b, memT], writes=[p])
        kb.op("act", lambda e: e.copy(out=Vs[:, mc, :], in_=p[:]), reads=[p], writes=[Vs.sub(mc)])

    oTs = [kb.sb([128, NCH, 512], BF16, "oTs%d" % i) for i in range(1)]
    xin = [kb.sb([128, D], F32, "xin%d" % i) for i in range(1)]
    ht = [kb.sb([128, D], F32, "ht%d" % i) for i in range(2)]
    hnT = [kb.sb([128, NCH, 128], BF16, "hnT%d" % i) for i in range(1)]
    qxT = kb.sb([128, 4, 128], BF16, "qxT")
    pT = kb.sb([128, 8, 128], BF16, "pT")
    rden = kb.sb([128, 512], F32, "rden")
    oxT = kb.sb([128, 4, 128], BF16, "oxT")
    for ti in range(NT):
        gi, tj = ti // 4, ti % 4
        og = oTs[0]
        if tj == 0:
            kb.dma("pool", og[:], oT[:, gi * 512:(gi + 1) * 512].rearrange("(c p) t -> p c t", p=128), writes=[og])
        xt = xin[0]
        kb.dma("sp", xt[:], hin[ti * 128:(ti + 1) * 128, :], writes=[xt])
        h = ht[ti % 2]
        for cg in range(4):
            p = pmm[cg % 2]
            for c in range(NCH):
                kb.op("pe", lambda e: e.matmul(p[:], lhsT=og[:, c, tj * 128:(tj + 1) * 128], rhs=wout_b[:, c, cg * 512:(cg + 1) * 512],
                                               start=(c == 0), stop=(c == NCH - 1)), reads=[og, wout_b], writes=[p])
            kb.op("dve", lambda e: e.tensor_tensor(out=h[:, cg * 512:(cg + 1) * 512], in0=p[:], in1=xt[:, cg * 512:(cg + 1) * 512], op=ALU.add),
                  reads=[p, xt], writes=[h.sub(cg)])
        hn = hnT[0]
        nt.run(None, hn, slice(0, 128), xt=h)
        for hd in range(4):
            for c in range(NCH):
                kb.op("pe", lambda e: e.matmul(pq[:, hd * 128:(hd + 1) * 128], lhsT=wq_b[:, c, hd * 128:(hd + 1) * 128], rhs=hn[:, c, :],
                                               start=(c == 0), stop=(c == NCH - 1)), reads=[wq_b, hn], writes=[pq])
        kb.op("act", lambda e: e.copy(out=qxT[:], in_=pq[:].rearrange("p (a b) -> p a b", a=4)), reads=[pq], writes=[qxT])
        for hd in range(4):
            for mc in range(2):
                b = hd * 2 + mc
                kb.op("pe", lambda e: e.matmul(pl[b // 4][:, (b % 4) * 128:(b % 4 + 1) * 128], lhsT=KT[:, hd, mc * 128:(mc + 1) * 128],
                                               rhs=qxT[:, hd, :], start=True, stop=True), reads=[KT, qxT], writes=[pl[b // 4]])
        for k2 in range(2):
            kb.op("act", lambda e: e.activation(out=pT[:, k2 * 4:(k2 + 1) * 4, :], in_=pl[k2][:].rearrange("p (a b) -> p a b", a=4), func=AF.Exp),
                  reads=[pl[k2]], writes=[pT.sub(k2)])
        for hd in range(4):
            for mc in range(2):
                kb.op("pe", lambda e: e.matmul(po[:, hd * 128:(hd + 1) * 128], lhsT=Vs[:, mc, hd * 128:(hd + 1) * 128], rhs=pT[:, hd * 2 + mc, :],
                                               start=(mc == 0), stop=(mc == 1)), reads=[Vs, pT], writes=[po])
        for hd in range(4):
            for mc in range(2):
                kb.op("pe", lambda e: e.matmul(pq[:, hd * 128:(hd + 1) * 128], lhsT=ones_b[:], rhs=pT[:, hd * 2 + mc, :],
                                               start=(mc == 0), stop=(mc == 1)), reads=[ones_b, pT], writes=[pq])
        kb.op("dve", lambda e: e.reciprocal(out=rden[:], in_=pq[:]), reads=[pq], writes=[rden])
        kb.op("dve", lambda e: e.tensor_tensor(out=oxT[:], in0=po[:].rearrange("p (a b) -> p a b", a=4),
                                               in1=rden[:].rearrange("p (a b) -> p a b", a=4), op=ALU.mult), reads=[po, rden], writes=[oxT])
        for cg in range(4):
            p = pmm[cg % 2]
            for hd in range(4):
                kb.op("pe", lambda e: e.matmul(p[:], lhsT=oxT[:, hd, :], rhs=wo_b[:, hd, cg * 512:(cg + 1) * 512],
                                               start=(hd == 0), stop=(hd == 3)), reads=[oxT, wo_b], writes=[p])
            kb.op("dve", lambda e: e.tensor_tensor(out=h[:, cg * 512:(cg + 1) * 512], in0=p[:], in1=h[:, cg * 512:(cg + 1) * 512], op=ALU.add),
                  reads=[p, h.sub(cg)], writes=[h.sub(cg)])
        kb.dma("pool", hout[ti * 128:(ti + 1) * 128, :], h[:], reads=[h], is_output=True)
    return kb.finish()


def build_cast(N, CH=4096):
    kb = KB()
    a = kb.dram("a", [128, N], F32, "ExternalInput")
    o = kb.dram("o", [128, N], BF16, "ExternalOutput")
    st = [kb.sb([128, CH], F32, "cs%d" % i) for i in range(3)]
    ob = [kb.sb([128, CH], BF16, "co%d" % i) for i in range(3)]
    for i in range(N // CH):
        s_, o_ = st[i % 3], ob[i % 3]
        kb.dma("sp", s_[:], a[:, i * CH:(i + 1) * CH], writes=[s_])
        if i % 2 == 0:
            kb.op("dve", lambda e: e.tensor_copy(out=o_[:], in_=s_[:]), reads=[s_], writes=[o_])
        else:
            kb.op("act", lambda e: e.copy(out=o_[:], in_=s_[:]), reads=[s_], writes=[o_])
        kb.dma("pool", o[:, i * CH:(i + 1) * CH], o_[:], reads=[o_], is_output=True)
    return kb.finish()


NEG = -1.0e30


def build_p3b(T, final=False, NE=16384):
    kb = KB()
    NT = T // 128
    NEG_ = NE // 512
    hin = kb.dram("hin", [T, D], F32, "ExternalInput")
    gf = kb.dram("gf", [D], F32, "ExternalInput")
    wqb = kb.dram("wqb", [4, 128, NCH, 512], BF16, "ExternalInput")
    skT = kb.dram("skT", [128, 16, 128], F32, "ExternalInput")
    uT = kb.dram("uT", [NE // 512, 128, NCH, 512], BF16, "ExternalInput")
    vv = kb.dram("vv", [NE // 512, 128, 4, D], BF16, "ExternalInput")
    gfin = kb.dram("gfin", [D], F32, "ExternalInput")
    hout = kb.dram("hout", [T, D], F32, "ExternalOutput")

    ident = emit_identity(kb)
    B = [kb.ps([128, 512], F32, "B%d" % i) for i in range(8)]
    nt = NormT(kb, ident, nbuf=1, pts=[B[4], B[5]])
    gfg = load_gain(kb, gf, D, "gfg")
    gB = kb.sb([128, NCH, 128], F32, "gB")
    for c in range(NCH):
        kb.op("dve", lambda e: e.tensor_copy(out=gB[:, c, :], in_=gfg[:, c:c + 1].to_broadcast([128, 128])), reads=[gfg], writes=[gB.sub(c)])
    skst = kb.sb([128, 16, 128], F32, "skst")
    kb.dma("sp", skst[:], skT, writes=[skst])
    skb = kb.sb([128, 16, 128], BF16, "skb")
    kb.op("dve", lambda e: e.tensor_copy(out=skb[:], in_=skst[:]), reads=[skst], writes=[skb])
    if final:
        gfb = kb.sb([128, D], F32, "gfb")
        kb.dma("sp", gfb[:], gfin.partition_broadcast(128), writes=[gfb])

    G = kb.sb([128, NE], BF16, "G")
    sub = skst
    sub2 = kb.sb([128, 16, 128], F32, "sub2")
    m8 = kb.sb([128, 16, 16], F32, "m8")
    cand = kb.sb([128, 256], F32, "cand")
    cand2 = kb.sb([128, 256], F32, "cand2")
    tv = kb.sb([128, 8, 16], F32, "tv")
    e16 = kb.sb([128, 16], F32, "e16")
    negm = kb.sb([128, 8], F32, "negm")
    Z = kb.sb([128, 8], F32, "Z")
    nb = kb.sb([128, 8], F32, "nb")
    St = [kb.sb([128, 16, 128], F32, "St%d" % i) for i in range(2)]
    Et = kb.sb([128, 2048], BF16, "Et")
    Mt = kb.sb([128, 2048], BF16, "Mt")
    hnT = kb.sb([128, NCH, 128], BF16, "hnT")
    qTs = kb.sb([128, 16, 128], BF16, "qTs")
    ug = [kb.sb([128, NCH, 512], BF16, "ug%d" % i) for i in range(2)]
    vg = [kb.sb([128, 4, D], BF16, "vg%d" % i) for i in range(2)]
    at = [kb.sb([128, 512], BF16, "at%d" % i) for i in range(2)]
    gat = [kb.sb([128, 512], BF16, "gat%d" % i) for i in range(2)]
    gaT = [kb.sb([128, 4, 128], BF16, "gaT%d" % i) for i in range(2)]
    ss = kb.sb([128, 1], F32, "fss")
    rs = kb.sb([128, 1], F32, "frs")
    nug = 0
    for ti in range(NT):
        xt = nt.run(hin[ti * 128:(ti + 1) * 128, :], hnT, slice(0, 128), gB=gB)
        for grp in range(4):
            w_ = ug[nug % 2]
            nug += 1
            kb.dma("sp", w_[:], wqb[grp], writes=[w_])
            pq = B[6 + grp % 2]
            for j in range(4):
                for c in range(NCH):
                    kb.op("pe", lambda e: e.matmul(pq[:, j * 128:(j + 1) * 128], lhsT=w_[:, c, j * 128:(j + 1) * 128], rhs=hnT[:, c, :],
                                                   start=(c == 0), stop=(c == NCH - 1)), reads=[w_, hnT], writes=[pq])
            kb.op("act", lambda e: e.copy(out=qTs[:, grp * 4:(grp + 1) * 4, :], in_=pq[:].rearrange("p (a b) -> p a b", a=4)),
                  reads=[pq], writes=[qTs.sub(grp)])
        for b4 in range(4):
            ps_ = B[b4]
            for j in range(4):
                hp = b4 * 4 + j
                kb.op("pe", lambda e: e.matmul(ps_[:, j * 128:(j + 1) * 128], lhsT=qTs[:, hp, :], rhs=skb[:, hp, :], start=True, stop=True),
                      reads=[qTs, skb], writes=[ps_])
            if b4 % 2 == 0:
                kb.op("act", lambda e: e.copy(out=sub[:, b4 * 4:(b4 + 1) * 4, :], in_=ps_[:].rearrange("p (a b) -> p a b", a=4)), reads=[ps_], writes=[sub.sub(b4)])
            else:
                kb.op("dve", lambda e: e.tensor_copy(out=sub[:, b4 * 4:(b4 + 1) * 4, :], in_=ps_[:].rearrange("p (a b) -> p a b", a=4)), reads=[ps_], writes=[sub.sub(b4)])
        for hp in range(16):
            kb.op("dve", lambda e: e.max(out=m8[:, hp, 0:8], in_=sub[:, hp, :]), reads=[sub], writes=[m8.sub(hp)])
            kb.op("dve", lambda e: e.match_replace(out=sub2[:, hp, :], in_to_replace=m8[:, hp, 0:8], in_values=sub[:, hp, :], imm_value=NEG),
                  reads=[sub, m8.sub(hp)], writes=[sub2.sub(hp)])
            kb.op("dve", lambda e: e.max(out=m8[:, hp, 8:16], in_=sub2[:, hp, :]), reads=[sub2.sub(hp)], writes=[m8.sub(hp)])
        for h in range(8):
            kb.op("dve", lambda e: e.tensor_tensor(out=cand[:].rearrange("p (a b) -> p a b", a=16),
                                                   in0=m8[:, 2 * h, :].unsqueeze(2).to_broadcast([128, 16, 16]),
                                                   in1=m8[:, 2 * h + 1, :].unsqueeze(1).to_broadcast([128, 16, 16]), op=ALU.add),
                  reads=[m8], writes=[cand])
            kb.op("dve", lambda e: e.max(out=tv[:, h, 0:8], in_=cand[:]), reads=[cand], writes=[tv.sub(h)])
            kb.op("dve", lambda e: e.match_replace(out=cand2[:], in_to_replace=tv[:, h, 0:8], in_values=cand[:], imm_value=NEG),
                  reads=[cand, tv.sub(h)], writes=[cand2])
            kb.op("dve", lambda e: e.max(out=tv[:, h, 8:16], in_=cand2[:]), reads=[cand2], writes=[tv.sub(h)])
        kb.op("dve", lambda e: e.tensor_scalar_mul(out=negm[:], in0=tv[:, :, 0], scalar1=-1.0), reads=[tv], writes=[negm])
        for h in range(8):
            kb.op("act", lambda e: e.activation(out=e16[:], in_=tv[:, h, :], func=AF.Exp, bias=negm[:, h:h + 1], accum_out=Z[:, h:h + 1]),
                  reads=[tv, negm], writes=[e16, Z.sub(h)])
        kb.op("act", lambda e: e.activation(out=Z[:], in_=Z[:], func=AF.Ln), reads=[Z], writes=[Z])
        kb.op("dve", lambda e: e.tensor_sub(out=nb[:], in0=negm[:], in1=Z[:]), reads=[negm, Z], writes=[nb])
        npc = 0
        for h in range(8):
            for pc in range(8):
                S_ = St[npc % 2]
                npc += 1
                kb.op("pool", lambda e: e.tensor_tensor(out=S_[:], in0=sub[:, 2 * h, pc * 16:(pc + 1) * 16].unsqueeze(2).to_broadcast([128, 16, 128]),
                                                        in1=sub[:, 2 * h + 1, :].unsqueeze(1).to_broadcast([128, 16, 128]), op=ALU.add),
                      reads=[sub], writes=[S_])
                Sf = S_[:].rearrange("p a b -> p (a b)")
                kb.op("act", lambda e: e.activation(out=Et[:], in_=Sf, func=AF.Exp, bias=nb[:, h:h + 1]), reads=[S_, nb], writes=[Et])
                Gp = G[:, pc * 2048:(pc + 1) * 2048]
                if h == 0:
                    kb.op("dve", lambda e: e.scalar_tensor_tensor(out=Gp, in0=Sf, scalar=tv[:, h, 15:16], in1=Et[:], op0=ALU.is_ge, op1=ALU.mult),
                          reads=[S_, tv, Et], writes=[G.sub(pc)])
                else:
                    kb.op("dve", lambda e: e.scalar_tensor_tensor(out=Mt[:], in0=Sf, scalar=tv[:, h, 15:16], in1=Et[:], op0=ALU.is_ge, op1=ALU.mult),
                          reads=[S_, tv, Et], writes=[Mt])
                    kb.op("dve", lambda e: e.tensor_tensor(out=Gp, in0=Gp, in1=Mt[:], op=ALU.add), reads=[Mt, G.sub(pc)], writes=[G.sub(pc)])
        for eg in range(NEG_):
            u_ = ug[nug % 2]
            nug += 1
            v_ = vg[eg % 2]
            kb.dma("sp", u_[:], uT[eg], writes=[u_])
            kb.dma("pool", v_[:], vv[eg], writes=[v_])
            pa = B[4 + eg % 2]
            for c in range(NCH):
                kb.op("pe", lambda e: e.matmul(pa[:], lhsT=hnT[:, c, :], rhs=u_[:, c, :], start=(c == 0), stop=(c == NCH - 1)),
                      reads=[hnT, u_], writes=[pa])
            a_ = at[eg % 2]
            g_ = gat[eg % 2]
            kb.op("act", lambda e: e.activation(out=a_[:], in_=pa[:], func=AF.Gelu), reads=[pa], writes=[a_])
            kb.op("dve", lambda e: e.tensor_tensor(out=g_[:], in0=a_[:], in1=G[:, eg * 512:(eg + 1) * 512], op=ALU.mult),
                  reads=[a_, G], writes=[g_])
            ptb = B[6 + eg % 2]
            ptv = bf16_view(ptb)
            for j in range(4):
                kb.op("pe", lambda e: e.transpose(out=ptv[:, j * 128:(j + 1) * 128], in_=g_[:, j * 128:(j + 1) * 128], identity=ident[:]),
                      reads=[g_, ident], writes=[ptb])
            gT = gaT[eg % 2]
            kb.op("act", lambda e: e.copy(out=gT[:], in_=ptv[:, 0:512].rearrange("p (a b) -> p a b", a=4)), reads=[ptb], writes=[gT])
            for cg in range(4):
                for j in range(4):
                    kb.op("pe", lambda e: e.matmul(B[cg][:], lhsT=gT[:, j, :], rhs=v_[:, j, cg * 512:(cg + 1) * 512],
                                                   start=(eg == 0 and j == 0), stop=(eg == NEG_ - 1 and j == 3)), reads=[gT, v_], writes=[B[cg]])
        for cg in range(4):
            kb.op("dve", lambda e: e.tensor_tensor(out=xt[:, cg * 512:(cg + 1) * 512], in0=B[cg][:], in1=xt[:, cg * 512:(cg + 1) * 512], op=ALU.add),
                  reads=[B[cg], xt], writes=[xt])
        if final:
            kb.op("act", lambda e: e.activation(out=nt.sq[:], in_=xt[:], func=AF.Square, accum_out=ss[:]), reads=[xt], writes=[nt.sq, ss])
            kb.op("act", lambda e: e.activation(out=rs[:], in_=ss[:], func=AF.Sqrt, bias=EPS, scale=1.0 / D), reads=[ss], writes=[rs])
            kb.op("dve", lambda e: e.reciprocal(out=rs[:], in_=rs[:]), reads=[rs], writes=[rs])
            kb.op("dve", lambda e: e.scalar_tensor_tensor(out=xt[:], in0=xt[:], scalar=rs[:, 0:1], in1=gfb[:], op0=ALU.mult, op1=ALU.mult),
                  reads=[xt, rs, gfb], writes=[xt])
        kb.dma("pool", hout[ti * 128:(ti + 1) * 128, :], xt[:], reads=[xt], is_output=True)
    return kb.finish()


import ml_dtypes
_BF = ml_dtypes.bfloat16
_progs = {}


def _prog(key, fn, *a, **k):
    if key not in _progs:
        _progs[key] = fn(*a, **k)
    return _progs[key]


def _pool_mats(first):
    fm = np.zeros((3, 4, 128, 128), np.float32)
    s = np.arange(128)[:, None]
    t = np.arange(128)[None, :]
    for gi, win in enumerate((2, 4, 8, 16)):
        inwin = (s > t - win) & (s <= t)
        cnt_first = np.minimum(win, t + 1).astype(np.float32)
        cur = inwin / np.float32(win) - (s == t)
        cur0 = inwin / cnt_first - (s == t)
        prev = ((s - 128) > (t - win)) / np.float32(win)
        fm[0, gi] = cur0 if first else cur
        fm[1, gi] = cur
        fm[2, gi] = prev
    return fm


def _layer_tail(h_chunks, oT_chunks, layer, P, S, final):
    T = S // 4
    j = layer
    p3a = _prog(("p3a", T), build_p3a, T)
    ims = []
    for c in range(8):
        b = c // 4
        ims.append(dict(oT=oT_chunks[c], hin=h_chunks[c], wout=P["w_out"], gx=P["norm_cross"][j], mem=P["mem"][b], gm=P["norm_mem"][j],
                        wq=P["xattn_wq"][j], wkv=P["xattn_wkv"][j], wo=P["xattn_wo"][j]))
    r = run(p3a, ims)
    h1 = [np.asarray(r[c]["hout"]) for c in range(8)]
    flat = np.concatenate([np.ascontiguousarray(P["peer_u"][j].T).ravel(), P["peer_v"][j].ravel(), P["peer_wq"][j].ravel()])
    NPC = flat.size // (8 * 128)
    pc = _prog(("cast", NPC), build_cast, NPC)
    fl = flat.reshape(8, 128, NPC)
    rc = run(pc, [dict(a=fl[c]) for c in range(8)])
    fb = np.concatenate([np.asarray(rc[c]["o"]).reshape(-1) for c in range(8)])
    n_u = 16384 * D
    uTb = np.ascontiguousarray(fb[:n_u].reshape(NCH, 128, 32, 512).transpose(2, 1, 0, 3))
    vb = np.ascontiguousarray(fb[n_u:2 * n_u].reshape(32, 4, 128, D).transpose(0, 2, 1, 3))
    wqb = np.ascontiguousarray(fb[2 * n_u:].reshape(NCH, 128, 4, 512).transpose(2, 1, 0, 3))
    skT = np.ascontiguousarray(P["peer_subkeys"][j].reshape(16, 128, 128).transpose(2, 0, 1))
    p3b = _prog(("p3b", T, final), build_p3b, T, final=final)
    ims = [dict(hin=h1[c], gf=P["norm_ffn"][j], wqb=wqb, skT=skT, uT=uTb, vv=vb, gfin=P["norm_final"]) for c in range(8)]
    r = run(p3b, ims)
    return [np.asarray(r[c]["hout"]) for c in range(8)]


def _layer0(xc, P, S):
    T = S // 4
    p1 = _prog(("p1", T), build_p1, T)
    ims = []
    for c in range(8):
        b, ch = c // 4, c % 4
        halo = np.zeros((128, D), np.float32) if ch == 0 else xc[c - 1][-128:]
        ims.append(dict(x=np.concatenate([halo, xc[c]], 0), g=P["norm_mix"][0], w=P["w_in_ab"][0], pw=P["pool_w"][0],
                        psc=P["pool_scale"][0], fm=_pool_mats(ch == 0)))
    r = run(p1, ims)
    qkT = [np.asarray(r[c]["qkT"]) for c in range(8)]
    vtm = [np.asarray(r[c]["v"]) for c in range(8)]
    obT = [np.asarray(r[c]["obT"]) for c in range(8)]
    p2 = _prog(("p2", S), build_p2, S, 2)
    ims = []
    for c in range(8):
        qs, ks, vs = [], [], []
        for u in range(2):
            b, h = divmod(2 * c + u, 8)
            qs.append(np.concatenate([qkT[b * 4 + ch][h * 128:(h + 1) * 128] for ch in range(4)], 1))
            ks.append(np.concatenate([qkT[b * 4 + ch][1024 + h * 128:1024 + (h + 1) * 128] for ch in range(4)], 1))
            vs.append(np.concatenate([vtm[b * 4 + ch][:, h * 128:(h + 1) * 128] for ch in range(4)], 0))
        ims.append(dict(qT=np.stack(qs), kT=np.stack(ks), v=np.stack(vs)))
    r = run(p2, ims)
    oa = {}
    for c in range(8):
        o = np.asarray(r[c]["oT"])
        for u in range(2):
            oa[divmod(2 * c + u, 8)] = o[u]
    oT_chunks = []
    for c in range(8):
        b, ch = c // 4, c % 4
        oaT = np.concatenate([oa[(b, h)][:, ch * T:(ch + 1) * T] for h in range(8)], 0)
        oT_chunks.append(np.concatenate([oaT, obT[c]], 0))
    P0 = dict(P)
    P0["w_out"] = P["w_out_ab"][0]
    return _layer_tail(xc, oT_chunks, 0, P0, S, final=False)


def _layer1(hc, P, S):
    T = S // 4
    NR = S // 512
    p4 = _prog(("p4", T), build_p4, T)
    ims = []
    for c in range(8):
        ch = c % 4
        halo = np.zeros((128, D), np.float32) if ch == 0 else hc[c - 1][-128:]
        ims.append(dict(x=np.concatenate([halo, hc[c]], 0), g=P["norm_mix"][1], w=P["w_in_cd"][0], wuq=P["w_uq"][0], wiq=P["w_iq"][0],
                        ncq=P["norm_cq"][0], nki=P["norm_kidx"][0], cw=P["conv_w"][0], alog=P["a_log"][0], dtb=P["dt_bias"][0]))
    r = run(p4, ims)
    cat = lambda key, b, axis: np.concatenate([np.asarray(r[b * 4 + ch][key]) for ch in range(4)], axis)
    p5 = _prog(("p5", S), build_p5, S)
    ims = []
    per_b = {}
    for b in range(2):
        per_b[b] = dict(kiT=cat("kiT", b, 1), kcT=cat("kcT", b, 1).reshape(8, 128, S), vc=cat("vc", b, 0),
                        qiT=cat("qiT", b, 1), wi=cat("wi", b, 0), qcT=cat("qcT", b, 1))
    for c in range(8):
        b, rr = c // 4, c % 4
        pb = per_b[b]
        tiles = [4 * m + rr for m in range(NR)]
        cm, cm01, BT, b31 = _dsa_consts(rr, P["rel_bias"])
        qiT = np.stack([pb["qiT"][:, i * 128:(i + 1) * 128].reshape(16, 64, 128).transpose(1, 0, 2) for i in tiles])
        wi = np.stack([pb["wi"][i * 128:(i + 1) * 128] for i in tiles])
        qc = np.stack([pb["qcT"][:, i * 128:(i + 1) * 128].reshape(8, 128, 128) for i in tiles])
        ims.append(dict(kiT=pb["kiT"], qiT=np.ascontiguousarray(qiT), wi=np.ascontiguousarray(wi), cm=cm, cm01=cm01, kcT=pb["kcT"], vc=pb["vc"],
                        qc=np.ascontiguousarray(qc), BT=BT, b31=b31))
    r5 = run(p5, ims)
    ocT = [np.zeros((1024, S), _BF) for _ in range(2)]
    for c in range(8):
        b, rr = c // 4, c % 4
        o = np.asarray(r5[c]["ocT"])
        for m in range(NR):
            i = 4 * m + rr
            ocT[b][:, i * 128:(i + 1) * 128] = o[m].reshape(1024, 128)
    p6 = _prog(("p6", S), build_p6, S, 2)
    qkv = {b: cat("qkvT", b, 1) for b in range(2)}
    gb = {b: cat("gb", b, 1) for b in range(2)}
    sz = {b: cat("szT", b, 1) for b in range(2)}
    ims = []
    for c in range(8):
        d = {k_: [] for k_ in ("qT", "kT", "vT", "g", "beta", "szT")}
        for u in range(2):
            b, h = divmod(2 * c + u, 8)
            d["qT"].append(qkv[b][h * 128:(h + 1) * 128])
            d["kT"].append(qkv[b][1024 + h * 128:1024 + (h + 1) * 128])
            d["vT"].append(qkv[b][2048 + h * 128:2048 + (h + 1) * 128])
            d["g"].append(gb[b][8 + h])
            d["beta"].append(gb[b][h])
            d["szT"].append(sz[b][h * 128:(h + 1) * 128])
        im = {k_: np.ascontiguousarray(np.stack(v_)) for k_, v_ in d.items()}
        im["gno"] = P["norm_delta_out"][0]
        ims.append(im)
    r6 = run(p6, ims)
    odT = [np.zeros((1024, S), _BF) for _ in range(2)]
    for c in range(8):
        o = np.asarray(r6[c]["odT"])
        for u in range(2):
            b, h = divmod(2 * c + u, 8)
            odT[b][h * 128:(h + 1) * 128] = o[u]
    oT_chunks = []
    for c in range(8):
        b, ch = c // 4, c % 4
        oT_chunks.append(np.ascontiguousarray(np.concatenate([ocT[b][:, ch * T:(ch + 1) * T], odT[b][:, ch * T:(ch + 1) * T]], 0)))
    P1 = dict(P)
    P1["w_out"] = P["w_out_cd"][0]
    return _layer_tail(hc, oT_chunks, 1, P1, S, final=True)


def kernel(**inp):
    P = {k: np.asarray(v) for k, v in inp.items()}
    x = P["x"]
    S = x.shape[1]
    T = S // 4
    xc = [np.ascontiguousarray(x[c // 4, (c % 4) * T:(c % 4 + 1) * T]) for c in range(8)]
    h = _layer0(xc, P, S)
    h = _layer1(h, P, S)
    out = np.stack([np.concatenate(h[b * 4:(b + 1) * 4], 0) for b in range(2)])
    return out.astype(np.float32)


C_CQ, C_KC, C_VC, C_KI, C_WI, C_QKV, C_BETA, C_A, C_Z = 0, 256, 1280, 2304, 2368, 2384, 5456, 5464, 5472
N_CD = 6496


def build_p4(T):
    kb = KB()
    NGp = T // 512
    x = kb.dram("x", [T + 128, D], F32, "ExternalInput")
    g = kb.dram("g", [D], F32, "ExternalInput")
    w = kb.dram("w", [D, N_CD], F32, "ExternalInput")
    wuq = kb.dram("wuq", [256, 1024], F32, "ExternalInput")
    wiq = kb.dram("wiq", [256, 1024], F32, "ExternalInput")
    ncq = kb.dram("ncq", [256], F32, "ExternalInput")
    nki = kb.dram("nki", [64], F32, "ExternalInput")
    cw = kb.dram("cw", [4, 3072], F32, "ExternalInput")
    alog = kb.dram("alog", [8], F32, "ExternalInput")
    dtb = kb.dram("dtb", [8], F32, "ExternalInput")
    o_qcT = kb.dram("qcT", [1024, T], BF16, "ExternalOutput")
    o_kcT = kb.dram("kcT", [1024, T], BF16, "ExternalOutput")
    o_vc = kb.dram("vc", [T, 1024], BF16, "ExternalOutput")
    o_qiT = kb.dram("qiT", [1024, T], BF16, "ExternalOutput")
    o_kiT = kb.dram("kiT", [64, T], BF16, "ExternalOutput")
    o_wi = kb.dram("wi", [T, 32], F32, "ExternalOutput")
    o_qkvT = kb.dram("qkvT", [3072, T], F32, "ExternalOutput")
    o_gb = kb.dram("gb", [16, T], F32, "ExternalOutput")
    o_szT = kb.dram("szT", [1024, T], BF16, "ExternalOutput")

    ident = emit_identity(kb)
    gain = load_gain(kb, g, D, "gain")
    nt = NormT(kb, ident, nbuf=1)
    wb = kb.sb([128, NCH, 3072], BF16, "wb")
    stage = [kb.sb([128, 512], F32, "wst%d" % i) for i in range(3)]
    ones_f = kb.sb([128, 128], F32, "ones_f")
    kb.op("pool", lambda e: e.memset(ones_f[:], 1.0), writes=[ones_f])

    def load_w(col_off, ncols):
        n = 0
        for c in range(NCH):
            for j0 in range(0, ncols, 512):
                jw = min(512, ncols - j0)
                st = stage[n % 3]
                kb.dma("sp" if n % 2 == 0 else "pool", st[:, :jw], w[c * 128:(c + 1) * 128, col_off + j0:col_off + j0 + jw], writes=[st])
                kb.op("dve", lambda e: e.tensor_scalar_mul(out=wb[:, c, j0:j0 + jw], in0=st[:, :jw], scalar1=gain[:, c:c + 1]),
                      reads=[st, gain], writes=[wb.sub((c, j0))])
                n += 1

    hnT = [kb.sb([128, NCH, 512], BF16, "hnT%d" % i) for i in range(2)]
    pm = [kb.ps([128, 512], F32, "pm%d" % i) for i in range(3)]
    pw_ = kb.ps([128, 512], F32, "pw_")
    npm = [0]

    def norm_group(gi, ntile=4):
        h = hnT[gi % 2]
        for j in range(ntile):
            r0 = 128 + gi * 512 + j * 128
            nt.run(x[r0:r0 + 128, :], h, slice(j * 128, (j + 1) * 128))
        return h

    def fm_mm(h, col0, ncols, ntok=512):
        p = pm[npm[0] % 3]
        npm[0] += 1
        for c in range(NCH):
            kb.op("pe", lambda e: e.matmul(p[0:ncols, 0:ntok], lhsT=wb[:, c, col0:col0 + ncols], rhs=h[:, c, 0:ntok],
                                           start=(c == 0), stop=(c == NCH - 1)), reads=[wb, h], writes=[p])
        return p

    load_w(0, 2384)
    gcq = load_gain(kb, ncq, 256, "gcq")
    gki = kb.sb([64, 1], F32, "gki")
    kb.dma("sp", gki[:], nki.rearrange("(p o) -> p o", o=1), writes=[gki], allow_slow_non_contiguous=True)
    wuq_b = load_w_scaled(kb, wuq, 256, 1024, "wuq_b", gcq, 128 ** -0.5, stage)
    wiq_b = load_w_scaled(kb, wiq, 256, 1024, "wiq_b", gcq, 1.0, stage)
    cq = kb.sb([128, 2, 512], F32, "cq")
    cqs = kb.sb([128, 2, 512], F32, "cqs")
    rsd = kb.sb([128, 512], F32, "rsd")
    cqn = kb.sb([128, 2, 512], BF16, "cqn")
    ob16 = [kb.sb([128, 512], BF16, "ob16_%d" % i) for i in range(3)]
    vo = [kb.sb([128, 1024], BF16, "vo%d" % i) for i in range(2)]
    wo_ = [kb.sb([128, 32], F32, "wo_%d" % i) for i in range(2)]
    kis = kb.sb([64, 512], F32, "kis")
    kiq = kb.sb([64, 512], F32, "kiq")
    n16 = [0]

    def out16(p, nrow, dst_ap, eng="act", ntok=512):
        o = ob16[n16[0] % 3]
        n16[0] += 1
        if eng == "act":
            kb.op("act", lambda e: e.copy(out=o[0:nrow, 0:ntok], in_=p[0:nrow, 0:ntok]), reads=[p], writes=[o])
        else:
            kb.op("dve", lambda e: e.tensor_copy(out=o[0:nrow, 0:ntok], in_=p[0:nrow, 0:ntok]), reads=[p], writes=[o])
        kb.dma("sp", dst_ap, o[0:nrow, 0:ntok], reads=[o], is_output=True)

    for gi in range(NGp):
        h = norm_group(gi)
        tsl = slice(gi * 512, (gi + 1) * 512)
        for c2 in range(2):
            p = fm_mm(h, C_CQ + c2 * 128, 128)
            kb.op("act", lambda e: e.copy(out=cq[:, c2, :], in_=p[:]), reads=[p], writes=[cq.sub(c2)])
            kb.op("act", lambda e: e.activation(out=cqs[:, c2, :], in_=p[:], func=AF.Square), reads=[p], writes=[cqs.sub(c2)])
        for c2 in range(2):
            kb.op("pe", lambda e: e.matmul(pw_[:], lhsT=ones_f[:], rhs=cqs[:, c2, :], start=(c2 == 0), stop=(c2 == 1)), reads=[ones_f, cqs], writes=[pw_])
        kb.op("act", lambda e: e.activation(out=rsd[:], in_=pw_[:], func=AF.Sqrt, bias=EPS, scale=1.0 / 256), reads=[pw_], writes=[rsd])
        kb.op("dve", lambda e: e.reciprocal(out=rsd[:], in_=rsd[:]), reads=[rsd], writes=[rsd])
        for c2 in range(2):
            kb.op("dve", lambda e: e.tensor_tensor(out=cqn[:, c2, :], in0=cq[:, c2, :], in1=rsd[:], op=ALU.mult), reads=[cq, rsd], writes=[cqn.sub(c2)])
        for which, wsb, dst in ((0, wuq_b, o_qcT), (1, wiq_b, o_qiT)):
            for jc in range(8):
                p = pm[npm[0] % 3]
                npm[0] += 1
                for c2 in range(2):
                    kb.op("pe", lambda e: e.matmul(p[:], lhsT=wsb[:, c2, jc * 128:(jc + 1) * 128], rhs=cqn[:, c2, :], start=(c2 == 0), stop=(c2 == 1)),
                          reads=[wsb, cqn], writes=[p])
                out16(p, 128, dst[jc * 128:(jc + 1) * 128, tsl], "act" if jc % 2 == 0 else "dve")
        for jc in range(8):
            p = fm_mm(h, C_KC + jc * 128, 128)
            out16(p, 128, o_kcT[jc * 128:(jc + 1) * 128, tsl], "act" if jc % 2 == 0 else "dve")
        p = fm_mm(h, C_KI, 64)
        kb.op("act", lambda e: e.copy(out=kis[:], in_=p[0:64, :]), reads=[p], writes=[kis])
        kb.op("act", lambda e: e.activation(out=kiq[:], in_=p[0:64, :], func=AF.Square), reads=[p], writes=[kiq])
        kb.op("pe", lambda e: e.matmul(pw_[0:64, :], lhsT=ones_f[0:64, 0:64], rhs=kiq[:], start=True, stop=True), reads=[ones_f, kiq], writes=[pw_])
        kb.op("act", lambda e: e.activation(out=kiq[:], in_=pw_[0:64, :], func=AF.Sqrt, bias=EPS, scale=1.0 / 64), reads=[pw_], writes=[kiq])
        kb.op("dve", lambda e: e.reciprocal(out=kiq[:], in_=kiq[:]), reads=[kiq], writes=[kiq])
        o = ob16[n16[0] % 3]
        n16[0] += 1
        kb.op("dve", lambda e: e.scalar_tensor_tensor(out=o[0:64, :], in0=kis[:], scalar=gki[:, 0:1], in1=kiq[:], op0=ALU.mult, op1=ALU.mult),
              reads=[kis, gki, kiq], writes=[o])
        kb.dma("sp", o_kiT[:, tsl], o[0:64, :], reads=[o], is_output=True)
        for j in range(4):
            ti = gi * 4 + j
            v_ = vo[ti % 2]
            for half in range(2):
                p = pm[npm[0] % 3]
                npm[0] += 1
                for c in range(NCH):
                    kb.op("pe", lambda e: e.matmul(p[:], lhsT=h[:, c, j * 128:(j + 1) * 128], rhs=wb[:, c, C_VC + half * 512:C_VC + (half + 1) * 512],
                                                   start=(c == 0), stop=(c == NCH - 1)), reads=[h, wb], writes=[p])
                kb.op("act" if half == 0 else "dve",
                      (lambda e: e.copy(out=v_[:, 0:512], in_=p[:])) if half == 0 else (lambda e: e.tensor_copy(out=v_[:, 512:1024], in_=p[:])),
                      reads=[p], writes=[v_.sub(half)])
            kb.dma("pool", o_vc[ti * 128:(ti + 1) * 128, :], v_[:], reads=[v_], is_output=True)
            p = pm[npm[0] % 3]
            npm[0] += 1
            for c in range(NCH):
                kb.op("pe", lambda e: e.matmul(p[:, 0:16], lhsT=h[:, c, j * 128:(j + 1) * 128], rhs=wb[:, c, C_WI:C_WI + 16],
                                               start=(c == 0), stop=(c == NCH - 1)), reads=[h, wb], writes=[p])
            w2 = wo_[ti % 2]
            kb.op("act", lambda e: e.activation(out=w2[:, 0:16], in_=p[:, 0:16], func=AF.Abs, scale=float(16 ** -0.5 * 64 ** -0.5)),
                  reads=[p], writes=[w2.sub(0)])
            kb.op("act", lambda e: e.activation(out=w2[:, 16:32], in_=p[:, 0:16], func=AF.Sign), reads=[p], writes=[w2.sub(1)])
            kb.dma("pool", o_wi[ti * 128:(ti + 1) * 128, :], w2[:], reads=[w2], is_output=True)

    load_w(C_QKV, 3072)
    cwt = kb.sb([128, 4, 24], F32, "cwt")
    for k_ in range(4):
        kb.dma("sp", cwt[:, k_, :], cw[k_].rearrange("(c p) -> p c", p=128), writes=[cwt.sub(k_)], allow_slow_non_contiguous=True)
    xc = [kb.sb([128, 515], F32, "xc%d" % i) for i in range(2)]
    carry = kb.sb([128, 24, 3], F32, "carry")
    acc = [kb.sb([128, 512], F32, "acc%d" % i) for i in range(2)]
    sq_ = rsd
    of32 = [kb.sb([128, 512], F32, "of32_%d" % i) for i in range(3)]
    nxc = 0
    for gi in range(-1, NGp):
        if gi < 0:
            h = hnT[1]
            nt.run(x[0:128, :], h, slice(0, 128))
            ntok = 128
        else:
            h = norm_group(gi)
            ntok = 512
        for cc in range(24):
            p = fm_mm(h, cc * 128, 128, ntok)
            xb = xc[nxc % 2]
            a_ = acc[nxc % 2]
            nxc += 1
            if gi >= 0:
                kb.op("dve", lambda e: e.tensor_copy(out=xb[:, 0:3], in_=carry[:, cc, :]), reads=[carry.sub(cc)], writes=[xb])
            kb.op("act", lambda e: e.copy(out=xb[:, 3:3 + ntok], in_=p[:, 0:ntok]), reads=[p], writes=[xb])
            kb.op("dve", lambda e: e.tensor_copy(out=carry[:, cc, :], in_=xb[:, ntok:ntok + 3]), reads=[xb], writes=[carry.sub(cc)])
            if gi < 0:
                continue
            kb.op("dve", lambda e: e.tensor_scalar_mul(out=a_[:], in0=xb[:, 3:515], scalar1=cwt[:, 3, cc:cc + 1]), reads=[xb, cwt], writes=[a_])
            for k_ in range(3):
                kb.op("dve", lambda e: e.scalar_tensor_tensor(out=a_[:], in0=xb[:, k_:k_ + 512], scalar=cwt[:, k_, cc:cc + 1], in1=a_[:],
                                                              op0=ALU.mult, op1=ALU.add), reads=[xb, cwt, a_], writes=[a_])
            o = of32[nxc % 3]
            kb.op("act", lambda e: e.activation(out=o[:], in_=a_[:], func=AF.Silu), reads=[a_], writes=[o])
            if cc < 16:
                kb.op("act", lambda e: e.activation(out=sq_[:], in_=o[:], func=AF.Square), reads=[o], writes=[sq_])
                kb.op("pe", lambda e: e.matmul(pw_[:], lhsT=ones_f[:], rhs=sq_[:], start=True, stop=True), reads=[ones_f, sq_], writes=[pw_])
                kb.op("act", lambda e: e.activation(out=sq_[:], in_=pw_[:], func=AF.Sqrt, bias=EPS, scale=1.0), reads=[pw_], writes=[sq_])
                kb.op("dve", lambda e: e.reciprocal(out=sq_[:], in_=sq_[:]), reads=[sq_], writes=[sq_])
                kb.op("dve", lambda e: e.tensor_tensor(out=o[:], in0=o[:], in1=sq_[:], op=ALU.mult), reads=[o, sq_], writes=[o])
            kb.dma("pool" if cc % 2 else "sp", o_qkvT[cc * 128:(cc + 1) * 128, gi * 512:(gi + 1) * 512], o[:], reads=[o], is_output=True)

    load_w(C_BETA, 1040)
    ab = kb.sb([16, 2], F32, "ab")
    kb.op("pool", lambda e: e.memset(ab[:], 0.0), writes=[ab])
    kb.dma("sp", ab[8:16, 0:1], dtb.rearrange("(p o) -> p o", o=1), reads=[], writes=[ab], allow_slow_non_contiguous=True)
    kb.dma("sp", ab[8:16, 1:2], alog.rearrange("(p o) -> p o", o=1), reads=[], writes=[ab], allow_slow_non_contiguous=True)
    nea = kb.sb([16, 1], F32, "nea")
    kb.op("act", lambda e: e.activation(out=nea[:], in_=ab[:, 1:2], func=AF.Exp), reads=[ab], writes=[nea])
    kb.op("dve", lambda e: e.tensor_scalar_mul(out=nea[:], in0=nea[:], scalar1=-1.0), reads=[nea], writes=[nea])
    gbt = [acc[0], acc[1]]
    gtm = [of32[0], of32[1]]
    for gi in range(NGp):
        h = norm_group(gi)
        tsl = slice(gi * 512, (gi + 1) * 512)
        p = fm_mm(h, 0, 16)
        t_ = gbt[gi % 2]
        g_ = gtm[gi % 2]
        kb.op("act", lambda e: e.activation(out=g_[0:16, :], in_=p[0:16, :], func=AF.Exp, bias=ab[:, 0:1]), reads=[p, ab], writes=[g_])
        kb.op("act", lambda e: e.activation(out=g_[0:16, :], in_=g_[0:16, :], func=AF.Ln, bias=1.0), reads=[g_], writes=[g_])
        kb.op("dve", lambda e: e.tensor_scalar_mul(out=g_[0:16, :], in0=g_[0:16, :], scalar1=nea[:, 0:1]), reads=[g_, nea], writes=[g_])
        kb.op("act", lambda e: e.activation(out=t_[0:16, :], in_=p[0:16, :], func=AF.Sigmoid), reads=[p], writes=[t_])
        kb.dma("sp", o_gb[0:8, tsl], t_[0:8, :], reads=[t_], is_output=True)
        kb.dma("sp", o_gb[8:16, tsl], g_[8:16, :], reads=[g_], is_output=True)
        for jc in range(8):
            p = fm_mm(h, 16 + jc * 128, 128)
            o = ob16[n16[0] % 3]
            n16[0] += 1
            kb.op("act", lambda e: e.activation(out=o[:], in_=p[:], func=AF.Silu), reads=[p], writes=[o])
            kb.dma("pool", o_szT[jc * 128:(jc + 1) * 128, tsl], o[:], reads=[o], is_output=True)
    return kb.finish()


def build_p6(S, NU=2, NBC=32):
    kb = KB()
    CH = 64
    BT = NBC * CH
    NBLK = S // BT
    qT = kb.dram("qT", [NU, 128, S], F32, "ExternalInput")
    kT = kb.dram("kT", [NU, 128, S], F32, "ExternalInput")
    vT = kb.dram("vT", [NU, 128, S], F32, "ExternalInput")
    gg = kb.dram("g", [NU, S], F32, "ExternalInput")
    bb = kb.dram("beta", [NU, S], F32, "ExternalInput")
    szT = kb.dram("szT", [NU, 128, S], BF16, "ExternalInput")
    gno = kb.dram("gno", [128], F32, "ExternalInput")
    odT = kb.dram("odT", [NU, 128, S], BF16, "ExternalOutput")

    identf = emit_identity(kb, F32)
    ones_f = kb.sb([128, 128], F32, "ones_f")
    kb.op("pool", lambda e: e.memset(ones_f[:], 1.0), writes=[ones_f])
    Lt = kb.sb([64, 64], F32, "Lt")
    kb.op("pool", lambda e: e.memset(Lt[:], 1.0), writes=[Lt])
    kb.op("pool", lambda e: e.affine_select(out=Lt[:], in_=Lt[:], pattern=[[1, 64]], compare_op=ALU.is_ge, fill=0.0, base=0,
                                             channel_multiplier=-1), reads=[Lt], writes=[Lt])
    gnt = kb.sb([128, 1], F32, "gnt")
    kb.dma("sp", gnt[:], gno.rearrange("(p o) -> p o", o=1), writes=[gnt], allow_slow_non_contiguous=True)

    B = [kb.ps([128, 512], F32, "B%d" % i) for i in range(8)]
    qb = [kb.sb([128, BT], F32, "qb%d" % i) for i in range(2)]
    kbk = [kb.sb([128, BT], F32, "kbk%d" % i) for i in range(2)]
    vb = [kb.sb([128, BT], F32, "vb%d" % i) for i in range(2)]
    szb = [kb.sb([128, BT], BF16, "szb%d" % i) for i in range(2)]
    gB = [kb.sb([64, NBC], F32, "gB%d" % i) for i in range(2)]
    bB = [kb.sb([64, NBC], F32, "bB%d" % i) for i in range(2)]
    gcB = [kb.sb([64, NBC], F32, "gcB%d" % i) for i in range(2)]
    egl = [kb.sb([128, NBC], F32, "egl%d" % i) for i in range(2)]
    ekg = [kb.sb([64, NBC], F32, "ekg%d" % i) for i in range(2)]
    sqe = [kb.sb([64, NBC], F32, "sqe%d" % i) for i in range(2)]
    bw = [kb.sb([64, NBC], F32, "bw%d" % i) for i in range(2)]
    nbt = [kb.sb([64, NBC], F32, "nbt%d" % i) for i in range(2)]
    ob = [kb.sb([128, BT], BF16, "ob%d" % i) for i in range(2)]
    rr = [kb.sb([64, 256], F32, "rr%d" % i) for i in range(4)]
    kg = [kb.sb([64, 128], F32, "kg%d" % i) for i in range(2)]
    dg = [kb.sb([64, 64], F32, "dg%d" % i) for i in range(2)]
    Dm = [kb.sb([64, 64], F32, "Dm%d" % i) for i in range(2)]
    CC = [kb.sb([64, 128], F32, "CC%d" % i) for i in range(4)]
    c0a = [kb.sb([64, 128], F32, "c0a%d" % i) for i in range(2)]
    aT = [kb.sb([64, 64], F32, "aT%d" % i) for i in range(2)]
    wT = [kb.sb([128, 64], F32, "wT%d" % i) for i in range(2)]
    vnew = [kb.sb([64, 128], F32, "vnew%d" % i) for i in range(2)]
    t1 = [kb.sb([64, 128], F32, "t1_%d" % i) for i in range(2)]
    ot = [kb.sb([64, 128], F32, "ot%d" % i) for i in range(2)]
    osq = kb.sb([64, 128], F32, "osq")
    oss = [kb.sb([64, 1], F32, "oss%d" % i) for i in range(2)]
    St = [kb.sb([128, 128], F32, "St%d" % i) for i in range(2)]
    nblk = 0
    nch = 0
    for u in range(NU):
        S_cur = St[0]
        kb.op("pool", lambda e: e.memset(S_cur[:], 0.0), writes=[S_cur])
        si = 0
        for blk in range(NBLK):
            k2 = nblk % 2
            nblk += 1
            tsl = slice(blk * BT, (blk + 1) * BT)
            kb.dma("sp", qb[k2][:], qT[u, :, tsl], writes=[qb[k2]])
            kb.dma("pool", kbk[k2][:], kT[u, :, tsl], writes=[kbk[k2]])
            kb.dma("sp", vb[k2][:], vT[u, :, tsl], writes=[vb[k2]])
            kb.dma("pool", szb[k2][:], szT[u, :, tsl], writes=[szb[k2]])
            kb.dma("sp", gB[k2][:], gg[u, tsl].rearrange("(n t) -> t n", t=CH), writes=[gB[k2]], allow_slow_non_contiguous=True)
            kb.dma("pool", bB[k2][:], bb[u, tsl].rearrange("(n t) -> t n", t=CH), writes=[bB[k2]], allow_slow_non_contiguous=True)
            pg = B[3]
            kb.op("pe", lambda e: e.matmul(pg[0:64, 0:NBC], lhsT=Lt[:], rhs=gB[k2][:], start=True, stop=True), reads=[Lt, gB[k2]], writes=[pg])
            kb.op("pe", lambda e: e.matmul(pg[:, 64:64 + NBC], lhsT=ones_f[0:64, :], rhs=gB[k2][:], start=True, stop=True), reads=[ones_f, gB[k2]], writes=[pg])
            kb.op("act", lambda e: e.copy(out=gcB[k2][:], in_=pg[0:64, 0:NBC]), reads=[pg], writes=[gcB[k2]])
            kb.op("act", lambda e: e.activation(out=egl[k2][:], in_=pg[:, 64:64 + NBC], func=AF.Exp), reads=[pg], writes=[egl[k2]])
            kb.op("dve", lambda e: e.tensor_tensor(out=ekg[k2][:], in0=pg[0:64, 64:64 + NBC], in1=gcB[k2][:], op=ALU.subtract), reads=[pg, gcB[k2]], writes=[ekg[k2]])
            kb.op("act", lambda e: e.activation(out=ekg[k2][:], in_=ekg[k2][:], func=AF.Exp), reads=[ekg[k2]], writes=[ekg[k2]])
            kb.op("act", lambda e: e.activation(out=sqe[k2][:], in_=gcB[k2][:], func=AF.Exp), reads=[gcB[k2]], writes=[sqe[k2]])
            kb.op("dve", lambda e: e.tensor_tensor(out=bw[k2][:], in0=sqe[k2][:], in1=bB[k2][:], op=ALU.mult), reads=[sqe[k2], bB[k2]], writes=[bw[k2]])
            kb.op("dve", lambda e: e.tensor_scalar_mul(out=sqe[k2][:], in0=sqe[k2][:], scalar1=float(128 ** -0.5)), reads=[sqe[k2]], writes=[sqe[k2]])
            kb.op("dve", lambda e: e.tensor_scalar_mul(out=nbt[k2][:], in0=bB[k2][:], scalar1=-1.0), reads=[bB[k2]], writes=[nbt[k2]])
            for n in range(NBC):
                c2 = nch % 2
                nch += 1
                cs = slice(n * CH, (n + 1) * CH)
                kTc, qTc, vTc = kbk[k2][:, cs], qb[k2][:, cs], vb[k2][:, cs]
                col = lambda t_: t_[:, n:n + 1]
                p0 = B[0]
                kb.op("pe", lambda e: e.transpose(out=p0[0:64, 0:128], in_=vTc, identity=identf[:]), reads=[vb[k2], identf], writes=[p0])
                kb.op("pe", lambda e: e.transpose(out=p0[0:64, 128:256], in_=kTc, identity=identf[:]), reads=[kbk[k2], identf], writes=[p0])
                r0 = rr[(nch * 2) % 4]
                r1 = rr[(nch * 2 + 1) % 4]
                kb.op("dve", lambda e: e.tensor_scalar_mul(out=r0[:, 0:128], in0=p0[0:64, 0:128], scalar1=col(bB[k2])), reads=[p0, bB[k2]], writes=[r0])
                kb.op("act", lambda e: e.activation(out=r0[:, 128:256], in_=p0[0:64, 128:256], func=AF.Copy, scale=col(bw[k2])), reads=[p0, bw[k2]], writes=[r0])
                kg_ = kg[c2]
                kb.op("act", lambda e: e.activation(out=kg_[:], in_=p0[0:64, 128:256], func=AF.Copy, scale=col(ekg[k2])), reads=[p0, ekg[k2]], writes=[kg_])
                dg_ = dg[c2]
                kb.op("dve", lambda e: e.tensor_scalar_mul(out=dg_[:], in0=identf[0:64, 0:64], scalar1=col(gcB[k2])), reads=[identf, gcB[k2]], writes=[dg_])
                p1 = B[1]
                kb.op("pe", lambda e: e.matmul(p1[0:64, 0:64], lhsT=ones_f[0:64, 0:64], rhs=dg_[:], start=True, stop=True), reads=[ones_f, dg_], writes=[p1])
                kb.op("pe", lambda e: e.matmul(p1[0:64, 64:128], lhsT=kTc, rhs=kTc, start=True, stop=True), reads=[kbk[k2]], writes=[p1])
                kb.op("pe", lambda e: e.matmul(p1[0:64, 128:192], lhsT=qTc, rhs=kTc, start=True, stop=True), reads=[qb[k2], kbk[k2]], writes=[p1])
                Dm_ = Dm[c2]
                kb.op("act", lambda e: e.activation(out=Dm_[:], in_=p1[0:64, 0:64], func=AF.Exp, scale=-1.0, bias=col(gcB[k2])), reads=[p1, gcB[k2]], writes=[Dm_])
                kb.op("pool", lambda e: e.affine_select(out=Dm_[:], in_=Dm_[:], pattern=[[-1, 64]], compare_op=ALU.is_ge, fill=0.0, base=0,
                                                         channel_multiplier=1), reads=[Dm_], writes=[Dm_])
                ca = c0a[c2]
                kb.op("dve", lambda e: e.scalar_tensor_tensor(out=ca[:, 0:64], in0=p1[0:64, 64:128], scalar=col(nbt[k2]), in1=Dm_[:], op0=ALU.mult, op1=ALU.mult),
                      reads=[p1, nbt[k2], Dm_], writes=[ca])
                kb.op("dve", lambda e: e.scalar_tensor_tensor(out=ca[:, 64:128], in0=p1[0:64, 128:192], scalar=float(128 ** -0.5), in1=Dm_[:], op0=ALU.mult, op1=ALU.mult),
                      reads=[p1, Dm_], writes=[ca])
                kb.op("pool", lambda e: e.affine_select(out=ca[:, 0:64], in_=ca[:, 0:64], pattern=[[-1, 64]], compare_op=ALU.is_gt, fill=0.0, base=0,
                                                         channel_multiplier=1), reads=[ca], writes=[ca])
                p2 = B[2]
                kb.op("pe", lambda e: e.transpose(out=p2[0:64, 0:64], in_=ca[:, 0:64], identity=identf[0:64, 0:64]), reads=[ca, identf], writes=[p2])
                kb.op("pe", lambda e: e.transpose(out=p2[0:64, 64:128], in_=ca[:, 64:128], identity=identf[0:64, 0:64]), reads=[ca, identf], writes=[p2])
                cc = CC[(nch * 2) % 4]
                cc2 = CC[(nch * 2 + 1) % 4]
                kb.op("act", lambda e: e.copy(out=cc[:, 0:64], in_=ca[:, 0:64]), reads=[ca], writes=[cc])
                kb.op("dve", lambda e: e.tensor_copy(out=cc[:, 64:128], in_=p2[0:64, 0:64]), reads=[p2], writes=[cc])
                aT_ = aT[c2]
                kb.op("act", lambda e: e.copy(out=aT_[:], in_=p2[0:64, 64:128]), reads=[p2], writes=[aT_])
                rc, rn = r0, r1
                ck, cn = cc, cc2
                for k_ in range(6):
                    p3 = B[3]
                    kb.op("pe", lambda e: e.matmul(p3[0:64, 0:256], lhsT=ck[:, 64:128], rhs=rc[:], start=True, stop=True), reads=[ck, rc], writes=[p3])
                    kb.op("dve", lambda e: e.tensor_tensor(out=rn[:], in0=p3[0:64, 0:256], in1=rc[:], op=ALU.add), reads=[p3, rc], writes=[rn])
                    rc, rn = rn, rc
                    if k_ < 5:
                        p4 = B[4]
                        kb.op("pe", lambda e: e.matmul(p4[0:64, 0:64], lhsT=ck[:, 64:128], rhs=ck[:, 0:64], start=True, stop=True), reads=[ck], writes=[p4])
                        kb.op("pe", lambda e: e.matmul(p4[0:64, 64:128], lhsT=ck[:, 0:64], rhs=ck[:, 64:128], start=True, stop=True), reads=[ck], writes=[p4])
                        kb.op("act", lambda e: e.copy(out=cn[:], in_=p4[0:64, 0:128]), reads=[p4], writes=[cn])
                        ck, cn = cn, ck
                kb.op("pe", lambda e: e.transpose(out=p0[:, 256:320], in_=rc[:, 128:256], identity=identf[0:64, 0:64]), reads=[rc, identf], writes=[p0])
                wT_ = wT[c2]
                kb.op("act", lambda e: e.copy(out=wT_[:], in_=p0[:, 256:320]), reads=[p0], writes=[wT_])
                p5 = B[5]
                kb.op("pe", lambda e: e.matmul(p5[0:64, 0:128], lhsT=wT_[:], rhs=S_cur[:], start=True, stop=True), reads=[wT_, S_cur], writes=[p5])
                kb.op("pe", lambda e: e.matmul(p5[0:64, 128:256], lhsT=qTc, rhs=S_cur[:], start=True, stop=True), reads=[qb[k2], S_cur], writes=[p5])
                vn = vnew[c2]
                kb.op("dve", lambda e: e.tensor_tensor(out=vn[:], in0=rc[:, 0:128], in1=p5[0:64, 0:128], op=ALU.subtract), reads=[rc, p5], writes=[vn])
                t1_ = t1[c2]
                kb.op("act", lambda e: e.activation(out=t1_[:], in_=p5[0:64, 128:256], func=AF.Copy, scale=col(sqe[k2])), reads=[p5, sqe[k2]], writes=[t1_])
                p6 = B[6]
                kb.op("pe", lambda e: e.matmul(p6[0:64, 0:128], lhsT=aT_[:], rhs=vn[:], start=True, stop=True), reads=[aT_, vn], writes=[p6])
                p7 = B[7]
                kb.op("pe", lambda e: e.matmul(p7[:, 0:128], lhsT=kg_[:], rhs=vn[:], start=True, stop=True), reads=[kg_, vn], writes=[p7])
                S_nxt = St[1 - si]
                kb.op("dve", lambda e: e.scalar_tensor_tensor(out=S_nxt[:], in0=S_cur[:], scalar=col(egl[k2]), in1=p7[:, 0:128], op0=ALU.mult, op1=ALU.add),
                      reads=[S_cur, egl[k2], p7], writes=[S_nxt])
                S_cur = S_nxt
                si = 1 - si
                o_ = ot[c2]
                kb.op("dve", lambda e: e.tensor_tensor(out=o_[:], in0=p6[0:64, 0:128], in1=t1_[:], op=ALU.add), reads=[p6, t1_], writes=[o_])
                ss_ = oss[c2]
                kb.op("act", lambda e: e.activation(out=osq[:], in_=o_[:], func=AF.Square, accum_out=ss_[:]), reads=[o_], writes=[osq, ss_])
                kb.op("act", lambda e: e.activation(out=ss_[:], in_=ss_[:], func=AF.Sqrt, bias=EPS, scale=1.0 / 128), reads=[ss_], writes=[ss_])
                kb.op("dve", lambda e: e.reciprocal(out=ss_[:], in_=ss_[:]), reads=[ss_], writes=[ss_])
                kb.op("dve", lambda e: e.tensor_scalar_mul(out=o_[:], in0=o_[:], scalar1=ss_[:, 0:1]), reads=[o_, ss_], writes=[o_])
                kb.op("pe", lambda e: e.transpose(out=p2[:, 128:192], in_=o_[:], identity=identf[0:64, 0:64]), reads=[o_, identf], writes=[p2])
                kb.op("dve", lambda e: e.scalar_tensor_tensor(out=ob[k2][:, cs], in0=p2[:, 128:192], scalar=gnt[:, 0:1], in1=szb[k2][:, cs], op0=ALU.mult, op1=ALU.mult),
                      reads=[p2, gnt, szb[k2]], writes=[ob[k2].sub(n)])
            kb.dma("sp", odT[u, :, tsl], ob[k2][:], reads=[ob[k2]], is_output=True)
    return kb.finish()


MARK = -2.0e30


def build_p5(S, TOPK=256):
    kb = KB()
    NR = S // 512
    NB = S // 128
    kiT = kb.dram("kiT", [64, S], BF16, "ExternalInput")
    qiT = kb.dram("qiT", [NR, 64, 16, 128], BF16, "ExternalInput")
    wi = kb.dram("wi", [NR, 128, 32], F32, "ExternalInput")
    cm = kb.dram("cm", [128, 512], F32, "ExternalInput")
    cm01 = kb.dram("cm01", [128, 512], BF16, "ExternalInput")
    kcT = kb.dram("kcT", [8, 128, S], BF16, "ExternalInput")
    vc = kb.dram("vc", [S, 1024], BF16, "ExternalInput")
    qc = kb.dram("qc", [NR, 8, 128, 128], BF16, "ExternalInput")
    BTd = kb.dram("BT", [8, 128, 16, 128], F32, "ExternalInput")
    b31 = kb.dram("b31", [128, 8], F32, "ExternalInput")
    ocT = kb.dram("ocT", [NR, 8, 128, 128], BF16, "ExternalOutput")
    MS = kb.nc.dram_tensor("MS", [NR, 128, NR * 512], BF16).ap()
    ms_t = [T(None) for _ in range(NR)]

    ident = emit_identity(kb)
    ones_b = kb.sb([128, 128], BF16, "ones_b")
    kb.op("pool", lambda e: e.memset(ones_b[:], 1.0), writes=[ones_b])
    kis = kb.sb([64, S], BF16, "kis")
    kb.dma("sp", kis[:], kiT, writes=[kis])
    cmt = kb.sb([128, 512], F32, "cmt")
    kb.dma("pool", cmt[:], cm, writes=[cmt])
    cm1 = kb.sb([128, 512], BF16, "cm1")
    kb.dma("pool", cm1[:], cm01, writes=[cm1])
    b31t = kb.sb([128, 8], F32, "b31t")
    kb.dma("sp", b31t[:], b31, writes=[b31t])
    work = kb.sb([128, S], F32, "work")
    mk = kb.sb([128, S], BF16, "mk")
    B = [kb.ps([128, 512], F32, "B%d" % i) for i in range(8)]
    qit = [kb.sb([64, 16, 128], BF16, "qit%d" % i) for i in range(2)]
    wit = [kb.sb([128, 32], F32, "wit%d" % i) for i in range(2)]
    rt = [kb.sb([128, 512], F32, "rt%d" % i) for i in range(2)]
    m8 = kb.sb([128, 8], F32, "m8")
    mts = [kb.sb([128, 512], BF16, "mts%d" % i) for i in range(3)]
    nps = 0
    nmt = 0
    for m in range(NR):
        q_, w_ = qit[m % 2], wit[m % 2]
        kb.dma("sp", q_[:], qiT[m], writes=[q_])
        kb.dma("pool", w_[:], wi[m], writes=[w_])
        nel = (m + 1) * 512
        for kg in range(m + 1):
            gsl = slice(kg * 512, (kg + 1) * 512)
            wk = work.sub(("g", kg))
            for h in range(16):
                ps = B[nps % 2]
                r_ = rt[nps % 2]
                nps += 1
                kb.op("pe", lambda e: e.matmul(ps[:], lhsT=q_[:, h, :], rhs=kis[:, gsl], start=True, stop=True), reads=[q_, kis], writes=[ps])
                kb.op("act", lambda e: e.activation(out=r_[:], in_=ps[:], func=AF.Relu, scale=w_[:, h:h + 1]), reads=[ps, w_], writes=[r_])
                if h == 0:
                    kb.op("dve", lambda e: e.tensor_scalar_mul(out=work[:, gsl], in0=r_[:], scalar1=w_[:, 16:17]), reads=[r_, w_], writes=[wk])
                else:
                    kb.op("dve", lambda e: e.scalar_tensor_tensor(out=work[:, gsl], in0=r_[:], scalar=w_[:, 16 + h:17 + h], in1=work[:, gsl],
                                                                  op0=ALU.mult, op1=ALU.add), reads=[r_, w_, wk], writes=[wk])
            if kg == m:
                kb.op("dve", lambda e: e.tensor_tensor(out=work[:, gsl], in0=work[:, gsl], in1=cmt[:], op=ALU.add), reads=[wk, cmt], writes=[wk])
        for rd in range(TOPK // 8):
            kb.op("dve", lambda e: e.max(out=m8[:], in_=work[:, 0:nel]), reads=[work], writes=[m8])
            kb.op("dve", lambda e: e.match_replace(out=work[:, 0:nel], in_to_replace=m8[:], in_values=work[:, 0:nel], imm_value=MARK),
                  reads=[work, m8], writes=[work])
        kb.op("dve", lambda e: e.tensor_single_scalar(out=mk[:, 0:nel], in_=work[:, 0:nel], scalar=-1.5e30, op=ALU.is_lt), reads=[work], writes=[mk])
        kb.op("dve", lambda e: e.tensor_tensor(out=mk[:, m * 512:nel], in0=mk[:, m * 512:nel], in1=cm1[:], op=ALU.mult), reads=[mk, cm1], writes=[mk])
        for kg in range(m + 1):
            pt = B[2 + kg % 2]
            ptv = bf16_view(pt)
            for jj in range(4):
                blk = kg * 4 + jj
                kb.op("pe", lambda e: e.transpose(out=ptv[:, jj * 128:(jj + 1) * 128], in_=mk[:, blk * 128:(blk + 1) * 128], identity=ident[:]),
                      reads=[mk, ident], writes=[pt])
            ms_ = mts[nmt % 3]
            nmt += 1
            if kg % 2 == 0:
                kb.op("act", lambda e: e.copy(out=ms_[:], in_=ptv[:, 0:512]), reads=[pt], writes=[ms_])
            else:
                kb.op("dve", lambda e: e.tensor_copy(out=ms_[:], in_=ptv[:, 0:512]), reads=[pt], writes=[ms_])
            kb.dma("pool", MS[m, :, kg * 512:(kg + 1) * 512], ms_[:], reads=[ms_], writes=[ms_t[m]])
    wbf = work[:].bitcast(BF16)
    mtb = [work.sub(("mt", 0)), work.sub(("mt", 1))]
    ks = kb.sb([128, S], BF16, "ks")
    vs = mk.sub("vs")
    vs.h = mk[:].rearrange("p (j d) -> p j d", d=128)
    bts = kb.sb([128, 16, 128], BF16, "bts")
    btf = kb.sb([128, 16, 128], F32, "btf")
    qct = [kb.sb([128, 128], BF16, "qct%d" % i) for i in range(2)]
    pt_ = [kb.sb([128, 512], BF16, "pt_%d" % i) for i in range(2)]
    pmt = [kb.sb([128, 512], BF16, "pmt%d" % i) for i in range(2)]
    rden = kb.sb([128, 128], F32, "rden")
    ost = [kb.sb([128, 128], BF16, "ost%d" % i) for i in range(2)]
    nz = 0
    nrd = 0
    for h in range(8):
        kb.dma("sp", ks[:], kcT[h], writes=[ks])
        nv = max(1, NB // 16)
        for i in range(nv):
            j0, j1 = i * NB // nv, (i + 1) * NB // nv
            kb.dma("sp" if i % 2 == 0 else "pool", vs[:, j0:j1, :], vc[j0 * 128:j1 * 128, h * 128:(h + 1) * 128].rearrange("(j p) d -> p j d", p=128),
                   writes=[vs])
        kb.dma("pool", btf[:], BTd[h], writes=[btf])
        kb.op("dve", lambda e: e.tensor_copy(out=bts[:], in_=btf[:]), reads=[btf], writes=[bts])
        for m in range(NR):
            k2 = nrd % 2
            nrd += 1
            nel = (m + 1) * 512
            mt_ap = wbf[:, k2 * S:k2 * S + nel]
            kb.dma("sp", mt_ap, MS[m, :, 0:nel], reads=[ms_t[m]], writes=[mtb[k2]])
            qc_ = qct[k2]
            kb.dma("pool", qc_[:], qc[m, h], writes=[qc_])
            po, pden = B[6], B[7]
            ngrp = m + 1
            for kg in range(ngrp):
                near = (m - kg) <= 3
                pz = B[4 + nz % 2]
                p_ = pt_[nz % 2]
                pm_ = pmt[nz % 2]
                nz += 1
                for jj in range(4):
                    j = kg * 4 + jj
                    kb.op("pe", lambda e: e.matmul(pz[:, jj * 128:(jj + 1) * 128], lhsT=ks[:, j * 128:(j + 1) * 128], rhs=qc_[:], start=True, stop=not near),
                          reads=[ks, qc_], writes=[pz])
                    if near:
                        e_ = 4 * (m - kg) + (3 - jj)
                        kb.op("pe", lambda e: e.matmul(pz[:, jj * 128:(jj + 1) * 128], lhsT=ident[:], rhs=bts[:, e_, :], start=False, stop=True),
                              reads=[ident, bts], writes=[pz])
                if near:
                    kb.op("act", lambda e: e.activation(out=p_[:], in_=pz[:], func=AF.Exp), reads=[pz], writes=[p_])
                else:
                    kb.op("act", lambda e: e.activation(out=p_[:], in_=pz[:], func=AF.Exp, bias=b31t[:, h:h + 1]), reads=[pz, b31t], writes=[p_])
                kb.op("dve", lambda e: e.tensor_tensor(out=pm_[:], in0=p_[:], in1=wbf[:, k2 * S + kg * 512:k2 * S + (kg + 1) * 512], op=ALU.mult),
                      reads=[p_, mtb[k2]], writes=[pm_])
                for jj in range(4):
                    j = kg * 4 + jj
                    first = (kg == 0 and jj == 0)
                    last = (kg == ngrp - 1 and jj == 3)
                    kb.op("pe", lambda e: e.matmul(po[:, 0:128], lhsT=vs[:, j, :], rhs=pm_[:, jj * 128:(jj + 1) * 128], start=first, stop=last),
                          reads=[vs, pm_], writes=[po])
                    kb.op("pe", lambda e: e.matmul(pden[:, 0:128], lhsT=ones_b[:], rhs=pm_[:, jj * 128:(jj + 1) * 128], start=first, stop=last),
                          reads=[ones_b, pm_], writes=[pden])
            kb.op("dve", lambda e: e.reciprocal(out=rden[:], in_=pden[:, 0:128]), reads=[pden], writes=[rden])
            o_ = ost[k2]
            kb.op("dve", lambda e: e.tensor_tensor(out=o_[:], in0=po[:, 0:128], in1=rden[:], op=ALU.mult), reads=[po, rden], writes=[o_])
            kb.dma("pool", ocT[m, h], o_[:], reads=[o_], is_output=True)
    return kb.finish()


def _t5_bucket_np(dist):
    n = np.maximum(dist, 0)
    nf = np.maximum(n, 1).astype(np.float32)
    lr = np.log(nf / np.float32(16)) / np.float32(math.log(2048 / 16))
    large = 16 + (lr * np.float32(16)).astype(np.int32)
    return np.where(n < 16, n, np.minimum(large, 31))


def _dsa_consts(r, rel_bias):
    t = np.arange(128)[:, None]
    sp = np.arange(512)[None, :]
    ok = sp <= 128 * r + t
    cm = np.where(ok, 0.0, -1.0e30).astype(np.float32)
    cm01 = ok.astype(np.float32).astype(_BF)
    s = np.arange(128)[:, None, None]
    e = np.arange(16)[None, :, None]
    tt = np.arange(128)[None, None, :]
    dist = 128 * (e - 3 + r) + tt - s
    bucket = _t5_bucket_np(dist)
    BT = np.ascontiguousarray(np.transpose(rel_bias[bucket], (3, 0, 1, 2)))
    BT = np.where((dist >= 0)[None], BT, 0.0).astype(np.float32)
    b31 = np.ascontiguousarray(np.broadcast_to(rel_bias[31][None, :], (128, 8))).astype(np.float32)
    return cm, cm01, BT, b31
```

```python
from contextlib import ExitStack
import numpy as np
import math
import concourse.bass as bass
import concourse.mybir as mybir
from concourse.bass_utils import run_bass_kernel_spmd

F32 = mybir.dt.float32
BF16 = mybir.dt.bfloat16
I32 = mybir.dt.int32
U32 = mybir.dt.uint32
AF = mybir.ActivationFunctionType
ALU = mybir.AluOpType
AX = mybir.AxisListType

SAME_ENGINE_SYNC = True
NDMA_SEM = 6


class T:
    def __init__(self, h, parent=None, atomic=False):
        self.h = h
        self.parent = parent
        self.atomic = atomic
        self.kids = {}
        self.w = None
        self.r = []

    def sub(self, key):
        if self.atomic:
            return self
        if key not in self.kids:
            self.kids[key] = T(self.h, self)
        return self.kids[key]

    def __getitem__(self, idx):
        return self.h[idx]

    def _related(self):
        out = [self]
        p = self.parent
        while p is not None:
            out.append(p)
            p = p.parent
        stack = list(self.kids.values())
        while stack:
            k = stack.pop()
            out.append(k)
            stack.extend(k.kids.values())
        return out


class KB:
    def __init__(self):
        self.nc = bass.Bass("TRN2", target_bir_lowering=False)
        nc = self.nc
        self.es = ExitStack()
        self.es.enter_context(nc.allow_low_precision("bf16 matmul operands, fp32 accumulation"))
        self.eng = {"pe": nc.tensor, "act": nc.scalar, "dve": nc.vector, "pool": nc.gpsimd, "sp": nc.sync}
        self.sem = {}
        self.cnt = {}
        for e in ("pe", "act", "dve", "pool"):
            self.sem[e] = self.es.enter_context(nc.semaphore("s_" + e))
            self.cnt[e] = 0
        self.dsem = {}
        self.dcnt = {}
        for q in ("sp", "pool", "act"):
            self.dsem[q] = [self.es.enter_context(nc.semaphore("d_%s%d" % (q, i))) for i in range(NDMA_SEM)]
            self.dcnt[q] = 0
        self.waited = {e: {} for e in self.eng}
        self.out_tokens = []
        self.n_inst = 0
        self._uid = 0

    def dram(self, name, shape, dt, kind):
        return self.nc.dram_tensor(name, list(shape), dt, kind=kind).ap()

    def sb(self, shape, dt, name=None):
        self._uid += 1
        h = self.es.enter_context(self.nc.sbuf_tensor(name or ("t%d" % self._uid), list(shape), dt))
        return T(h)

    def ps(self, shape, dt=F32, name=None):
        self._uid += 1
        h = self.es.enter_context(self.nc.psum_tensor(name or ("p%d" % self._uid), list(shape), dt))
        return T(h, atomic=True)

    def _wait(self, e, tok):
        if tok is None:
            return
        sem, val, src = tok
        if src == e and (e == "pe" or not SAME_ENGINE_SYNC):
            return
        if src == e and e == "sp":
            pass
        w = self.waited[e]
        if w.get(sem.name, 0) >= val:
            return
        self.eng[e].wait_ge(sem, val)
        w[sem.name] = val

    def _deps(self, e, reads, writes):
        for t in reads:
            for x in t._related():
                self._wait(e, x.w)
        for t in writes:
            for x in t._related():
                self._wait(e, x.w)
                for tok in x.r:
                    self._wait(e, tok)

    def _commit(self, tok, reads, writes):
        for t in reads:
            t.r.append(tok)
            if len(t.r) > 24:
                t.r = t.r[-24:] if False else self._compact(t.r)
        for t in writes:
            t.w = tok
            t.r = []
            stack = list(t.kids.values())
            while stack:
                k = stack.pop()
                k.w = None
                k.r = []
                stack.extend(k.kids.values())

    @staticmethod
    def _compact(toks):
        best = {}
        for sem, val, src in toks:
            k = sem.name
            if k not in best or best[k][1] < val:
                best[k] = (sem, val, src)
        return list(best.values())

    def op(self, e, fn, reads=(), writes=()):
        self._deps(e, reads, writes)
        ins = fn(self.eng[e])
        self.cnt[e] += 1
        ins.then_inc(self.sem[e], 1)
        tok = (self.sem[e], self.cnt[e], e)
        self._commit(tok, reads, writes)
        self.n_inst += 1
        return tok

    def dma(self, q, out, in_, reads=(), writes=(), is_output=False, **kw):
        i = self.dcnt[q]
        k = i % NDMA_SEM
        sem = self.dsem[q][k]
        prev = 16 * (i // NDMA_SEM)
        if prev > 0:
            w = self.waited[q]
            if w.get(sem.name, 0) < prev:
                self.eng[q].wait_ge(sem, prev)
                w[sem.name] = prev
        self._deps(q, reads, writes)
        ins = self.eng[q].dma_start(out=out, in_=in_, **kw)
        ins.then_inc(sem, 16)
        self.dcnt[q] += 1
        tok = (sem, prev + 16, "dma_" + q)
        self._commit(tok, reads, writes)
        if is_output:
            self.out_tokens.append(tok)
        self.n_inst += 1
        return tok

    def finish(self):
        for tok in self.out_tokens:
            self._wait("sp", tok)
        for e in ("pe", "act", "dve", "pool"):
            if self.cnt[e] > 0:
                self._wait("sp", (self.sem[e], self.cnt[e], e))
        for q in self.dsem:
            i = self.dcnt[q]
            for k in range(NDMA_SEM):
                n = (i - k + NDMA_SEM - 1) // NDMA_SEM if i > k else 0
                if n > 0:
                    self._wait("sp", (self.dsem[q][k], 16 * n, "dma_" + q))
        self.es.close()
        return self.nc


def run(nc, in_maps, n=8):
    res = run_bass_kernel_spmd(nc, in_maps, core_ids=list(range(n)))
    return res.results


D = 2048
NCH = D // 128
EPS = 1e-6


def emit_identity(kb, dt=BF16):
    ident = kb.sb([128, 128], dt, "ident")
    kb.op("pool", lambda e: e.memset(ident[:], 1.0), writes=[ident])
    kb.op("pool", lambda e: e.affine_select(out=ident[:], in_=ident[:], pattern=[[-1, 128]],
                                             compare_op=ALU.is_equal, fill=0.0, base=0,
                                             channel_multiplier=1), reads=[ident], writes=[ident])
    return ident


def bf16_view(pt):
    ap = pt[:]
    if ap.dtype == BF16:
        return ap
    return ap.bitcast(BF16)


class NormT:
    def __init__(self, kb, ident, nbuf=2, pts=None):
        self.kb = kb
        self.ident = ident
        self.xt = [kb.sb([128, D], F32, "nx%d" % i) for i in range(nbuf)]
        self.sq = kb.sb([128, D], BF16, "nsq")
        self.xn = [kb.sb([128, D], BF16, "nxn%d" % i) for i in range(2)]
        self.ss = [kb.sb([128, 1], F32, "nss%d" % i) for i in range(2)]
        self.rs = [kb.sb([128, 1], F32, "nrs%d" % i) for i in range(2)]
        self.pt = pts if pts is not None else [kb.ps([128, 512], BF16, "npt%d" % i) for i in range(2)]
        self.i = 0
        self.nbuf = nbuf

    def load(self, src_ap, q="sp"):
        kb = self.kb
        b = self.i % self.nbuf
        xt = self.xt[b]
        kb.dma(q, xt[:], src_ap, writes=[xt])
        return xt

    def run(self, src_ap, dst, dst_sl, q="sp", xt=None, gB=None):
        kb = self.kb
        if xt is None:
            xt = self.load(src_ap, q)
        k = self.i % 2
        self.i += 1
        ss, rs, xn = self.ss[k], self.rs[k], self.xn[k]
        kb.op("act", lambda e: e.activation(out=self.sq[:], in_=xt[:], func=AF.Square, accum_out=ss[:]),
              reads=[xt], writes=[self.sq, ss])
        kb.op("act", lambda e: e.activation(out=rs[:], in_=ss[:], func=AF.Sqrt, bias=EPS, scale=1.0 / D),
              reads=[ss], writes=[rs])
        kb.op("dve", lambda e: e.reciprocal(out=rs[:], in_=rs[:]), reads=[rs], writes=[rs])
        kb.op("dve", lambda e: e.tensor_scalar_mul(out=xn[:], in0=xt[:], scalar1=rs[:, 0:1]),
              reads=[xt, rs], writes=[xn])
        for g in range(4):
            pt = self.pt[g % 2]
            ptv = bf16_view(pt)
            for j in range(4):
                c = g * 4 + j
                kb.op("pe", lambda e: e.transpose(out=ptv[:, j * 128:(j + 1) * 128], in_=xn[:, c * 128:(c + 1) * 128],
                                                  identity=self.ident[:]),
                      reads=[xn, self.ident], writes=[pt.sub(j)])
            eng = "act" if g % 2 == 0 else "dve"
            o = dst[:, g * 4:(g + 1) * 4, dst_sl]
            i_ = ptv[:, 0:512].rearrange("p (a b) -> p a b", a=4)
            if gB is not None:
                kb.op("dve", lambda e: e.tensor_tensor(out=o, in0=i_, in1=gB[:, g * 4:(g + 1) * 4, :], op=ALU.mult),
                      reads=[pt, gB], writes=[dst.sub(("n", g, dst_sl.start))])
            elif eng == "act":
                kb.op("act", lambda e: e.copy(out=o, in_=i_), reads=[pt], writes=[dst.sub(("n", g, dst_sl.start))])
            else:
                kb.op("dve", lambda e: e.tensor_copy(out=o, in_=i_), reads=[pt], writes=[dst.sub(("n", g, dst_sl.start))])
        return xt


_eps_cache = {}


def EPS_AP(kb):
    if id(kb) not in _eps_cache:
        t = kb.sb([128, 1], F32, "epsc")
        kb.op("pool", lambda e: e.memset(t[:], EPS), writes=[t])
        _eps_cache[id(kb)] = t
    t = _eps_cache[id(kb)]
    return t[:, 0:1]


def load_weight_bf16(kb, w_ap, rows, cols, name, gain=None, stage=None, q="sp", col_off=0):
    rc = rows // 128
    wb = kb.sb([128, rc, cols], BF16, name)
    if stage is None:
        stage = [kb.sb([128, 512], F32, name + "_st%d" % i) for i in range(3)]
    n = 0
    for c in range(rc):
        for j0 in range(0, cols, 512):
            jw = min(512, cols - j0)
            st = stage[n % len(stage)]
            kb.dma(q if n % 2 == 0 else "pool", st[:, :jw], w_ap[c * 128:(c + 1) * 128, col_off + j0:col_off + j0 + jw], writes=[st])
            if gain is not None:
                kb.op("dve", lambda e: e.tensor_scalar_mul(out=wb[:, c, j0:j0 + jw], in0=st[:, :jw], scalar1=gain[:, c:c + 1]),
                      reads=[st, gain], writes=[wb.sub((c, j0))])
            else:
                if n % 2 == 0:
                    kb.op("dve", lambda e: e.tensor_copy(out=wb[:, c, j0:j0 + jw], in_=st[:, :jw]), reads=[st], writes=[wb.sub((c, j0))])
                else:
                    kb.op("act", lambda e: e.copy(out=wb[:, c, j0:j0 + jw], in_=st[:, :jw]), reads=[st], writes=[wb.sub((c, j0))])
            n += 1
    return wb, stage


def load_gain(kb, g_ap, n, name, q="sp"):
    t = kb.sb([128, n // 128], F32, name)
    kb.dma(q, t[:], g_ap.rearrange("(c p) -> p c", p=128), writes=[t], allow_slow_non_contiguous=True)
    return t


def build_p1(T):
    kb = KB()
    NT = T // 128
    x = kb.dram("x", [T + 128, D], F32, "ExternalInput")
    g = kb.dram("g", [D], F32, "ExternalInput")
    w = kb.dram("w", [D, 4096], F32, "ExternalInput")
    pw = kb.dram("pw", [4, 256, 256], F32, "ExternalInput")
    psc = kb.dram("psc", [1024], F32, "ExternalInput")
    fm = kb.dram("fm", [3, 4, 128, 128], F32, "ExternalInput")
    qkT = kb.dram("qkT", [2048, T], BF16, "ExternalOutput")
    v = kb.dram("v", [T, 1024], BF16, "ExternalOutput")
    obT = kb.dram("obT", [1024, T], BF16, "ExternalOutput")

    ident = emit_identity(kb)
    gain = load_gain(kb, g, D, "gain")
    nt = NormT(kb, ident)
    wb = kb.sb([128, NCH, 2048], BF16, "wb")
    stage = None

    def load_w(col_off):
        nonlocal stage
        n = 0
        if stage is None:
            stage = [kb.sb([128, 512], F32, "wst%d" % i) for i in range(3)]
        for c in range(NCH):
            for j0 in range(0, 2048, 512):
                st = stage[n % 3]
                kb.dma("sp" if n % 2 == 0 else "pool", st[:], w[c * 128:(c + 1) * 128, col_off + j0:col_off + j0 + 512], writes=[st])
                sc = 128 ** -0.5 if (col_off == 0 and j0 < 1024) else 1.0
                kb.op("dve", lambda e: e.tensor_scalar(out=wb[:, c, j0:j0 + 512], in0=st[:], scalar1=gain[:, c:c + 1], scalar2=float(sc),
                                                       op0=ALU.mult, op1=ALU.mult),
                      reads=[st, gain], writes=[wb.sub((c, j0))])
                n += 1

    load_w(0)
    hnT = [kb.sb([128, NCH, 512], BF16, "hnT%d" % i) for i in range(2)]
    pA = [kb.ps([128, 512], F32, "pA%d" % i) for i in range(2)]
    oA = [kb.sb([128, 512], BF16, "oA%d" % i) for i in range(3)]
    n_o = 0
    for gi in range(T // 512):
        h = hnT[gi % 2]
        for j in range(4):
            r0 = 128 + gi * 512 + j * 128
            nt.run(x[r0:r0 + 128, :], h, slice(j * 128, (j + 1) * 128))
        for cb in range(16):
            p = pA[cb % 2]
            for c in range(NCH):
                kb.op("pe", lambda e: e.matmul(p[:], lhsT=wb[:, c, cb * 128:(cb + 1) * 128], rhs=h[:, c, :],
                                               start=(c == 0), stop=(c == NCH - 1)),
                      reads=[wb, h], writes=[p])
            o = oA[n_o % 3]
            n_o += 1
            if cb % 2 == 0:
                kb.op("act", lambda e: e.copy(out=o[:], in_=p[:]), reads=[p], writes=[o])
            else:
                kb.op("dve", lambda e: e.tensor_copy(out=o[:], in_=p[:]), reads=[p], writes=[o])
            kb.dma("sp", qkT[cb * 128:(cb + 1) * 128, gi * 512:(gi + 1) * 512], o[:], reads=[o], is_output=True)

    load_w(2048)
    fmt = kb.sb([128, 12, 128], F32, "fmt")
    kb.dma("sp", fmt[:], fm.rearrange("a g s t -> s (a g) t"), writes=[fmt], allow_slow_non_contiguous=True)
    pwb, _ = load_weight_bf16(kb, pw.rearrange("g c d -> (g c) d"), 1024, 256, "pwb", stage=stage)
    pscale = load_gain(kb, psc, 1024, "pscale")
    hB = [kb.sb([128, NCH, 128], BF16, "hB%d" % i) for i in range(2)]
    pV = [kb.ps([128, 512], F32, "pV%d" % i) for i in range(2)]
    vo = [kb.sb([128, 1024], BF16, "vo%d" % i) for i in range(2)]
    ut = [kb.sb([128, 1024], F32, "ut%d" % i) for i in range(2)]
    pD = [kb.ps([128, 512], F32, "pD%d" % i) for i in range(2)]
    dT = [kb.sb([128, 8, 128], BF16, "dT%d" % i) for i in range(2)]
    ob = [kb.sb([128, 8, 512], BF16, "ob%d" % i) for i in range(2)]
    for ti in range(-1, NT):
        r0 = 128 + ti * 128
        k = (ti + 1) % 2
        h = hB[k]
        nt.run(x[r0:r0 + 128, :], h, slice(0, 128))
        u_cur, u_prev = ut[k], ut[1 - k]
        for half in range(4):
            if ti < 0 and half < 2:
                continue
            p = pV[half % 2]
            for c in range(NCH):
                kb.op("pe", lambda e: e.matmul(p[:], lhsT=h[:, c, :], rhs=wb[:, c, half * 512:(half + 1) * 512],
                                               start=(c == 0), stop=(c == NCH - 1)), reads=[h, wb], writes=[p])
            if half < 2:
                dst = vo[ti % 2]
                kb.op("act", lambda e: e.copy(out=dst[:, half * 512:(half + 1) * 512], in_=p[:]), reads=[p], writes=[dst.sub(half)])
            else:
                hh = half - 2
                kb.op("dve", lambda e: e.tensor_copy(out=u_cur[:, hh * 512:(hh + 1) * 512], in_=p[:]), reads=[p], writes=[u_cur.sub(hh)])
        if ti < 0:
            continue
        kb.dma("pool", v[ti * 128:(ti + 1) * 128, :], vo[ti % 2][:], reads=[vo[ti % 2]], is_output=True)
        d = dT[ti % 2]
        for half in range(2):
            p = pD[half]
            for j in range(4):
                cc = half * 4 + j
                gq = cc // 2
                fcur = fmt[:, (0 if ti == 0 else 4) + gq, :]
                fprev = fmt[:, 8 + gq, :]
                kb.op("pe", lambda e: e.matmul(p[:, j * 128:(j + 1) * 128], lhsT=u_cur[:, cc * 128:(cc + 1) * 128], rhs=fcur,
                                               start=True, stop=False), reads=[u_cur, fmt], writes=[p.sub(j)])
                kb.op("pe", lambda e: e.matmul(p[:, j * 128:(j + 1) * 128], lhsT=u_prev[:, cc * 128:(cc + 1) * 128], rhs=fprev,
                                               start=False, stop=True), reads=[u_prev, fmt], writes=[p.sub(j)])
            kb.op("act" if half == 0 else "dve",
                  (lambda e: e.copy(out=d[:, half * 4:(half + 1) * 4, :], in_=p[:].rearrange("p (a b) -> p a b", a=4))) if half == 0 else
                  (lambda e: e.tensor_copy(out=d[:, half * 4:(half + 1) * 4, :], in_=p[:].rearrange("p (a b) -> p a b", a=4))),
                  reads=[p], writes=[d.sub(half)])
        o = ob[(ti // 4) % 2]
        tj = ti % 4
        for half in range(2):
            p = pD[half]
            for j in range(4):
                oc = half * 4 + j
                gq, dh = oc // 2, oc % 2
                for cc in range(2):
                    kb.op("pe", lambda e: e.matmul(p[:, j * 128:(j + 1) * 128], lhsT=pwb[:, gq * 2 + cc, dh * 128:(dh + 1) * 128],
                                                   rhs=d[:, gq * 2 + cc, :], start=(cc == 0), stop=(cc == 1)),
                          reads=[pwb, d], writes=[p.sub(j)])
                kb.op("dve", lambda e: e.tensor_scalar_mul(out=o[:, oc, tj * 128:(tj + 1) * 128], in0=p[:, j * 128:(j + 1) * 128],
                                                           scalar1=pscale[:, oc:oc + 1]),
                      reads=[p.sub(j), pscale], writes=[o.sub((oc, tj))])
        if tj == 3:
            t0 = (ti // 4) * 512
            kb.dma("pool", obT[:, t0:t0 + 512].rearrange("(c p) t -> p c t", p=128), o[:], reads=[o], is_output=True)
    return kb.finish()


def build_p2(S, NU=2):
    kb = KB()
    NG = S // 512
    NB = S // 128
    qT = kb.dram("qT", [NU, 128, S], BF16, "ExternalInput")
    kT = kb.dram("kT", [NU, 128, S], BF16, "ExternalInput")
    v = kb.dram("v", [NU, S, 128], BF16, "ExternalInput")
    oT = kb.dram("oT", [NU, 128, S], BF16, "ExternalOutput")

    ntri = kb.sb([128, 128], F32, "ntri")
    kb.op("pool", lambda e: e.memset(ntri[:], -1.0), writes=[ntri])
    kb.op("pool", lambda e: e.affine_select(out=ntri[:], in_=ntri[:], pattern=[[-1, 128]], compare_op=ALU.is_ge,
                                             fill=0.0, base=0, channel_multiplier=1), reads=[ntri], writes=[ntri])
    nones = kb.sb([128, 128], F32, "nones")
    kb.op("pool", lambda e: e.memset(nones[:], -1.0), writes=[nones])

    qs = kb.sb([128, S], BF16, "qs")
    ks = kb.sb([128, S], BF16, "ks")
    vs = kb.sb([128, NB, 128], BF16, "vs")
    pz = [kb.ps([128, 512], F32, "pz%d" % i) for i in range(2)]
    px = [kb.ps([128, 512], F32, "px%d" % i) for i in range(2)]
    po = [kb.ps([128, 512], F32, "po%d" % i) for i in range(2)]
    et = [kb.sb([128, 512], F32, "et%d" % i) for i in range(2)]
    spt = [kb.sb([128, 512], F32, "spt%d" % i) for i in range(3)]
    wt = [kb.sb([128, 512], BF16, "wt%d" % i) for i in range(3)]
    srun = kb.sb([128, 512], F32, "srun")
    ot = [kb.sb([128, 512], BF16, "ot%d" % i) for i in range(2)]
    n = 0
    for u in range(NU):
        nq = 4 if S >= 2048 else 1
        for i in range(nq):
            sl = slice(i * S // nq, (i + 1) * S // nq)
            kb.dma("sp", qs[:, sl], qT[u, :, sl], writes=[qs.sub(i)] if u == 0 else [qs])
            kb.dma("pool", ks[:, sl], kT[u, :, sl], writes=[ks.sub(i)] if u == 0 else [ks])
        nv = max(1, NB // 16)
        for i in range(nv):
            j0, j1 = i * NB // nv, (i + 1) * NB // nv
            kb.dma("sp" if i % 2 == 0 else "pool", vs[:, j0:j1, :], v[u, j0 * 128:j1 * 128, :].rearrange("(j p) d -> p j d", p=128),
                   writes=[vs.sub(i)] if u == 0 else [vs])
        pairs = []
        for G in range(NG):
            jl = list(range(4 * G + 3, -1, -1))
            for idx, j in enumerate(jl):
                pairs.append((G, j, idx == 0, idx == len(jl) - 1))
        st1 = {}

        def stage1(pi):
            G, j, first, last = pairs[pi]
            n = n0 + pi
            qg = qs[:, G * 512:(G + 1) * 512]
            z, e_t, sp_t = pz[n % 2], et[n % 2], spt[n % 3]
            kblk = ks[:, j * 128:(j + 1) * 128]
            kb.op("pe", lambda e: e.matmul(z[:], lhsT=kblk, rhs=qg, start=True, stop=True), reads=[qs, ks], writes=[z])
            kb.op("act", lambda e: e.activation(out=e_t[:], in_=z[:], func=AF.Exp), reads=[z], writes=[e_t])
            kb.op("act", lambda e: e.activation(out=sp_t[:], in_=e_t[:], func=AF.Ln, bias=1.0), reads=[e_t], writes=[sp_t])
            if j >= 4 * G:
                kb.op("pool", lambda e: e.affine_select(out=sp_t[:], in_=sp_t[:], pattern=[[1, 512]], compare_op=ALU.is_gt,
                                                         fill=0.0, base=512 * G - 128 * j, channel_multiplier=-1),
                      reads=[sp_t], writes=[sp_t])

        def stage2(pi):
            G, j, first, last = pairs[pi]
            n = n0 + pi
            qg = qs[:, G * 512:(G + 1) * 512]
            oacc = po[G % 2]
            xx, sp_t, w_t = px[n % 2], spt[n % 3], wt[n % 3]
            kblk = ks[:, j * 128:(j + 1) * 128]
            kb.op("pe", lambda e: e.matmul(xx[:], lhsT=kblk, rhs=qg, start=True, stop=False), reads=[qs, ks], writes=[xx])
            kb.op("pe", lambda e: e.matmul(xx[:], lhsT=ntri[:], rhs=sp_t[:], start=False, stop=first), reads=[ntri, sp_t], writes=[xx])
            if not first:
                kb.op("pe", lambda e: e.matmul(xx[:], lhsT=nones[:], rhs=srun[:], start=False, stop=True), reads=[nones, srun], writes=[xx])
            kb.op("act", lambda e: e.activation(out=w_t[:], in_=xx[:], func=AF.Exp), reads=[xx], writes=[w_t])
            if j >= 4 * G:
                kb.op("pool", lambda e: e.affine_select(out=w_t[:], in_=w_t[:], pattern=[[1, 512]], compare_op=ALU.is_gt,
                                                         fill=0.0, base=512 * G - 128 * j, channel_multiplier=-1),
                      reads=[w_t], writes=[w_t])
            if not last:
                if first:
                    kb.op("dve", lambda e: e.tensor_copy(out=srun[:], in_=sp_t[:]), reads=[sp_t], writes=[srun])
                else:
                    kb.op("dve", lambda e: e.tensor_add(out=srun[:], in0=srun[:], in1=sp_t[:]), reads=[sp_t, srun], writes=[srun])
            kb.op("pe", lambda e: e.matmul(oacc[:], lhsT=vs[:, j, :], rhs=w_t[:], start=first, stop=last), reads=[vs, w_t], writes=[oacc])
            if last:
                o_t = ot[G % 2]
                kb.op("dve", lambda e: e.tensor_copy(out=o_t[:], in_=oacc[:]), reads=[oacc], writes=[o_t])
                kb.dma("sp", oT[u, :, G * 512:(G + 1) * 512], o_t[:], reads=[o_t], is_output=True)

        n0 = n
        stage1(0)
        for pi in range(len(pairs)):
            if pi + 1 < len(pairs):
                stage1(pi + 1)
            stage2(pi)
        n += len(pairs)
    return kb.finish()


def load_w_scaled(kb, w_ap, rows, cols, name, gain, scale, stage, dst=None, col_off=0):
    rc = rows // 128
    wb = dst if dst is not None else kb.sb([128, rc, cols], BF16, name)
    n = 0
    for c in range(rc):
        for j0 in range(0, cols, 512):
            jw = min(512, cols - j0)
            st = stage[n % len(stage)]
            kb.dma("sp" if n % 2 == 0 else "pool", st[:, :jw], w_ap[c * 128:(c + 1) * 128, col_off + j0:col_off + j0 + jw], writes=[st])
            kb.op("dve", lambda e: e.tensor_scalar(out=wb[:, c, j0:j0 + jw], in0=st[:, :jw], scalar1=gain[:, c:c + 1], scalar2=float(scale),
                                                   op0=ALU.mult, op1=ALU.mult), reads=[st, gain], writes=[wb.sub((c, j0))])
            n += 1
    return wb


def build_p3a(T):
    kb = KB()
    NT = T // 128
    oT = kb.dram("oT", [2048, T], BF16, "ExternalInput")
    hin = kb.dram("hin", [T, D], F32, "ExternalInput")
    wout = kb.dram("wout", [D, D], F32, "ExternalInput")
    gx = kb.dram("gx", [D], F32, "ExternalInput")
    mem = kb.dram("mem", [256, D], F32, "ExternalInput")
    gm = kb.dram("gm", [D], F32, "ExternalInput")
    wq = kb.dram("wq", [D, 512], F32, "ExternalInput")
    wkv = kb.dram("wkv", [D, 1024], F32, "ExternalInput")
    wo = kb.dram("wo", [512, D], F32, "ExternalInput")
    hout = kb.dram("hout", [T, D], F32, "ExternalOutput")

    ident = emit_identity(kb)
    ones_b = kb.sb([128, 128], BF16, "ones_b")
    kb.op("pool", lambda e: e.memset(ones_b[:], 1.0), writes=[ones_b])
    nt = NormT(kb, ident, nbuf=1)
    wout_b, stage = load_weight_bf16(kb, wout, D, D, "wout_b")
    gxg = load_gain(kb, gx, D, "gxg")
    gmg = load_gain(kb, gm, D, "gmg")
    wq_b = load_w_scaled(kb, wq, D, 512, "wq_b", gxg, 128 ** -0.5, stage)
    wo_b, _ = load_weight_bf16(kb, wo, 512, D, "wo_b", stage=stage)
    wkv_b = load_w_scaled(kb, wkv, D, 512, "wkv_b", gmg, 1.0, stage)

    pmm = [kb.ps([128, 512], F32, "pmm%d" % i) for i in range(2)]
    pq = kb.ps([128, 512], F32, "pq")
    pl = [kb.ps([128, 512], F32, "pl%d" % i) for i in range(2)]
    po = kb.ps([128, 512], F32, "po")

    memT = kb.sb([128, NCH, 256], BF16, "memT")
    for mt in range(2):
        nt.run(mem[mt * 128:(mt + 1) * 128, :], memT, slice(mt * 128, (mt + 1) * 128))
    KT = kb.sb([128, 4, 256], BF16, "KT")
    Vs = kb.sb([128, 2, 512], BF16, "Vs")
    for hd in range(4):
        p = pmm[hd % 2]
        for c in range(NCH):
            kb.op("pe", lambda e: e.matmul(p[:, 0:256], lhsT=wkv_b[:, c, hd * 128:(hd + 1) * 128], rhs=memT[:, c, :],
                                           start=(c == 0), stop=(c == NCH - 1)), reads=[wkv_b, memT], writes=[p])
        kb.op("act", lambda e: e.copy(out=KT[:, hd, :], in_=p[:, 0:256]), reads=[p], writes=[KT.sub(hd)])
    load_w_scaled(kb, wkv, D, 512, "wkv_b", gmg, 1.0, stage, dst=wkv_b, col_off=512)
    for mc in range(2):
        p = pmm[mc % 2]
        for c in range(NCH):
            kb.op("pe", lambda e: e.matmul(p[:], lhsT=memT[:, c, mc * 128:(mc + 1) * 128], rhs=wkv_b[:, c, 0:512],
                                           start=(c == 0), stop=(c == NCH - 1)), reads=[wkv_b, memT], writes=[p])
        kb.op("act", lambda e: e.copy(out=Vs[:, mc, :], in_=p[:]), reads=[p], writes=[Vs.sub(mc)])

    oTs = [kb.sb([128, NCH, 512], BF16, "oTs%d" % i) for i in range(1)]
    xin = [kb.sb([128, D], F32, "xin%d" % i) for i in range(1)]
    ht = [kb.sb([128, D], F32, "ht%d" % i) for i in range(2)]
    hnT = [kb.sb([128, NCH, 128], BF16, "hnT%d" % i) for i in range(1)]
    qxT = kb.sb([128, 4, 128], BF16, "qxT")
    pT = kb.sb([128, 8, 128], BF16, "pT")
    rden = kb.sb([128, 512], F32, "rden")
    oxT = kb.sb([128, 4, 128], BF16, "oxT")
    for ti in range(NT):
        gi, tj = ti // 4, ti % 4
        og = oTs[0]
        if tj == 0:
            kb.dma("pool", og[:], oT[:, gi * 512:(gi + 1) * 512].rearrange("(c p) t -> p c t", p=128), writes=[og])
        xt = xin[0]
        kb.dma("sp", xt[:], hin[ti * 128:(ti + 1) * 128, :], writes=[xt])
        h = ht[ti % 2]
        for cg in range(4):
            p = pmm[cg % 2]
            for c in range(NCH):
                kb.op("pe", lambda e: e.matmul(p[:], lhsT=og[:, c, tj * 128:(tj + 1) * 128], rhs=wout_b[:, c, cg * 512:(cg + 1) * 512],
                                               start=(c == 0), stop=(c == NCH - 1)), reads=[og, wout_b], writes=[p])
            kb.op("dve", lambda e: e.tensor_tensor(out=h[:, cg * 512:(cg + 1) * 512], in0=p[:], in1=xt[:, cg * 512:(cg + 1) * 512], op=ALU.add),
                  reads=[p, xt], writes=[h.sub(cg)])
        hn = hnT[0]
        nt.run(None, hn, slice(0, 128), xt=h)
        for hd in range(4):
            for c in range(NCH):
                kb.op("pe", lambda e: e.matmul(pq[:, hd * 128:(hd + 1) * 128], lhsT=wq_b[:, c, hd * 128:(hd + 1) * 128], rhs=hn[:, c, :],
                                               start=(c == 0), stop=(c == NCH - 1)), reads=[wq_b, hn], writes=[pq])
        kb.op("act", lambda e: e.copy(out=qxT[:], in_=pq[:].rearrange("p (a b) -> p a b", a=4)), reads=[pq], writes=[qxT])
        for hd in range(4):
            for mc in range(2):
                b = hd * 2 + mc
                kb.op("pe", lambda e: e.matmul(pl[b // 4][:, (b % 4) * 128:(b % 4 + 1) * 128], lhsT=KT[:, hd, mc * 128:(mc + 1) * 128],
                                               rhs=qxT[:, hd, :], start=True, stop=True), reads=[KT, qxT], writes=[pl[b // 4]])
        for k2 in range(2):
            kb.op("act", lambda e: e.activation(out=pT[:, k2 * 4:(k2 + 1) * 4, :], in_=pl[k2][:].rearrange("p (a b) -> p a b", a=4), func=AF.Exp),
                  reads=[pl[k2]], writes=[pT.sub(k2)])
        for hd in range(4):
            for mc in range(2):
                kb.op("pe", lambda e: e.matmul(po[:, hd * 128:(hd + 1) * 128], lhsT=Vs[:, mc, hd * 128:(hd + 1) * 128], rhs=pT[:, hd * 2 + mc, :],
                                               start=(mc == 0), stop=(mc == 1)), reads=[Vs, pT], writes=[po])
        for hd in range(4):
            for mc in range(2):
                kb.op("pe", lambda e: e.matmul(pq[:, hd * 128:(hd + 1) * 128], lhsT=ones_b[:], rhs=pT[:, hd * 2 + mc, :],
                                               start=(mc == 0), stop=(mc == 1)), reads=[ones_b, pT], writes=[pq])
        kb.op("dve", lambda e: e.reciprocal(out=rden[:], in_=pq[:]), reads=[pq], writes=[rden])
        kb.op("dve", lambda e: e.tensor_tensor(out=oxT[:], in0=po[:].rearrange("p (a b) -> p a b", a=4),
                                               in1=rden[:].rearrange("p (a b) -> p a b", a=4), op=ALU.mult), reads=[po, rden], writes=[oxT])
        for cg in range(4):
            p = pmm[cg % 2]
            for hd in range(4):
                kb.op("pe", lambda e: e.matmul(p[:], lhsT=oxT[:, hd, :], rhs=wo_b[:, hd, cg * 512:(cg + 1) * 512],
                                               start=(hd == 0), stop=(hd == 3)), reads=[oxT, wo_b], writes=[p])
            kb.op("dve", lambda e: e.tensor_tensor(out=h[:, cg * 512:(cg + 1) * 512], in0=p[:], in1=h[:, cg * 512:(cg + 1) * 512], op=ALU.add),
                  reads=[p, h.sub(cg)], writes=[h.sub(cg)])
        kb.dma("pool", hout[ti * 128:(ti + 1) * 128, :], h[:], reads=[h], is_output=True)
    return kb.finish()


def build_cast(N, CH=4096):
    kb = KB()
    a = kb.dram("a", [128, N], F32, "ExternalInput")
    o = kb.dram("o", [128, N], BF16, "ExternalOutput")
    st = [kb.sb([128, CH], F32, "cs%d" % i) for i in range(3)]
    ob = [kb.sb([128, CH], BF16, "co%d" % i) for i in range(3)]
    for i in range(N // CH):
        s_, o_ = st[i % 3], ob[i % 3]
        kb.dma("sp", s_[:], a[:, i * CH:(i + 1) * CH], writes=[s_])
        if i % 2 == 0:
            kb.op("dve", lambda e: e.tensor_copy(out=o_[:], in_=s_[:]), reads=[s_], writes=[o_])
        else:
            kb.op("act", lambda e: e.copy(out=o_[:], in_=s_[:]), reads=[s_], writes=[o_])
        kb.dma("pool", o[:, i * CH:(i + 1) * CH], o_[:], reads=[o_], is_output=True)
    return kb.finish()


NEG = -1.0e30


def build_p3b(T, final=False, NE=16384):
    kb = KB()
    NT = T // 128
    NEG_ = NE // 512
    hin = kb.dram("hin", [T, D], F32, "ExternalInput")
    gf = kb.dram("gf", [D], F32, "ExternalInput")
    wqb = kb.dram("wqb", [4, 128, NCH, 512], BF16, "ExternalInput")
    skT = kb.dram("skT", [128, 16, 128], F32, "ExternalInput")
    uT = kb.dram("uT", [NE // 512, 128, NCH, 512], BF16, "ExternalInput")
    vv = kb.dram("vv", [NE // 512, 128, 4, D], BF16, "ExternalInput")
    gfin = kb.dram("gfin", [D], F32, "ExternalInput")
    hout = kb.dram("hout", [T, D], F32, "ExternalOutput")

    ident = emit_identity(kb)
    B = [kb.ps([128, 512], F32, "B%d" % i) for i in range(8)]
    nt = NormT(kb, ident, nbuf=1, pts=[B[4], B[5]])
    gfg = load_gain(kb, gf, D, "gfg")
    gB = kb.sb([128, NCH, 128], F32, "gB")
    for c in range(NCH):
        kb.op("dve", lambda e: e.tensor_copy(out=gB[:, c, :], in_=gfg[:, c:c + 1].to_broadcast([128, 128])), reads=[gfg], writes=[gB.sub(c)])
    skst = kb.sb([128, 16, 128], F32, "skst")
    kb.dma("sp", skst[:], skT, writes=[skst])
    skb = kb.sb([128, 16, 128], BF16, "skb")
    kb.op("dve", lambda e: e.tensor_copy(out=skb[:], in_=skst[:]), reads=[skst], writes=[skb])
    if final:
        gfb = kb.sb([128, D], F32, "gfb")
        kb.dma("sp", gfb[:], gfin.partition_broadcast(128), writes=[gfb])

    G = kb.sb([128, NE], BF16, "G")
    sub = skst
    sub2 = kb.sb([128, 16, 128], F32, "sub2")
    m8 = kb.sb([128, 16, 16], F32, "m8")
    cand = kb.sb([128, 256], F32, "cand")
    cand2 = kb.sb([128, 256], F32, "cand2")
    tv = kb.sb([128, 8, 16], F32, "tv")
    e16 = kb.sb([128, 16], F32, "e16")
    negm = kb.sb([128, 8], F32, "negm")
    Z = kb.sb([128, 8], F32, "Z")
    nb = kb.sb([128, 8], F32, "nb")
    St = [kb.sb([128, 16, 128], F32, "St%d" % i) for i in range(2)]
    Et = kb.sb([128, 2048], BF16, "Et")
    Mt = kb.sb([128, 2048], BF16, "Mt")
    hnT = kb.sb([128, NCH, 128], BF16, "hnT")
    qTs = kb.sb([128, 16, 128], BF16, "qTs")
    ug = [kb.sb([128, NCH, 512], BF16, "ug%d" % i) for i in range(2)]
    vg = [kb.sb([128, 4, D], BF16, "vg%d" % i) for i in range(2)]
    at = [kb.sb([128, 512], BF16, "at%d" % i) for i in range(2)]
    gat = [kb.sb([128, 512], BF16, "gat%d" % i) for i in range(2)]
    gaT = [kb.sb([128, 4, 128], BF16, "gaT%d" % i) for i in range(2)]
    ss = kb.sb([128, 1], F32, "fss")
    rs = kb.sb([128, 1], F32, "frs")
    nug = 0
    for ti in range(NT):
        xt = nt.run(hin[ti * 128:(ti + 1) * 128, :], hnT, slice(0, 128), gB=gB)
        for grp in range(4):
            w_ = ug[nug % 2]
            nug += 1
            kb.dma("sp", w_[:], wqb[grp], writes=[w_])
            pq = B[6 + grp % 2]
            for j in range(4):
                for c in range(NCH):
                    kb.op("pe", lambda e: e.matmul(pq[:, j * 128:(j + 1) * 128], lhsT=w_[:, c, j * 128:(j + 1) * 128], rhs=hnT[:, c, :],
                                                   start=(c == 0), stop=(c == NCH - 1)), reads=[w_, hnT], writes=[pq])
            kb.op("act", lambda e: e.copy(out=qTs[:, grp * 4:(grp + 1) * 4, :], in_=pq[:].rearrange("p (a b) -> p a b", a=4)),
                  reads=[pq], writes=[qTs.sub(grp)])
        for b4 in range(4):
            ps_ = B[b4]
            for j in range(4):
                hp = b4 * 4 + j
                kb.op("pe", lambda e: e.matmul(ps_[:, j * 128:(j + 1) * 128], lhsT=qTs[:, hp, :], rhs=skb[:, hp, :], start=True, stop=True),
                      reads=[qTs, skb], writes=[ps_])
            if b4 % 2 == 0:
                kb.op("act", lambda e: e.copy(out=sub[:, b4 * 4:(b4 + 1) * 4, :], in_=ps_[:].rearrange("p (a b) -> p a b", a=4)), reads=[ps_], writes=[sub.sub(b4)])
            else:
                kb.op("dve", lambda e: e.tensor_copy(out=sub[:, b4 * 4:(b4 + 1) * 4, :], in_=ps_[:].rearrange("p (a b) -> p a b", a=4)), reads=[ps_], writes=[sub.sub(b4)])
        for hp in range(16):
            kb.op("dve", lambda e: e.max(out=m8[:, hp, 0:8], in_=sub[:, hp, :]), reads=[sub], writes=[m8.sub(hp)])
            kb.op("dve", lambda e: e.match_replace(out=sub2[:, hp, :], in_to_replace=m8[:, hp, 0:8], in_values=sub[:, hp, :], imm_value=NEG),
                  reads=[sub, m8.sub(hp)], writes=[sub2.sub(hp)])
            kb.op("dve", lambda e: e.max(out=m8[:, hp, 8:16], in_=sub2[:, hp, :]), reads=[sub2.sub(hp)], writes=[m8.sub(hp)])
        for h in range(8):
            kb.op("dve", lambda e: e.tensor_tensor(out=cand[:].rearrange("p (a b) -> p a b", a=16),
                                                   in0=m8[:, 2 * h, :].unsqueeze(2).to_broadcast([128, 16, 16]),
                                                   in1=m8[:, 2 * h + 1, :].unsqueeze(1).to_broadcast([128, 16, 16]), op=ALU.add),
                  reads=[m8], writes=[cand])
            kb.op("dve", lambda e: e.max(out=tv[:, h, 0:8], in_=cand[:]), reads=[cand], writes=[tv.sub(h)])
            kb.op("dve", lambda e: e.match_replace(out=cand2[:], in_to_replace=tv[:, h, 0:8], in_values=cand[:], imm_value=NEG),
                  reads=[cand, tv.sub(h)], writes=[cand2])
            kb.op("dve", lambda e: e.max(out=tv[:, h, 8:16], in_=cand2[:]), reads=[cand2], writes=[tv.sub(h)])
        kb.op("dve", lambda e: e.tensor_scalar_mul(out=negm[:], in0=tv[:, :, 0], scalar1=-1.0), reads=[tv], writes=[negm])
        for h in range(8):
            kb.op("act", lambda e: e.activation(out=e16[:], in_=tv[:, h, :], func=AF.Exp, bias=negm[:, h:h + 1], accum_out=Z[:, h:h + 1]),
                  reads=[tv, negm], writes=[e16, Z.sub(h)])
        kb.op("act", lambda e: e.activation(out=Z[:], in_=Z[:], func=AF.Ln), reads=[Z], writes=[Z])
        kb.op("dve", lambda e: e.tensor_sub(out=nb[:], in0=negm[:], in1=Z[:]), reads=[negm, Z], writes=[nb])
        npc = 0
        for h in range(8):
            for pc in range(8):
                S_ = St[npc % 2]
                npc += 1
                kb.op("pool", lambda e: e.tensor_tensor(out=S_[:], in0=sub[:, 2 * h, pc * 16:(pc + 1) * 16].unsqueeze(2).to_broadcast([128, 16, 128]),
                                                        in1=sub[:, 2 * h + 1, :].unsqueeze(1).to_broadcast([128, 16, 128]), op=ALU.add),
                      reads=[sub], writes=[S_])
                Sf = S_[:].rearrange("p a b -> p (a b)")
                kb.op("act", lambda e: e.activation(out=Et[:], in_=Sf, func=AF.Exp, bias=nb[:, h:h + 1]), reads=[S_, nb], writes=[Et])
                Gp = G[:, pc * 2048:(pc + 1) * 2048]
                if h == 0:
                    kb.op("dve", lambda e: e.scalar_tensor_tensor(out=Gp, in0=Sf, scalar=tv[:, h, 15:16], in1=Et[:], op0=ALU.is_ge, op1=ALU.mult),
                          reads=[S_, tv, Et], writes=[G.sub(pc)])
                else:
                    kb.op("dve", lambda e: e.scalar_tensor_tensor(out=Mt[:], in0=Sf, scalar=tv[:, h, 15:16], in1=Et[:], op0=ALU.is_ge, op1=ALU.mult),
                          reads=[S_, tv, Et], writes=[Mt])
                    kb.op("dve", lambda e: e.tensor_tensor(out=Gp, in0=Gp, in1=Mt[:], op=ALU.add), reads=[Mt, G.sub(pc)], writes=[G.sub(pc)])
        for eg in range(NEG_):
            u_ = ug[nug % 2]
            nug += 1
            v_ = vg[eg % 2]
            kb.dma("sp", u_[:], uT[eg], writes=[u_])
            kb.dma("pool", v_[:], vv[eg], writes=[v_])
            pa = B[4 + eg % 2]
            for c in range(NCH):
                kb.op("pe", lambda e: e.matmul(pa[:], lhsT=hnT[:, c, :], rhs=u_[:, c, :], start=(c == 0), stop=(c == NCH - 1)),
                      reads=[hnT, u_], writes=[pa])
            a_ = at[eg % 2]
            g_ = gat[eg % 2]
            kb.op("act", lambda e: e.activation(out=a_[:], in_=pa[:], func=AF.Gelu), reads=[pa], writes=[a_])
            kb.op("dve", lambda e: e.tensor_tensor(out=g_[:], in0=a_[:], in1=G[:, eg * 512:(eg + 1) * 512], op=ALU.mult),
                  reads=[a_, G], writes=[g_])
            ptb = B[6 + eg % 2]
            ptv = bf16_view(ptb)
            for j in range(4):
                kb.op("pe", lambda e: e.transpose(out=ptv[:, j * 128:(j + 1) * 128], in_=g_[:, j * 128:(j + 1) * 128], identity=ident[:]),
                      reads=[g_, ident], writes=[ptb])
            gT = gaT[eg % 2]
            kb.op("act", lambda e: e.copy(out=gT[:], in_=ptv[:, 0:512].rearrange("p (a b) -> p a b", a=4)), reads=[ptb], writes=[gT])
            for cg in range(4):
                for j in range(4):
                    kb.op("pe", lambda e: e.matmul(B[cg][:], lhsT=gT[:, j, :], rhs=v_[:, j, cg * 512:(cg + 1) * 512],
                                                   start=(eg == 0 and j == 0), stop=(eg == NEG_ - 1 and j == 3)), reads=[gT, v_], writes=[B[cg]])
        for cg in range(4):
            kb.op("dve", lambda e: e.tensor_tensor(out=xt[:, cg * 512:(cg + 1) * 512], in0=B[cg][:], in1=xt[:, cg * 512:(cg + 1) * 512], op=ALU.add),
                  reads=[B[cg], xt], writes=[xt])
        if final:
            kb.op("act", lambda e: e.activation(out=nt.sq[:], in_=xt[:], func=AF.Square, accum_out=ss[:]), reads=[xt], writes=[nt.sq, ss])
            kb.op("act", lambda e: e.activation(out=rs[:], in_=ss[:], func=AF.Sqrt, bias=EPS, scale=1.0 / D), reads=[ss], writes=[rs])
            kb.op("dve", lambda e: e.reciprocal(out=rs[:], in_=rs[:]), reads=[rs], writes=[rs])
            kb.op("dve", lambda e: e.scalar_tensor_tensor(out=xt[:], in0=xt[:], scalar=rs[:, 0:1], in1=gfb[:], op0=ALU.mult, op1=ALU.mult),
                  reads=[xt, rs, gfb], writes=[xt])
        kb.dma("pool", hout[ti * 128:(ti + 1) * 128, :], xt[:], reads=[xt], is_output=True)
    return kb.finish()


import ml_dtypes
_BF = ml_dtypes.bfloat16
_progs = {}


def _prog(key, fn, *a, **k):
    if key not in _progs:
        _progs[key] = fn(*a, **k)
    return _progs[key]


def _pool_mats(first):
    fm = np.zeros((3, 4, 128, 128), np.float32)
    s = np.arange(128)[:, None]
    t = np.arange(128)[None, :]
    for gi, win in enumerate((2, 4, 8, 16)):
        inwin = (s > t - win) & (s <= t)
        cnt_first = np.minimum(win, t + 1).astype(np.float32)
        cur = inwin / np.float32(win) - (s == t)
        cur0 = inwin / cnt_first - (s == t)
        prev = ((s - 128) > (t - win)) / np.float32(win)
        fm[0, gi] = cur0 if first else cur
        fm[1, gi] = cur
        fm[2, gi] = prev
    return fm


def _layer_tail(h_chunks, oT_chunks, layer, P, S, final):
    T = S // 4
    j = layer
    p3a = _prog(("p3a", T), build_p3a, T)
    ims = []
    for c in range(8):
        b = c // 4
        ims.append(dict(oT=oT_chunks[c], hin=h_chunks[c], wout=P["w_out"], gx=P["norm_cross"][j], mem=P["mem"][b], gm=P["norm_mem"][j],
                        wq=P["xattn_wq"][j], wkv=P["xattn_wkv"][j], wo=P["xattn_wo"][j]))
    r = run(p3a, ims)
    h1 = [np.asarray(r[c]["hout"]) for c in range(8)]
    flat = np.concatenate([np.ascontiguousarray(P["peer_u"][j].T).ravel(), P["peer_v"][j].ravel(), P["peer_wq"][j].ravel()])
    NPC = flat.size // (8 * 128)
    pc = _prog(("cast", NPC), build_cast, NPC)
    fl = flat.reshape(8, 128, NPC)
    rc = run(pc, [dict(a=fl[c]) for c in range(8)])
    fb = np.concatenate([np.asarray(rc[c]["o"]).reshape(-1) for c in range(8)])
    n_u = 16384 * D
    uTb = np.ascontiguousarray(fb[:n_u].reshape(NCH, 128, 32, 512).transpose(2, 1, 0, 3))
    vb = np.ascontiguousarray(fb[n_u:2 * n_u].reshape(32, 4, 128, D).transpose(0, 2, 1, 3))
    wqb = np.ascontiguousarray(fb[2 * n_u:].reshape(NCH, 128, 4, 512).transpose(2, 1, 0, 3))
    skT = np.ascontiguousarray(P["peer_subkeys"][j].reshape(16, 128, 128).transpose(2, 0, 1))
    p3b = _prog(("p3b", T, final), build_p3b, T, final=final)
    ims = [dict(hin=h1[c], gf=P["norm_ffn"][j], wqb=wqb, skT=skT, uT=uTb, vv=vb, gfin=P["norm_final"]) for c in range(8)]
    r = run(p3b, ims)
    return [np.asarray(r[c]["hout"]) for c in range(8)]


def _layer0(xc, P, S):
    T = S // 4
    p1 = _prog(("p1", T), build_p1, T)
    ims = []
    for c in range(8):
        b, ch = c // 4, c % 4
        halo = np.zeros((128, D), np.float32) if ch == 0 else xc[c - 1][-128:]
        ims.append(dict(x=np.concatenate([halo, xc[c]], 0), g=P["norm_mix"][0], w=P["w_in_ab"][0], pw=P["pool_w"][0],
                        psc=P["pool_scale"][0], fm=_pool_mats(ch == 0)))
    r = run(p1, ims)
    qkT = [np.asarray(r[c]["qkT"]) for c in range(8)]
    vtm = [np.asarray(r[c]["v"]) for c in range(8)]
    obT = [np.asarray(r[c]["obT"]) for c in range(8)]
    p2 = _prog(("p2", S), build_p2, S, 2)
    ims = []
    for c in range(8):
        qs, ks, vs = [], [], []
        for u in range(2):
            b, h = divmod(2 * c + u, 8)
            qs.append(np.concatenate([qkT[b * 4 + ch][h * 128:(h + 1) * 128] for ch in range(4)], 1))
            ks.append(np.concatenate([qkT[b * 4 + ch][1024 + h * 128:1024 + (h + 1) * 128] for ch in range(4)], 1))
            vs.append(np.concatenate([vtm[b * 4 + ch][:, h * 128:(h + 1) * 128] for ch in range(4)], 0))
        ims.append(dict(qT=np.stack(qs), kT=np.stack(ks), v=np.stack(vs)))
    r = run(p2, ims)
    oa = {}
    for c in range(8):
        o = np.asarray(r[c]["oT"])
        for u in range(2):
            oa[divmod(2 * c + u, 8)] = o[u]
    oT_chunks = []
    for c in range(8):
        b, ch = c // 4, c % 4
        oaT = np.concatenate([oa[(b, h)][:, ch * T:(ch + 1) * T] for h in range(8)], 0)
        oT_chunks.append(np.concatenate([oaT, obT[c]], 0))
    P0 = dict(P)
    P0["w_out"] = P["w_out_ab"][0]
    return _layer_tail(xc, oT_chunks, 0, P0, S, final=False)


def _layer1(hc, P, S):
    T = S // 4
    NR = S // 512
    p4 = _prog(("p4", T), build_p4, T)
    ims = []
    for c in range(8):
        ch = c % 4
        halo = np.zeros((128, D), np.float32) if ch == 0 else hc[c - 1][-128:]
        ims.append(dict(x=np.concatenate([halo, hc[c]], 0), g=P["norm_mix"][1], w=P["w_in_cd"][0], wuq=P["w_uq"][0], wiq=P["w_iq"][0],
                        ncq=P["norm_cq"][0], nki=P["norm_kidx"][0], cw=P["conv_w"][0], alog=P["a_log"][0], dtb=P["dt_bias"][0]))
    r = run(p4, ims)
    cat = lambda key, b, axis: np.concatenate([np.asarray(r[b * 4 + ch][key]) for ch in range(4)], axis)
    p5 = _prog(("p5", S), build_p5, S)
    ims = []
    per_b = {}
    for b in range(2):
        per_b[b] = dict(kiT=cat("kiT", b, 1), kcT=cat("kcT", b, 1).reshape(8, 128, S), vc=cat("vc", b, 0),
                        qiT=cat("qiT", b, 1), wi=cat("wi", b, 0), qcT=cat("qcT", b, 1))
    for c in range(8):
        b, rr = c // 4, c % 4
        pb = per_b[b]
        tiles = [4 * m + rr for m in range(NR)]
        cm, cm01, BT, b31 = _dsa_consts(rr, P["rel_bias"])
        qiT = np.stack([pb["qiT"][:, i * 128:(i + 1) * 128].reshape(16, 64, 128).transpose(1, 0, 2) for i in tiles])
        wi = np.stack([pb["wi"][i * 128:(i + 1) * 128] for i in tiles])
        qc = np.stack([pb["qcT"][:, i * 128:(i + 1) * 128].reshape(8, 128, 128) for i in tiles])
        ims.append(dict(kiT=pb["kiT"], qiT=np.ascontiguousarray(qiT), wi=np.ascontiguousarray(wi), cm=cm, cm01=cm01, kcT=pb["kcT"], vc=pb["vc"],
                        qc=np.ascontiguousarray(qc), BT=BT, b31=b31))
    r5 = run(p5, ims)
    ocT = [np.zeros((1024, S), _BF) for _ in range(2)]
    for c in range(8):
        b, rr = c // 4, c % 4
        o = np.asarray(r5[c]["ocT"])
        for m in range(NR):
            i = 4 * m + rr
            ocT[b][:, i * 128:(i + 1) * 128] = o[m].reshape(1024, 128)
    p6 = _prog(("p6", S), build_p6, S, 2)
    qkv = {b: cat("qkvT", b, 1) for b in range(2)}
    gb = {b: cat("gb", b, 1) for b in range(2)}
    sz = {b: cat("szT", b, 1) for b in range(2)}
    ims = []
    for c in range(8):
        d = {k_: [] for k_ in ("qT", "kT", "vT", "g", "beta", "szT")}
        for u in range(2):
            b, h = divmod(2 * c + u, 8)
            d["qT"].append(qkv[b][h * 128:(h + 1) * 128])
            d["kT"].append(qkv[b][1024 + h * 128:1024 + (h + 1) * 128])
            d["vT"].append(qkv[b][2048 + h * 128:2048 + (h + 1) * 128])
            d["g"].append(gb[b][8 + h])
            d["beta"].append(gb[b][h])
            d["szT"].append(sz[b][h * 128:(h + 1) * 128])
        im = {k_: np.ascontiguousarray(np.stack(v_)) for k_, v_ in d.items()}
        im["gno"] = P["norm_delta_out"][0]
        ims.append(im)
    r6 = run(p6, ims)
    odT = [np.zeros((1024, S), _BF) for _ in range(2)]
    for c in range(8):
        o = np.asarray(r6[c]["odT"])
        for u in range(2):
            b, h = divmod(2 * c + u, 8)
            odT[b][h * 128:(h + 1) * 128] = o[u]
    oT_chunks = []
    for c in range(8):
        b, ch = c // 4, c % 4
        oT_chunks.append(np.ascontiguousarray(np.concatenate([ocT[b][:, ch * T:(ch + 1) * T], odT[b][:, ch * T:(ch + 1) * T]], 0)))
    P1 = dict(P)
    P1["w_out"] = P["w_out_cd"][0]
    return _layer_tail(hc, oT_chunks, 1, P1, S, final=True)


def kernel(**inp):
    P = {k: np.asarray(v) for k, v in inp.items()}
    x = P["x"]
    S = x.shape[1]
    T = S // 4
    xc = [np.ascontiguousarray(x[c // 4, (c % 4) * T:(c % 4 + 1) * T]) for c in range(8)]
    h = _layer0(xc, P, S)
    h = _layer1(h, P, S)
    out = np.stack([np.concatenate(h[b * 4:(b + 1) * 4], 0) for b in range(2)])
    return out.astype(np.float32)


C_CQ, C_KC, C_VC, C_KI, C_WI, C_QKV, C_BETA, C_A, C_Z = 0, 256, 1280, 2304, 2368, 2384, 5456, 5464, 5472
N_CD = 6496


def build_p4(T):
    kb = KB()
    NGp = T // 512
    x = kb.dram("x", [T + 128, D], F32, "ExternalInput")
    g = kb.dram("g", [D], F32, "ExternalInput")
    w = kb.dram("w", [D, N_CD], F32, "ExternalInput")
    wuq = kb.dram("wuq", [256, 1024], F32, "ExternalInput")
    wiq = kb.dram("wiq", [256, 1024], F32, "ExternalInput")
    ncq = kb.dram("ncq", [256], F32, "ExternalInput")
    nki = kb.dram("nki", [64], F32, "ExternalInput")
    cw = kb.dram("cw", [4, 3072], F32, "ExternalInput")
    alog = kb.dram("alog", [8], F32, "ExternalInput")
    dtb = kb.dram("dtb", [8], F32, "ExternalInput")
    o_qcT = kb.dram("qcT", [1024, T], BF16, "ExternalOutput")
    o_kcT = kb.dram("kcT", [1024, T], BF16, "ExternalOutput")
    o_vc = kb.dram("vc", [T, 1024], BF16, "ExternalOutput")
    o_qiT = kb.dram("qiT", [1024, T], BF16, "ExternalOutput")
    o_kiT = kb.dram("kiT", [64, T], BF16, "ExternalOutput")
    o_wi = kb.dram("wi", [T, 32], F32, "ExternalOutput")
    o_qkvT = kb.dram("qkvT", [3072, T], F32, "ExternalOutput")
    o_gb = kb.dram("gb", [16, T], F32, "ExternalOutput")
    o_szT = kb.dram("szT", [1024, T], BF16, "ExternalOutput")

    ident = emit_identity(kb)
    gain = load_gain(kb, g, D, "gain")
    nt = NormT(kb, ident, nbuf=1)
    wb = kb.sb([128, NCH, 3072], BF16, "wb")
    stage = [kb.sb([128, 512], F32, "wst%d" % i) for i in range(3)]
    ones_f = kb.sb([128, 128], F32, "ones_f")
    kb.op("pool", lambda e: e.memset(ones_f[:], 1.0), writes=[ones_f])

    def load_w(col_off, ncols):
        n = 0
        for c in range(NCH):
            for j0 in range(0, ncols, 512):
                jw = min(512, ncols - j0)
                st = stage[n % 3]
                kb.dma("sp" if n % 2 == 0 else "pool", st[:, :jw], w[c * 128:(c + 1) * 128, col_off + j0:col_off + j0 + jw], writes=[st])
                kb.op("dve", lambda e: e.tensor_scalar_mul(out=wb[:, c, j0:j0 + jw], in0=st[:, :jw], scalar1=gain[:, c:c + 1]),
                      reads=[st, gain], writes=[wb.sub((c, j0))])
                n += 1

    hnT = [kb.sb([128, NCH, 512], BF16, "hnT%d" % i) for i in range(2)]
    pm = [kb.ps([128, 512], F32, "pm%d" % i) for i in range(3)]
    pw_ = kb.ps([128, 512], F32, "pw_")
    npm = [0]

    def norm_group(gi, ntile=4):
        h = hnT[gi % 2]
        for j in range(ntile):
            r0 = 128 + gi * 512 + j * 128
            nt.run(x[r0:r0 + 128, :], h, slice(j * 128, (j + 1) * 128))
        return h

    def fm_mm(h, col0, ncols, ntok=512):
        p = pm[npm[0] % 3]
        npm[0] += 1
        for c in range(NCH):
            kb.op("pe", lambda e: e.matmul(p[0:ncols, 0:ntok], lhsT=wb[:, c, col0:col0 + ncols], rhs=h[:, c, 0:ntok],
                                           start=(c == 0), stop=(c == NCH - 1)), reads=[wb, h], writes=[p])
        return p

    load_w(0, 2384)
    gcq = load_gain(kb, ncq, 256, "gcq")
    gki = kb.sb([64, 1], F32, "gki")
    kb.dma("sp", gki[:], nki.rearrange("(p o) -> p o", o=1), writes=[gki], allow_slow_non_contiguous=True)
    wuq_b = load_w_scaled(kb, wuq, 256, 1024, "wuq_b", gcq, 128 ** -0.5, stage)
    wiq_b = load_w_scaled(kb, wiq, 256, 1024, "wiq_b", gcq, 1.0, stage)
    cq = kb.sb([128, 2, 512], F32, "cq")
    cqs = kb.sb([128, 2, 512], F32, "cqs")
    rsd = kb.sb([128, 512], F32, "rsd")
    cqn = kb.sb([128, 2, 512], BF16, "cqn")
    ob16 = [kb.sb([128, 512], BF16, "ob16_%d" % i) for i in range(3)]
    vo = [kb.sb([128, 1024], BF16, "vo%d" % i) for i in range(2)]
    wo_ = [kb.sb([128, 32], F32, "wo_%d" % i) for i in range(2)]
    kis = kb.sb([64, 512], F32, "kis")
    kiq = kb.sb([64, 512], F32, "kiq")
    n16 = [0]

    def out16(p, nrow, dst_ap, eng="act", ntok=512):
        o = ob16[n16[0] % 3]
        n16[0] += 1
        if eng == "act":
            kb.op("act", lambda e: e.copy(out=o[0:nrow, 0:ntok], in_=p[0:nrow, 0:ntok]), reads=[p], writes=[o])
        else:
            kb.op("dve", lambda e: e.tensor_copy(out=o[0:nrow, 0:ntok], in_=p[0:nrow, 0:ntok]), reads=[p], writes=[o])
        kb.dma("sp", dst_ap, o[0:nrow, 0:ntok], reads=[o], is_output=True)

    for gi in range(NGp):
        h = norm_group(gi)
        tsl = slice(gi * 512, (gi + 1) * 512)
        for c2 in range(2):
            p = fm_mm(h, C_CQ + c2 * 128, 128)
            kb.op("act", lambda e: e.copy(out=cq[:, c2, :], in_=p[:]), reads=[p], writes=[cq.sub(c2)])
            kb.op("act", lambda e: e.activation(out=cqs[:, c2, :], in_=p[:], func=AF.Square), reads=[p], writes=[cqs.sub(c2)])
        for c2 in range(2):
            kb.op("pe", lambda e: e.matmul(pw_[:], lhsT=ones_f[:], rhs=cqs[:, c2, :], start=(c2 == 0), stop=(c2 == 1)), reads=[ones_f, cqs], writes=[pw_])
        kb.op("act", lambda e: e.activation(out=rsd[:], in_=pw_[:], func=AF.Sqrt, bias=EPS, scale=1.0 / 256), reads=[pw_], writes=[rsd])
        kb.op("dve", lambda e: e.reciprocal(out=rsd[:], in_=rsd[:]), reads=[rsd], writes=[rsd])
        for c2 in range(2):
            kb.op("dve", lambda e: e.tensor_tensor(out=cqn[:, c2, :], in0=cq[:, c2, :], in1=rsd[:], op=ALU.mult), reads=[cq, rsd], writes=[cqn.sub(c2)])
        for which, wsb, dst in ((0, wuq_b, o_qcT), (1, wiq_b, o_qiT)):
            for jc in range(8):
                p = pm[npm[0] % 3]
                npm[0] += 1
                for c2 in range(2):
                    kb.op("pe", lambda e: e.matmul(p[:], lhsT=wsb[:, c2, jc * 128:(jc + 1) * 128], rhs=cqn[:, c2, :], start=(c2 == 0), stop=(c2 == 1)),
                          reads=[wsb, cqn], writes=[p])
                out16(p, 128, dst[jc * 128:(jc + 1) * 128, tsl], "act" if jc % 2 == 0 else "dve")
        for jc in range(8):
            p = fm_mm(h, C_KC + jc * 128, 128)
            out16(p, 128, o_kcT[jc * 128:(jc + 1) * 128, tsl], "act" if jc % 2 == 0 else "dve")
        p = fm_mm(h, C_KI, 64)
        kb.op("act", lambda e: e.copy(out=kis[:], in_=p[0:64, :]), reads=[p], writes=[kis])
        kb.op("act", lambda e: e.activation(out=kiq[:], in_=p[0:64, :], func=AF.Square), reads=[p], writes=[kiq])
        kb.op("pe", lambda e: e.matmul(pw_[0:64, :], lhsT=ones_f[0:64, 0:64], rhs=kiq[:], start=True, stop=True), reads=[ones_f, kiq], writes=[pw_])
        kb.op("act", lambda e: e.activation(out=kiq[:], in_=pw_[0:64, :], func=AF.Sqrt, bias=EPS, scale=1.0 / 64), reads=[pw_], writes=[kiq])
        kb.op("dve", lambda e: e.reciprocal(out=kiq[:], in_=kiq[:]), reads=[kiq], writes=[kiq])
        o = ob16[n16[0] % 3]
        n16[0] += 1
        kb.op("dve", lambda e: e.scalar_tensor_tensor(out=o[0:64, :], in0=kis[:], scalar=gki[:, 0:1], in1=kiq[:], op0=ALU.mult, op1=ALU.mult),
              reads=[kis, gki, kiq], writes=[o])
        kb.dma("sp", o_kiT[:, tsl], o[0:64, :], reads=[o], is_output=True)
        for j in range(4):
            ti = gi * 4 + j
            v_ = vo[ti % 2]
            for half in range(2):
                p = pm[npm[0] % 3]
                npm[0] += 1
                for c in range(NCH):
                    kb.op("pe", lambda e: e.matmul(p[:], lhsT=h[:, c, j * 128:(j + 1) * 128], rhs=wb[:, c, C_VC + half * 512:C_VC + (half + 1) * 512],
                                                   start=(c == 0), stop=(c == NCH - 1)), reads=[h, wb], writes=[p])
                kb.op("act" if half == 0 else "dve",
                      (lambda e: e.copy(out=v_[:, 0:512], in_=p[:])) if half == 0 else (lambda e: e.tensor_copy(out=v_[:, 512:1024], in_=p[:])),
                      reads=[p], writes=[v_.sub(half)])
            kb.dma("pool", o_vc[ti * 128:(ti + 1) * 128, :], v_[:], reads=[v_], is_output=True)
            p = pm[npm[0] % 3]
            npm[0] += 1
            for c in range(NCH):
                kb.op("pe", lambda e: e.matmul(p[:, 0:16], lhsT=h[:, c, j * 128:(j + 1) * 128], rhs=wb[:, c, C_WI:C_WI + 16],
                                               start=(c == 0), stop=(c == NCH - 1)), reads=[h, wb], writes=[p])
            w2 = wo_[ti % 2]
            kb.op("act", lambda e: e.activation(out=w2[:, 0:16], in_=p[:, 0:16], func=AF.Abs, scale=float(16 ** -0.5 * 64 ** -0.5)),
                  reads=[p], writes=[w2.sub(0)])
            kb.op("act", lambda e: e.activation(out=w2[:, 16:32], in_=p[:, 0:16], func=AF.Sign), reads=[p], writes=[w2.sub(1)])
            kb.dma("pool", o_wi[ti * 128:(ti + 1) * 128, :], w2[:], reads=[w2], is_output=True)

    load_w(C_QKV, 3072)
    cwt = kb.sb([128, 4, 24], F32, "cwt")
    for k_ in range(4):
        kb.dma("sp", cwt[:, k_, :], cw[k_].rearrange("(c p) -> p c", p=128), writes=[cwt.sub(k_)], allow_slow_non_contiguous=True)
    xc = [kb.sb([128, 515], F32, "xc%d" % i) for i in range(2)]
    carry = kb.sb([128, 24, 3], F32, "carry")
    acc = [kb.sb([128, 512], F32, "acc%d" % i) for i in range(2)]
    sq_ = rsd
    of32 = [kb.sb([128, 512], F32, "of32_%d" % i) for i in range(3)]
    nxc = 0
    for gi in range(-1, NGp):
        if gi < 0:
            h = hnT[1]
            nt.run(x[0:128, :], h, slice(0, 128))
            ntok = 128
        else:
            h = norm_group(gi)
            ntok = 512
        for cc in range(24):
            p = fm_mm(h, cc * 128, 128, ntok)
            xb = xc[nxc % 2]
            a_ = acc[nxc % 2]
            nxc += 1
            if gi >= 0:
                kb.op("dve", lambda e: e.tensor_copy(out=xb[:, 0:3], in_=carry[:, cc, :]), reads=[carry.sub(cc)], writes=[xb])
            kb.op("act", lambda e: e.copy(out=xb[:, 3:3 + ntok], in_=p[:, 0:ntok]), reads=[p], writes=[xb])
            kb.op("dve", lambda e: e.tensor_copy(out=carry[:, cc, :], in_=xb[:, ntok:ntok + 3]), reads=[xb], writes=[carry.sub(cc)])
            if gi < 0:
                continue
            kb.op("dve", lambda e: e.tensor_scalar_mul(out=a_[:], in0=xb[:, 3:515], scalar1=cwt[:, 3, cc:cc + 1]), reads=[xb, cwt], writes=[a_])
            for k_ in range(3):
                kb.op("dve", lambda e: e.scalar_tensor_tensor(out=a_[:], in0=xb[:, k_:k_ + 512], scalar=cwt[:, k_, cc:cc + 1], in1=a_[:],
                                                              op0=ALU.mult, op1=ALU.add), reads=[xb, cwt, a_], writes=[a_])
            o = of32[nxc % 3]
            kb.op("act", lambda e: e.activation(out=o[:], in_=a_[:], func=AF.Silu), reads=[a_], writes=[o])
            if cc < 16:
                kb.op("act", lambda e: e.activation(out=sq_[:], in_=o[:], func=AF.Square), reads=[o], writes=[sq_])
                kb.op("pe", lambda e: e.matmul(pw_[:], lhsT=ones_f[:], rhs=sq_[:], start=True, stop=True), reads=[ones_f, sq_], writes=[pw_])
                kb.op("act", lambda e: e.activation(out=sq_[:], in_=pw_[:], func=AF.Sqrt, bias=EPS, scale=1.0), reads=[pw_], writes=[sq_])
                kb.op("dve", lambda e: e.reciprocal(out=sq_[:], in_=sq_[:]), reads=[sq_], writes=[sq_])
                kb.op("dve", lambda e: e.tensor_tensor(out=o[:], in0=o[:], in1=sq_[:], op=ALU.mult), reads=[o, sq_], writes=[o])
            kb.dma("pool" if cc % 2 else "sp", o_qkvT[cc * 128:(cc + 1) * 128, gi * 512:(gi + 1) * 512], o[:], reads=[o], is_output=True)

    load_w(C_BETA, 1040)
    ab = kb.sb([16, 2], F32, "ab")
    kb.op("pool", lambda e: e.memset(ab[:], 0.0), writes=[ab])
    kb.dma("sp", ab[8:16, 0:1], dtb.rearrange("(p o) -> p o", o=1), reads=[], writes=[ab], allow_slow_non_contiguous=True)
    kb.dma("sp", ab[8:16, 1:2], alog.rearrange("(p o) -> p o", o=1), reads=[], writes=[ab], allow_slow_non_contiguous=True)
    nea = kb.sb([16, 1], F32, "nea")
    kb.op("act", lambda e: e.activation(out=nea[:], in_=ab[:, 1:2], func=AF.Exp), reads=[ab], writes=[nea])
    kb.op("dve", lambda e: e.tensor_scalar_mul(out=nea[:], in0=nea[:], scalar1=-1.0), reads=[nea], writes=[nea])
    gbt = [acc[0], acc[1]]
    gtm = [of32[0], of32[1]]
    for gi in range(NGp):
        h = norm_group(gi)
        tsl = slice(gi * 512, (gi + 1) * 512)
        p = fm_mm(h, 0, 16)
        t_ = gbt[gi % 2]
        g_ = gtm[gi % 2]
        kb.op("act", lambda e: e.activation(out=g_[0:16, :], in_=p[0:16, :], func=AF.Exp, bias=ab[:, 0:1]), reads=[p, ab], writes=[g_])
        kb.op("act", lambda e: e.activation(out=g_[0:16, :], in_=g_[0:16, :], func=AF.Ln, bias=1.0), reads=[g_], writes=[g_])
        kb.op("dve", lambda e: e.tensor_scalar_mul(out=g_[0:16, :], in0=g_[0:16, :], scalar1=nea[:, 0:1]), reads=[g_, nea], writes=[g_])
        kb.op("act", lambda e: e.activation(out=t_[0:16, :], in_=p[0:16, :], func=AF.Sigmoid), reads=[p], writes=[t_])
        kb.dma("sp", o_gb[0:8, tsl], t_[0:8, :], reads=[t_], is_output=True)
        kb.dma("sp", o_gb[8:16, tsl], g_[8:16, :], reads=[g_], is_output=True)
        for jc in range(8):
            p = fm_mm(h, 16 + jc * 128, 128)
            o = ob16[n16[0] % 3]
            n16[0] += 1
            kb.op("act", lambda e: e.activation(out=o[:], in_=p[:], func=AF.Silu), reads=[p], writes=[o])
            kb.dma("pool", o_szT[jc * 128:(jc + 1) * 128, tsl], o[:], reads=[o], is_output=True)
    return kb.finish()


def build_p6(S, NU=2, NBC=32):
    kb = KB()
    CH = 64
    BT = NBC * CH
    NBLK = S // BT
    qT = kb.dram("qT", [NU, 128, S], F32, "ExternalInput")
    kT = kb.dram("kT", [NU, 128, S], F32, "ExternalInput")
    vT = kb.dram("vT", [NU, 128, S], F32, "ExternalInput")
    gg = kb.dram("g", [NU, S], F32, "ExternalInput")
    bb = kb.dram("beta", [NU, S], F32, "ExternalInput")
    szT = kb.dram("szT", [NU, 128, S], BF16, "ExternalInput")
    gno = kb.dram("gno", [128], F32, "ExternalInput")
    odT = kb.dram("odT", [NU, 128, S], BF16, "ExternalOutput")

    identf = emit_identity(kb, F32)
    ones_f = kb.sb([128, 128], F32, "ones_f")
    kb.op("pool", lambda e: e.memset(ones_f[:], 1.0), writes=[ones_f])
    Lt = kb.sb([64, 64], F32, "Lt")
    kb.op("pool", lambda e: e.memset(Lt[:], 1.0), writes=[Lt])
    kb.op("pool", lambda e: e.affine_select(out=Lt[:], in_=Lt[:], pattern=[[1, 64]], compare_op=ALU.is_ge, fill=0.0, base=0,
                                             channel_multiplier=-1), reads=[Lt], writes=[Lt])
    gnt = kb.sb([128, 1], F32, "gnt")
    kb.dma("sp", gnt[:], gno.rearrange("(p o) -> p o", o=1), writes=[gnt], allow_slow_non_contiguous=True)

    B = [kb.ps([128, 512], F32, "B%d" % i) for i in range(8)]
    qb = [kb.sb([128, BT], F32, "qb%d" % i) for i in range(2)]
    kbk = [kb.sb([128, BT], F32, "kbk%d" % i) for i in range(2)]
    vb = [kb.sb([128, BT], F32, "vb%d" % i) for i in range(2)]
    szb = [kb.sb([128, BT], BF16, "szb%d" % i) for i in range(2)]
    gB = [kb.sb([64, NBC], F32, "gB%d" % i) for i in range(2)]
    bB = [kb.sb([64, NBC], F32, "bB%d" % i) for i in range(2)]
    gcB = [kb.sb([64, NBC], F32, "gcB%d" % i) for i in range(2)]
    egl = [kb.sb([128, NBC], F32, "egl%d" % i) for i in range(2)]
    ekg = [kb.sb([64, NBC], F32, "ekg%d" % i) for i in range(2)]
    sqe = [kb.sb([64, NBC], F32, "sqe%d" % i) for i in range(2)]
    bw = [kb.sb([64, NBC], F32, "bw%d" % i) for i in range(2)]
    nbt = [kb.sb([64, NBC], F32, "nbt%d" % i) for i in range(2)]
    ob = [kb.sb([128, BT], BF16, "ob%d" % i) for i in range(2)]
    rr = [kb.sb([64, 256], F32, "rr%d" % i) for i in range(4)]
    kg = [kb.sb([64, 128], F32, "kg%d" % i) for i in range(2)]
    dg = [kb.sb([64, 64], F32, "dg%d" % i) for i in range(2)]
    Dm = [kb.sb([64, 64], F32, "Dm%d" % i) for i in range(2)]
    CC = [kb.sb([64, 128], F32, "CC%d" % i) for i in range(4)]
    c0a = [kb.sb([64, 128], F32, "c0a%d" % i) for i in range(2)]
    aT = [kb.sb([64, 64], F32, "aT%d" % i) for i in range(2)]
    wT = [kb.sb([128, 64], F32, "wT%d" % i) for i in range(2)]
    vnew = [kb.sb([64, 128], F32, "vnew%d" % i) for i in range(2)]
    t1 = [kb.sb([64, 128], F32, "t1_%d" % i) for i in range(2)]
    ot = [kb.sb([64, 128], F32, "ot%d" % i) for i in range(2)]
    osq = kb.sb([64, 128], F32, "osq")
    oss = [kb.sb([64, 1], F32, "oss%d" % i) for i in range(2)]
    St = [kb.sb([128, 128], F32, "St%d" % i) for i in range(2)]
    nblk = 0
    nch = 0
    for u in range(NU):
        S_cur = St[0]
        kb.op("pool", lambda e: e.memset(S_cur[:], 0.0), writes=[S_cur])
        si = 0
        for blk in range(NBLK):
            k2 = nblk % 2
            nblk += 1
            tsl = slice(blk * BT, (blk + 1) * BT)
            kb.dma("sp", qb[k2][:], qT[u, :, tsl], writes=[qb[k2]])
            kb.dma("pool", kbk[k2][:], kT[u, :, tsl], writes=[kbk[k2]])
            kb.dma("sp", vb[k2][:], vT[u, :, tsl], writes=[vb[k2]])
            kb.dma("pool", szb[k2][:], szT[u, :, tsl], writes=[szb[k2]])
            kb.dma("sp", gB[k2][:], gg[u, tsl].rearrange("(n t) -> t n", t=CH), writes=[gB[k2]], allow_slow_non_contiguous=True)
            kb.dma("pool", bB[k2][:], bb[u, tsl].rearrange("(n t) -> t n", t=CH), writes=[bB[k2]], allow_slow_non_contiguous=True)
            pg = B[3]
            kb.op("pe", lambda e: e.matmul(pg[0:64, 0:NBC], lhsT=Lt[:], rhs=gB[k2][:], start=True, stop=True), reads=[Lt, gB[k2]], writes=[pg])
            kb.op("pe", lambda e: e.matmul(pg[:, 64:64 + NBC], lhsT=ones_f[0:64, :], rhs=gB[k2][:], start=True, stop=True), reads=[ones_f, gB[k2]], writes=[pg])
            kb.op("act", lambda e: e.copy(out=gcB[k2][:], in_=pg[0:64, 0:NBC]), reads=[pg], writes=[gcB[k2]])
            kb.op("act", lambda e: e.activation(out=egl[k2][:], in_=pg[:, 64:64 + NBC], func=AF.Exp), reads=[pg], writes=[egl[k2]])
            kb.op("dve", lambda e: e.tensor_tensor(out=ekg[k2][:], in0=pg[0:64, 64:64 + NBC], in1=gcB[k2][:], op=ALU.subtract), reads=[pg, gcB[k2]], writes=[ekg[k2]])
            kb.op("act", lambda e: e.activation(out=ekg[k2][:], in_=ekg[k2][:], func=AF.Exp), reads=[ekg[k2]], writes=[ekg[k2]])
            kb.op("act", lambda e: e.activation(out=sqe[k2][:], in_=gcB[k2][:], func=AF.Exp), reads=[gcB[k2]], writes=[sqe[k2]])
            kb.op("dve", lambda e: e.tensor_tensor(out=bw[k2][:], in0=sqe[k2][:], in1=bB[k2][:], op=ALU.mult), reads=[sqe[k2], bB[k2]], writes=[bw[k2]])
            kb.op("dve", lambda e: e.tensor_scalar_mul(out=sqe[k2][:], in0=sqe[k2][:], scalar1=float(128 ** -0.5)), reads=[sqe[k2]], writes=[sqe[k2]])
            kb.op("dve", lambda e: e.tensor_scalar_mul(out=nbt[k2][:], in0=bB[k2][:], scalar1=-1.0), reads=[bB[k2]], writes=[nbt[k2]])
            for n in range(NBC):
                c2 = nch % 2
                nch += 1
                cs = slice(n * CH, (n + 1) * CH)
                kTc, qTc, vTc = kbk[k2][:, cs], qb[k2][:, cs], vb[k2][:, cs]
                col = lambda t_: t_[:, n:n + 1]
                p0 = B[0]
                kb.op("pe", lambda e: e.transpose(out=p0[0:64, 0:128], in_=vTc, identity=identf[:]), reads=[vb[k2], identf], writes=[p0])
                kb.op("pe", lambda e: e.transpose(out=p0[0:64, 128:256], in_=kTc, identity=identf[:]), reads=[kbk[k2], identf], writes=[p0])
                r0 = rr[(nch * 2) % 4]
                r1 = rr[(nch * 2 + 1) % 4]
                kb.op("dve", lambda e: e.tensor_scalar_mul(out=r0[:, 0:128], in0=p0[0:64, 0:128], scalar1=col(bB[k2])), reads=[p0, bB[k2]], writes=[r0])
                kb.op("act", lambda e: e.activation(out=r0[:, 128:256], in_=p0[0:64, 128:256], func=AF.Copy, scale=col(bw[k2])), reads=[p0, bw[k2]], writes=[r0])
                kg_ = kg[c2]
                kb.op("act", lambda e: e.activation(out=kg_[:], in_=p0[0:64, 128:256], func=AF.Copy, scale=col(ekg[k2])), reads=[p0, ekg[k2]], writes=[kg_])
                dg_ = dg[c2]
                kb.op("dve", lambda e: e.tensor_scalar_mul(out=dg_[:], in0=identf[0:64, 0:64], scalar1=col(gcB[k2])), reads=[identf, gcB[k2]], writes=[dg_])
                p1 = B[1]
                kb.op("pe", lambda e: e.matmul(p1[0:64, 0:64], lhsT=ones_f[0:64, 0:64], rhs=dg_[:], start=True, stop=True), reads=[ones_f, dg_], writes=[p1])
                kb.op("pe", lambda e: e.matmul(p1[0:64, 64:128], lhsT=kTc, rhs=kTc, start=True, stop=True), reads=[kbk[k2]], writes=[p1])
                kb.op("pe", lambda e: e.matmul(p1[0:64, 128:192], lhsT=qTc, rhs=kTc, start=True, stop=True), reads=[qb[k2], kbk[k2]], writes=[p1])
                Dm_ = Dm[c2]
                kb.op("act", lambda e: e.activation(out=Dm_[:], in_=p1[0:64, 0:64], func=AF.Exp, scale=-1.0, bias=col(gcB[k2])), reads=[p1, gcB[k2]], writes=[Dm_])
                kb.op("pool", lambda e: e.affine_select(out=Dm_[:], in_=Dm_[:], pattern=[[-1, 64]], compare_op=ALU.is_ge, fill=0.0, base=0,
                                                         channel_multiplier=1), reads=[Dm_], writes=[Dm_])
                ca = c0a[c2]
                kb.op("dve", lambda e: e.scalar_tensor_tensor(out=ca[:, 0:64], in0=p1[0:64, 64:128], scalar=col(nbt[k2]), in1=Dm_[:], op0=ALU.mult, op1=ALU.mult),
                      reads=[p1, nbt[k2], Dm_], writes=[ca])
                kb.op("dve", lambda e: e.scalar_tensor_tensor(out=ca[:, 64:128], in0=p1[0:64, 128:192], scalar=float(128 ** -0.5), in1=Dm_[:], op0=ALU.mult, op1=ALU.mult),
                      reads=[p1, Dm_], writes=[ca])
                kb.op("pool", lambda e: e.affine_select(out=ca[:, 0:64], in_=ca[:, 0:64], pattern=[[-1, 64]], compare_op=ALU.is_gt, fill=0.0, base=0,
                                                         channel_multiplier=1), reads=[ca], writes=[ca])
                p2 = B[2]
                kb.op("pe", lambda e: e.transpose(out=p2[0:64, 0:64], in_=ca[:, 0:64], identity=identf[0:64, 0:64]), reads=[ca, identf], writes=[p2])
                kb.op("pe", lambda e: e.transpose(out=p2[0:64, 64:128], in_=ca[:, 64:128], identity=identf[0:64, 0:64]), reads=[ca, identf], writes=[p2])
                cc = CC[(nch * 2) % 4]
                cc2 = CC[(nch * 2 + 1) % 4]
                kb.op("act", lambda e: e.copy(out=cc[:, 0:64], in_=ca[:, 0:64]), reads=[ca], writes=[cc])
                kb.op("dve", lambda e: e.tensor_copy(out=cc[:, 64:128], in_=p2[0:64, 0:64]), reads=[p2], writes=[cc])
                aT_ = aT[c2]
                kb.op("act", lambda e: e.copy(out=aT_[:], in_=p2[0:64, 64:128]), reads=[p2], writes=[aT_])
                rc, rn = r0, r1
                ck, cn = cc, cc2
                for k_ in range(6):
                    p3 = B[3]
                    kb.op("pe", lambda e: e.matmul(p3[0:64, 0:256], lhsT=ck[:, 64:128], rhs=rc[:], start=True, stop=True), reads=[ck, rc], writes=[p3])
                    kb.op("dve", lambda e: e.tensor_tensor(out=rn[:], in0=p3[0:64, 0:256], in1=rc[:], op=ALU.add), reads=[p3, rc], writes=[rn])
                    rc, rn = rn, rc
                    if k_ < 5:
                        p4 = B[4]
                        kb.op("pe", lambda e: e.matmul(p4[0:64, 0:64], lhsT=ck[:, 64:128], rhs=ck[:, 0:64], start=True, stop=True), reads=[ck], writes=[p4])
                        kb.op("pe", lambda e: e.matmul(p4[0:64, 64:128], lhsT=ck[:, 0:64], rhs=ck[:, 64:128], start=True, stop=True), reads=[ck], writes=[p4])
                        kb.op("act", lambda e: e.copy(out=cn[:], in_=p4[0:64, 0:128]), reads=[p4], writes=[cn])
                        ck, cn = cn, ck
                kb.op("pe", lambda e: e.transpose(out=p0[:, 256:320], in_=rc[:, 128:256], identity=identf[0:64, 0:64]), reads=[rc, identf], writes=[p0])
                wT_ = wT[c2]
                kb.op("act", lambda e: e.copy(out=wT_[:], in_=p0[:, 256:320]), reads=[p0], writes=[wT_])
                p5 = B[5]
                kb.op("pe", lambda e: e.matmul(p5[0:64, 0:128], lhsT=wT_[:], rhs=S_cur[:], start=True, stop=True), reads=[wT_, S_cur], writes=[p5])
                kb.op("pe", lambda e: e.matmul(p5[0:64, 128:256], lhsT=qTc, rhs=S_cur[:], start=True, stop=True), reads=[qb[k2], S_cur], writes=[p5])
                vn = vnew[c2]
                kb.op("dve", lambda e: e.tensor_tensor(out=vn[:], in0=rc[:, 0:128], in1=p5[0:64, 0:128], op=ALU.subtract), reads=[rc, p5], writes=[vn])
                t1_ = t1[c2]
                kb.op("act", lambda e: e.activation(out=t1_[:], in_=p5[0:64, 128:256], func=AF.Copy, scale=col(sqe[k2])), reads=[p5, sqe[k2]], writes=[t1_])
                p6 = B[6]
                kb.op("pe", lambda e: e.matmul(p6[0:64, 0:128], lhsT=aT_[:], rhs=vn[:], start=True, stop=True), reads=[aT_, vn], writes=[p6])
                p7 = B[7]
                kb.op("pe", lambda e: e.matmul(p7[:, 0:128], lhsT=kg_[:], rhs=vn[:], start=True, stop=True), reads=[kg_, vn], writes=[p7])
                S_nxt = St[1 - si]
                kb.op("dve", lambda e: e.scalar_tensor_tensor(out=S_nxt[:], in0=S_cur[:], scalar=col(egl[k2]), in1=p7[:, 0:128], op0=ALU.mult, op1=ALU.add),
                      reads=[S_cur, egl[k2], p7], writes=[S_nxt])
                S_cur = S_nxt
                si = 1 - si
                o_ = ot[c2]
                kb.op("dve", lambda e: e.tensor_tensor(out=o_[:], in0=p6[0:64, 0:128], in1=t1_[:], op=ALU.add), reads=[p6, t1_], writes=[o_])
                ss_ = oss[c2]
                kb.op("act", lambda e: e.activation(out=osq[:], in_=o_[:], func=AF.Square, accum_out=ss_[:]), reads=[o_], writes=[osq, ss_])
                kb.op("act", lambda e: e.activation(out=ss_[:], in_=ss_[:], func=AF.Sqrt, bias=EPS, scale=1.0 / 128), reads=[ss_], writes=[ss_])
                kb.op("dve", lambda e: e.reciprocal(out=ss_[:], in_=ss_[:]), reads=[ss_], writes=[ss_])
                kb.op("dve", lambda e: e.tensor_scalar_mul(out=o_[:], in0=o_[:], scalar1=ss_[:, 0:1]), reads=[o_, ss_], writes=[o_])
                kb.op("pe", lambda e: e.transpose(out=p2[:, 128:192], in_=o_[:], identity=identf[0:64, 0:64]), reads=[o_, identf], writes=[p2])
                kb.op("dve", lambda e: e.scalar_tensor_tensor(out=ob[k2][:, cs], in0=p2[:, 128:192], scalar=gnt[:, 0:1], in1=szb[k2][:, cs], op0=ALU.mult, op1=ALU.mult),
                      reads=[p2, gnt, szb[k2]], writes=[ob[k2].sub(n)])
            kb.dma("sp", odT[u, :, tsl], ob[k2][:], reads=[ob[k2]], is_output=True)
    return kb.finish()


MARK = -2.0e30


def build_p5(S, TOPK=256):
    kb = KB()
    NR = S // 512
    NB = S // 128
    kiT = kb.dram("kiT", [64, S], BF16, "ExternalInput")
    qiT = kb.dram("qiT", [NR, 64, 16, 128], BF16, "ExternalInput")
    wi = kb.dram("wi", [NR, 128, 32], F32, "ExternalInput")
    cm = kb.dram("cm", [128, 512], F32, "ExternalInput")
    cm01 = kb.dram("cm01", [128, 512], BF16, "ExternalInput")
    kcT = kb.dram("kcT", [8, 128, S], BF16, "ExternalInput")
    vc = kb.dram("vc", [S, 1024], BF16, "ExternalInput")
    qc = kb.dram("qc", [NR, 8, 128, 128], BF16, "ExternalInput")
    BTd = kb.dram("BT", [8, 128, 16, 128], F32, "ExternalInput")
    b31 = kb.dram("b31", [128, 8], F32, "ExternalInput")
    ocT = kb.dram("ocT", [NR, 8, 128, 128], BF16, "ExternalOutput")
    MS = kb.nc.dram_tensor("MS", [NR, 128, NR * 512], BF16).ap()
    ms_t = [T(None) for _ in range(NR)]

    ident = emit_identity(kb)
    ones_b = kb.sb([128, 128], BF16, "ones_b")
    kb.op("pool", lambda e: e.memset(ones_b[:], 1.0), writes=[ones_b])
    kis = kb.sb([64, S], BF16, "kis")
    kb.dma("sp", kis[:], kiT, writes=[kis])
    cmt = kb.sb([128, 512], F32, "cmt")
    kb.dma("pool", cmt[:], cm, writes=[cmt])
    cm1 = kb.sb([128, 512], BF16, "cm1")
    kb.dma("pool", cm1[:], cm01, writes=[cm1])
    b31t = kb.sb([128, 8], F32, "b31t")
    kb.dma("sp", b31t[:], b31, writes=[b31t])
    work = kb.sb([128, S], F32, "work")
    mk = kb.sb([128, S], BF16, "mk")
    B = [kb.ps([128, 512], F32, "B%d" % i) for i in range(8)]
    qit = [kb.sb([64, 16, 128], BF16, "qit%d" % i) for i in range(2)]
    wit = [kb.sb([128, 32], F32, "wit%d" % i) for i in range(2)]
    rt = [kb.sb([128, 512], F32, "rt%d" % i) for i in range(2)]
    m8 = kb.sb([128, 8], F32, "m8")
    mts = [kb.sb([128, 512], BF16, "mts%d" % i) for i in range(3)]
    nps = 0
    nmt = 0
    for m in range(NR):
        q_, w_ = qit[m % 2], wit[m % 2]
        kb.dma("sp", q_[:], qiT[m], writes=[q_])
        kb.dma("pool", w_[:], wi[m], writes=[w_])
        nel = (m + 1) * 512
        for kg in range(m + 1):
            gsl = slice(kg * 512, (kg + 1) * 512)
            wk = work.sub(("g", kg))
            for h in range(16):
                ps = B[nps % 2]
                r_ = rt[nps % 2]
                nps += 1
                kb.op("pe", lambda e: e.matmul(ps[:], lhsT=q_[:, h, :], rhs=kis[:, gsl], start=True, stop=True), reads=[q_, kis], writes=[ps])
                kb.op("act", lambda e: e.activation(out=r_[:], in_=ps[:], func=AF.Relu, scale=w_[:, h:h + 1]), reads=[ps, w_], writes=[r_])
                if h == 0:
                    kb.op("dve", lambda e: e.tensor_scalar_mul(out=work[:, gsl], in0=r_[:], scalar1=w_[:, 16:17]), reads=[r_, w_], writes=[wk])
                else:
                    kb.op("dve", lambda e: e.scalar_tensor_tensor(out=work[:, gsl], in0=r_[:], scalar=w_[:, 16 + h:17 + h], in1=work[:, gsl],
                                                                  op0=ALU.mult, op1=ALU.add), reads=[r_, w_, wk], writes=[wk])
            if kg == m:
                kb.op("dve", lambda e: e.tensor_tensor(out=work[:, gsl], in0=work[:, gsl], in1=cmt[:], op=ALU.add), reads=[wk, cmt], writes=[wk])
        for rd in range(TOPK // 8):
            kb.op("dve", lambda e: e.max(out=m8[:], in_=work[:, 0:nel]), reads=[work], writes=[m8])
            kb.op("dve", lambda e: e.match_replace(out=work[:, 0:nel], in_to_replace=m8[:], in_values=work[:, 0:nel], imm_value=MARK),
                  reads=[work, m8], writes=[work])
        kb.op("dve", lambda e: e.tensor_single_scalar(out=mk[:, 0:nel], in_=work[:, 0:nel], scalar=-1.5e30, op=ALU.is_lt), reads=[work], writes=[mk])
        kb.op("dve", lambda e: e.tensor_tensor(out=mk[:, m * 512:nel], in0=mk[:, m * 512:nel], in1=cm1[:], op=ALU.mult), reads=[mk, cm1], writes=[mk])
        for kg in range(m + 1):
            pt = B[2 + kg % 2]
            ptv = bf16_view(pt)
            for jj in range(4):
                blk = kg * 4 + jj
                kb.op("pe", lambda e: e.transpose(out=ptv[:, jj * 128:(jj + 1) * 128], in_=mk[:, blk * 128:(blk + 1) * 128], identity=ident[:]),
                      reads=[mk, ident], writes=[pt])
            ms_ = mts[nmt % 3]
            nmt += 1
            if kg % 2 == 0:
                kb.op("act", lambda e: e.copy(out=ms_[:], in_=ptv[:, 0:512]), reads=[pt], writes=[ms_])
            else:
                kb.op("dve", lambda e: e.tensor_copy(out=ms_[:], in_=ptv[:, 0:512]), reads=[pt], writes=[ms_])
            kb.dma("pool", MS[m, :, kg * 512:(kg + 1) * 512], ms_[:], reads=[ms_], writes=[ms_t[m]])
    wbf = work[:].bitcast(BF16)
    mtb = [work.sub(("mt", 0)), work.sub(("mt", 1))]
    ks = kb.sb([128, S], BF16, "ks")
    vs = mk.sub("vs")
    vs.h = mk[:].rearrange("p (j d) -> p j d", d=128)
    bts = kb.sb([128, 16, 128], BF16, "bts")
    btf = kb.sb([128, 16, 128], F32, "btf")
    qct = [kb.sb([128, 128], BF16, "qct%d" % i) for i in range(2)]
    pt_ = [kb.sb([128, 512], BF16, "pt_%d" % i) for i in range(2)]
    pmt = [kb.sb([128, 512], BF16, "pmt%d" % i) for i in range(2)]
    rden = kb.sb([128, 128], F32, "rden")
    ost = [kb.sb([128, 128], BF16, "ost%d" % i) for i in range(2)]
    for h in range(8):
        kb.dma("sp", ks[:], kcT[h], writes=[ks])
        nv = max(1, NB // 16)
        for i in range(nv):
            j0, j1 = i * NB // nv, (i + 1) * NB // nv
            kb.dma("sp" if i % 2 == 0 else "pool", vs[:, j0:j1, :], vc[j0 * 128:j1 * 128, h * 128:(h + 1) * 128].rearrange("(j p) d -> p j d", p=128),
                   writes=[vs])
        kb.dma("pool", btf[:], BTd[h], writes=[btf])
        kb.op("dve", lambda e: e.tensor_copy(out=bts[:], in_=btf[:]), reads=[btf], writes=[bts])
        items = [(m, kg) for m in range(NR) for kg in range(m + 1)]

        def stage1(ii):
            m, kg = items[ii]
            nz = h * len(items) + ii
            k2 = (h * NR + m) % 2
            if kg == 0:
                nel = (m + 1) * 512
                kb.dma("sp", wbf[:, k2 * S:k2 * S + nel], MS[m, :, 0:nel], reads=[ms_t[m]], writes=[mtb[k2]])
                kb.dma("pool", qct[k2][:], qc[m, h], writes=[qct[k2]])
            qc_ = qct[k2]
            near = (m - kg) <= 3
            pz, p_, pm_ = B[4 + nz % 2], pt_[nz % 2], pmt[nz % 2]
            for jj in range(4):
                j = kg * 4 + jj
                kb.op("pe", lambda e: e.matmul(pz[:, jj * 128:(jj + 1) * 128], lhsT=ks[:, j * 128:(j + 1) * 128], rhs=qc_[:], start=True, stop=not near),
                      reads=[ks, qc_], writes=[pz])
                if near:
                    e_ = 4 * (m - kg) + (3 - jj)
                    kb.op("pe", lambda e: e.matmul(pz[:, jj * 128:(jj + 1) * 128], lhsT=ident[:], rhs=bts[:, e_, :], start=False, stop=True),
                          reads=[ident, bts], writes=[pz])
            if near:
                kb.op("act", lambda e: e.activation(out=p_[:], in_=pz[:], func=AF.Exp), reads=[pz], writes=[p_])
            else:
                kb.op("act", lambda e: e.activation(out=p_[:], in_=pz[:], func=AF.Exp, bias=b31t[:, h:h + 1]), reads=[pz, b31t], writes=[p_])
            kb.op("dve", lambda e: e.tensor_tensor(out=pm_[:], in0=p_[:], in1=wbf[:, k2 * S + kg * 512:k2 * S + (kg + 1) * 512], op=ALU.mult),
                  reads=[p_, mtb[k2]], writes=[pm_])

        def stage2(ii):
            m, kg = items[ii]
            nz = h * len(items) + ii
            k2 = (h * NR + m) % 2
            pm_ = pmt[nz % 2]
            po, pden = B[6], B[7]
            ngrp = m + 1
            for jj in range(4):
                j = kg * 4 + jj
                first = (kg == 0 and jj == 0)
                last = (kg == ngrp - 1 and jj == 3)
                kb.op("pe", lambda e: e.matmul(po[:, 0:128], lhsT=vs[:, j, :], rhs=pm_[:, jj * 128:(jj + 1) * 128], start=first, stop=last),
                      reads=[vs, pm_], writes=[po])
                kb.op("pe", lambda e: e.matmul(pden[:, 0:128], lhsT=ones_b[:], rhs=pm_[:, jj * 128:(jj + 1) * 128], start=first, stop=last),
                      reads=[ones_b, pm_], writes=[pden])
            if kg == ngrp - 1:
                kb.op("dve", lambda e: e.reciprocal(out=rden[:], in_=pden[:, 0:128]), reads=[pden], writes=[rden])
                o_ = ost[k2]
                kb.op("dve", lambda e: e.tensor_tensor(out=o_[:], in0=po[:, 0:128], in1=rden[:], op=ALU.mult), reads=[po, rden], writes=[o_])
                kb.dma("pool", ocT[m, h], o_[:], reads=[o_], is_output=True)

        stage1(0)
        for ii in range(len(items)):
            if ii + 1 < len(items):
                stage1(ii + 1)
            stage2(ii)
    return kb.finish()


def _t5_bucket_np(dist):
    n = np.maximum(dist, 0)
    nf = np.maximum(n, 1).astype(np.float32)
    lr = np.log(nf / np.float32(16)) / np.float32(math.log(2048 / 16))
    large = 16 + (lr * np.float32(16)).astype(np.int32)
    return np.where(n < 16, n, np.minimum(large, 31))


def _dsa_consts(r, rel_bias):
    t = np.arange(128)[:, None]
    sp = np.arange(512)[None, :]
    ok = sp <= 128 * r + t
    cm = np.where(ok, 0.0, -1.0e30).astype(np.float32)
    cm01 = ok.astype(np.float32).astype(_BF)
    s = np.arange(128)[:, None, None]
    e = np.arange(16)[None, :, None]
    tt = np.arange(128)[None, None, :]
    dist = 128 * (e - 3 + r) + tt - s
    bucket = _t5_bucket_np(dist)
    BT = np.ascontiguousarray(np.transpose(rel_bias[bucket], (3, 0, 1, 2)))
    BT = np.where((dist >= 0)[None], BT, 0.0).astype(np.float32)
    b31 = np.ascontiguousarray(np.broadcast_to(rel_bias[31][None, :], (128, 8))).astype(np.float32)
    return cm, cm01, BT, b31
```
